# Optimizing a Trainium2 kernel written in Bass

```python
import jax, jax.numpy as jnp
from jax import lax
import numpy as np

D_MODEL = 2048
BATCH = 2
SEQ = 16384
DEPTH = 1

GLA_HEADS = 4
GLA_DK = 128
GLA_DV = 256
GLA_WK = GLA_HEADS * GLA_DK
GLA_WV = GLA_HEADS * GLA_DV
GLA_GATE_RANK = 16
GLA_TAU = 16.0
GLA_CHUNK = 64

MOBA_HEADS = 8
MOBA_DH = 128
MOBA_W = MOBA_HEADS * MOBA_DH
MOBA_BLOCK = 256
MOBA_TOPK = 3
MOBA_QCHUNK = 64

NORM_EPS = 1e-6
NEG_BIG = -1e30

SPLIT_SIZES = (GLA_WK, GLA_WK, GLA_WV, GLA_GATE_RANK, GLA_WV,
               MOBA_W, MOBA_W, MOBA_W, MOBA_W, D_MODEL, D_MODEL)
PROJ_WIDTH = sum(SPLIT_SIZES)
SPLIT_POINTS = tuple(int(v) for v in np.cumsum(SPLIT_SIZES)[:-1])

kernel_name = "hybrid_gla_moba_gated_block"


def rms_norm(x, g):
    xf = x.astype(jnp.float32)
    y = xf * lax.rsqrt(jnp.mean(xf * xf, axis=-1, keepdims=True) + NORM_EPS)
    return (y * g.astype(jnp.float32)).astype(x.dtype)


def alibi_slopes(n_heads):
    return jnp.exp2(-8.0 * jnp.arange(1, n_heads + 1, dtype=jnp.float32) / n_heads)


def gla_chunked(q, k, v, log_a):
    B, S, H, dk = q.shape
    dv = v.shape[-1]
    C = GLA_CHUNK
    n = S // C
    f32 = jnp.float32

    def to_chunks(t):
        return t.astype(f32).reshape(B, n, C, H, t.shape[-1]).transpose(1, 0, 3, 2, 4)

    qs, ks, vs, gs = to_chunks(q), to_chunks(k), to_chunks(v), to_chunks(log_a)
    causal = jnp.tril(jnp.ones((C, C), dtype=bool))

    def step(state, inp):
        qc, kc, vc, gc = inp
        b = jnp.cumsum(gc, axis=2)
        o_inter = jnp.einsum('bhcd,bhde->bhce', qc * jnp.exp(b), state)
        diff = b[:, :, :, None, :] - b[:, :, None, :, :]
        decay = jnp.exp(jnp.where(causal[:, :, None], diff, -jnp.inf))
        attn = jnp.sum(qc[:, :, :, None, :] * kc[:, :, None, :, :] * decay, axis=-1)
        o_intra = jnp.einsum('bhij,bhje->bhie', attn, vc)
        b_last = b[:, :, -1:, :]
        state = (jnp.exp(b_last[:, :, 0, :, None]) * state
                 + jnp.einsum('bhcd,bhce->bhde', kc * jnp.exp(b_last - b), vc))
        return state, o_inter + o_intra

    s0 = jnp.zeros((B, H, dk, dv), f32)
    _, o = lax.scan(step, s0, (qs, ks, vs, gs))
    return o.transpose(1, 0, 3, 2, 4).reshape(B, S, H, dv).astype(v.dtype)


def moba_attention(q, k, v):
    B, H, S, dh = q.shape
    f32 = jnp.float32
    S_pad = -(-S // MOBA_BLOCK) * MOBA_BLOCK
    pad = S_pad - S
    padw = ((0, 0), (0, 0), (0, pad), (0, 0))
    q, k, v = jnp.pad(q, padw), jnp.pad(k, padw), jnp.pad(v, padw)
    nb = S_pad // MOBA_BLOCK
    n_sel = min(MOBA_TOPK, nb)
    kb = k.reshape(B, H, nb, MOBA_BLOCK, dh)
    vb = v.reshape(B, H, nb, MOBA_BLOCK, dh)

    k_mean = jnp.mean(kb.astype(f32), axis=3)
    gate = jnp.einsum('bhsd,bhnd->bhsn', q.astype(f32), k_mean)
    q_blk = jnp.arange(S_pad) // MOBA_BLOCK
    past = jnp.arange(nb)[None, :] < q_blk[:, None]
    gate = jnp.where(past[None, None], gate, -jnp.inf)
    _, sel = lax.top_k(gate, n_sel)

    QC = MOBA_QCHUNK
    n_qc = S_pad // QC
    q_c = q.reshape(B, H, n_qc, QC, dh).transpose(2, 0, 1, 3, 4)
    sel_c = sel.reshape(B, H, n_qc, QC, n_sel).transpose(2, 0, 1, 3, 4)
    slopes = alibi_slopes(H)
    scale = dh ** -0.5
    bi = jnp.arange(B)[:, None, None, None]
    hi = jnp.arange(H)[None, :, None, None]
    offs = jnp.arange(MOBA_BLOCK)

    def attend(inp):
        ci, qc, selc = inp
        t = ci * QC + jnp.arange(QC)
        own = (ci * QC) // MOBA_BLOCK
        k_own = lax.dynamic_index_in_dim(kb, own, axis=2, keepdims=False)
        v_own = lax.dynamic_index_in_dim(vb, own, axis=2, keepdims=False)
        k_g = kb[bi, hi, selc]
        v_g = vb[bi, hi, selc]
        s_sel = jnp.einsum('bhqd,bhqnjd->bhqnj', qc, k_g,
                           preferred_element_type=f32) * scale
        s_own = jnp.einsum('bhqd,bhjd->bhqj', qc, k_own,
                           preferred_element_type=f32) * scale
        pos_sel = selc[..., None] * MOBA_BLOCK + offs
        pos_own = own * MOBA_BLOCK + offs
        dist_sel = (t[None, None, :, None, None] - pos_sel).astype(f32)
        dist_own = (t[:, None] - pos_own[None, :]).astype(f32)
        s_sel = s_sel - slopes[None, :, None, None, None] * dist_sel
        s_own = s_own - slopes[None, :, None, None] * dist_own
        valid_sel = jnp.arange(n_sel)[None, :] < (t // MOBA_BLOCK)[:, None]
        s_sel = jnp.where(valid_sel[None, None, :, :, None], s_sel, NEG_BIG)
        s_own = jnp.where((pos_own[None, :] <= t[:, None])[None, None], s_own, NEG_BIG)
        scores = jnp.concatenate([s_sel.reshape(B, H, QC, n_sel * MOBA_BLOCK), s_own], axis=-1)
        p = jax.nn.softmax(scores, axis=-1)
        p_sel = p[..., :n_sel * MOBA_BLOCK].reshape(B, H, QC, n_sel, MOBA_BLOCK).astype(v.dtype)
        p_own = p[..., n_sel * MOBA_BLOCK:].astype(v.dtype)
        out = (jnp.einsum('bhqnj,bhqnjd->bhqd', p_sel, v_g, preferred_element_type=f32)
               + jnp.einsum('bhqj,bhjd->bhqd', p_own, v_own, preferred_element_type=f32))
        return out.astype(v.dtype)

    o = lax.map(attend, (jnp.arange(n_qc), q_c, sel_c))
    o = o.transpose(1, 2, 0, 3, 4).reshape(B, H, S_pad, dh)
    return o[:, :, :S]


def setup_inputs(seed: int = 0) -> dict:
    key = jax.random.key(seed)
    ks = jax.random.split(key, 12)
    nrm = jax.random.normal
    f32 = jnp.float32
    x = nrm(ks[0], (BATCH, SEQ, D_MODEL), f32)
    norm_g = 1.0 + 0.02 * nrm(ks[1], (DEPTH, D_MODEL), f32)
    w_in = nrm(ks[2], (DEPTH, D_MODEL, PROJ_WIDTH), f32) * D_MODEL ** -0.5
    w_gla_gate = nrm(ks[3], (DEPTH, GLA_GATE_RANK, GLA_WK), f32) * GLA_GATE_RANK ** -0.5
    b_gla_gate = 0.1 * nrm(ks[4], (DEPTH, GLA_WK), f32)
    gla_out_g = 1.0 + 0.02 * nrm(ks[5], (DEPTH, GLA_DV), f32)
    q_norm_g = 1.0 + 0.02 * nrm(ks[6], (DEPTH, MOBA_DH), f32)
    k_norm_g = 1.0 + 0.02 * nrm(ks[7], (DEPTH, MOBA_DH), f32)
    w_branch_gla = nrm(ks[8], (DEPTH, GLA_WV, D_MODEL), f32) * GLA_WV ** -0.5
    w_branch_moba = nrm(ks[9], (DEPTH, MOBA_W, D_MODEL), f32) * MOBA_W ** -0.5
    w_out = nrm(ks[10], (DEPTH, D_MODEL, D_MODEL), f32) * D_MODEL ** -0.5
    return {"x": x, "norm_g": norm_g, "w_in": w_in, "w_gla_gate": w_gla_gate,
            "b_gla_gate": b_gla_gate, "gla_out_g": gla_out_g, "q_norm_g": q_norm_g,
            "k_norm_g": k_norm_g, "w_branch_gla": w_branch_gla,
            "w_branch_moba": w_branch_moba, "w_out": w_out}


def reference(x, norm_g, w_in, w_gla_gate, b_gla_gate, gla_out_g, q_norm_g, k_norm_g,
              w_branch_gla, w_branch_moba, w_out):
    B, S, _ = x.shape
    for layer in range(DEPTH):
        h = rms_norm(x, norm_g[layer])
        proj = h @ w_in[layer]
        (g_q, g_k, g_v, g_lr, g_silu, m_q, m_k, m_v, m_silu,
         gate_a, gate_b) = jnp.split(proj, SPLIT_POINTS, axis=-1)

        q = g_q.reshape(B, S, GLA_HEADS, GLA_DK) * (GLA_DK ** -0.5)
        k = g_k.reshape(B, S, GLA_HEADS, GLA_DK)
        v = g_v.reshape(B, S, GLA_HEADS, GLA_DV)
        log_a = jax.nn.log_sigmoid(
            (g_lr @ w_gla_gate[layer] + b_gla_gate[layer]).astype(jnp.float32)) / GLA_TAU
        log_a = log_a.reshape(B, S, GLA_HEADS, GLA_DK)
        o_gla = gla_chunked(q, k, v, log_a)
        o_gla = rms_norm(o_gla, gla_out_g[layer]).reshape(B, S, GLA_WV) * jax.nn.silu(g_silu)
        z_gla = o_gla @ w_branch_gla[layer]

        mq = rms_norm(m_q.reshape(B, S, MOBA_HEADS, MOBA_DH), q_norm_g[layer]).transpose(0, 2, 1, 3)
        mk = rms_norm(m_k.reshape(B, S, MOBA_HEADS, MOBA_DH), k_norm_g[layer]).transpose(0, 2, 1, 3)
        mv = m_v.reshape(B, S, MOBA_HEADS, MOBA_DH).transpose(0, 2, 1, 3)
        o_moba = moba_attention(mq, mk, mv)
        o_moba = o_moba.transpose(0, 2, 1, 3).reshape(B, S, MOBA_W) * jax.nn.silu(m_silu)
        z_moba = o_moba @ w_branch_moba[layer]

        merged = jax.nn.sigmoid(gate_a) * z_gla + jax.nn.sigmoid(gate_b) * z_moba
        x = x + (merged @ w_out[layer]).astype(x.dtype)
    return x
```

```python
import numpy as np
import ml_dtypes
import concourse.bass as bass
import concourse.mybir as mybir
from concourse.bass_utils import run_bass_kernel_spmd

F32 = mybir.dt.float32
BF16 = mybir.dt.bfloat16
AF = mybir.ActivationFunctionType
ALU = mybir.AluOpType
AX = mybir.AxisListType

D = 2048
NCH = 16
PROJ = 11280
C_GQ, C_GK, C_GV, C_LR, C_GS, C_MQ, C_MK, C_MV, C_MS, C_GA, C_GB = (
    0, 512, 1024, 2048, 2064, 3088, 4112, 5136, 6160, 7184, 9232)
EPS = 1e-6
NEG = -1.0e30


class Tok:
    __slots__ = ("name", "w", "r", "dsem")

    def __init__(self, name):
        self.name = name
        self.w = {}
        self.r = {}
        self.dsem = None


class Prog:
    ENG = ("pe", "act", "dve", "pool", "sp")

    def __init__(self, nc):
        self.nc = nc
        self.q = {e: [] for e in self.ENG}
        self.cnt = {e: 0 for e in self.ENG}
        self.esem = {e: nc.alloc_semaphore("es_" + e) for e in self.ENG}
        self.seen = {e: {} for e in self.ENG}
        self.dsems = []
        self.retired = []
        self.nsem = 0

    def _deps(self, reads, writes):
        deps = {}
        for t in reads:
            for s, v in t.w.items():
                if deps.get(s, 0) < v:
                    deps[s] = v
        for t in writes:
            for s, v in t.w.items():
                if deps.get(s, 0) < v:
                    deps[s] = v
            for s, v in t.r.items():
                if deps.get(s, 0) < v:
                    deps[s] = v
        return deps

    def _waits(self, e, deps, skip_own=False):
        waits = []
        seen = self.seen[e]
        own = self.esem[e]
        for s, v in deps.items():
            if skip_own and s is own:
                continue
            if seen.get(s, 0) < v:
                seen[s] = v
                waits.append((s, v))
        return waits

    def op(self, e, fn, reads=(), writes=(), wadd=()):
        allw = tuple(writes) + tuple(wadd)
        deps = self._deps(reads, allw)
        waits = self._waits(e, deps, skip_own=(e == "pe"))
        if self.cnt[e] >= 30000:
            self.retired.append((self.esem[e], self.cnt[e]))
            self.nsem += 1
            self.esem[e] = self.nc.alloc_semaphore("es_%s_%d" % (e, self.nsem))
            self.cnt[e] = 0
        self.cnt[e] += 1
        c = self.cnt[e]
        sem = self.esem[e]

        def run(eng, waits=waits, fn=fn, sem=sem):
            for s, v in waits:
                eng.wait_ge(s, v)
            fn(eng).then_inc(sem, 1)
        self.q[e].append(run)
        for t in writes:
            t.w = {sem: c}
            t.r = {}
        for t in wadd:
            t.w[sem] = c
        for t in reads:
            t.r[sem] = c

    def dma(self, qe, out, in_, reads=(), writes=(), wadd=(), st=None):
        allw = tuple(writes) + tuple(wadd)
        if st is None:
            st = (allw + tuple(reads))[0]
        if st.dsem is None:
            st.dsem = [self.nc.alloc_semaphore("d_" + st.name), 0]
            self.dsems.append(st)
        sem, tot = st.dsem
        deps = self._deps(reads, allw)
        if tot > 0:
            deps[sem] = max(deps.get(sem, 0), tot)
        waits = self._waits(qe, deps)
        v = tot + 16
        st.dsem[1] = v

        def run(eng, waits=waits, sem=sem, out=out, in_=in_):
            for s, vv in waits:
                eng.wait_ge(s, vv)
            eng.dma_start(out=out, in_=in_).then_inc(sem, 16)
        self.q[qe].append(run)
        for t in writes:
            t.w = {sem: v}
            t.r = {}
        for t in wadd:
            t.w[sem] = v
        for t in reads:
            t.r[sem] = max(t.r.get(sem, 0), v)

    def barrier(self):
        for e in self.ENG:
            deps = {}
            for st in self.dsems:
                sem, tot = st.dsem
                deps[sem] = tot
            for f in self.ENG:
                if f != e and self.cnt[f] > 0:
                    deps[self.esem[f]] = self.cnt[f]
            for rs, rv in self.retired:
                deps[rs] = rv
            waits = self._waits(e, deps)

            def run(eng, waits=waits):
                for s, v in waits:
                    eng.wait_ge(s, v)
            self.q[e].append(run)

    def finish(self):
        deps = {}
        for st in self.dsems:
            sem, tot = st.dsem
            deps[sem] = tot
        for e in self.ENG:
            if e != "sp" and self.cnt[e] > 0:
                deps[self.esem[e]] = self.cnt[e]
        for rs, rv in self.retired:
            deps[rs] = rv
        waits = self._waits("sp", deps)

        def run(eng, waits=waits):
            for s, v in waits:
                eng.wait_ge(s, v)
        self.q["sp"].append(run)

    def emit(self):
        nc = self.nc
        q = self.q
        with nc.Block() as block:
            @block.tensor
            def _(eng):
                for f in q["pe"]:
                    f(eng)

            @block.scalar
            def _(eng):
                for f in q["act"]:
                    f(eng)

            @block.vector
            def _(eng):
                for f in q["dve"]:
                    f(eng)

            @block.gpsimd
            def _(eng):
                for f in q["pool"]:
                    f(eng)

            @block.sync
            def _(eng):
                for f in q["sp"]:
                    f(eng)


class Buf:
    def __init__(self, t, name):
        self.t = t
        self.k = Tok(name)

    def __getitem__(self, key):
        return self.t[key]


class Arena:
    def __init__(self, nc, words):
        self.t = nc.alloc_sbuf_tensor("arena", [128, words], F32)
        self.words = words
        self.off = 0

    def reset(self):
        self.off = 0

    def alloc(self, name, shape, dt):
        nb = 2 if dt == BF16 else 4
        free = int(np.prod(shape[1:]))
        w = (free * nb + 3) // 4
        w = (w + 7) // 8 * 8
        assert self.off + w <= self.words, (name, self.off, w, self.words)
        v = self.t[0:shape[0], self.off:self.off + w]
        self.off += w
        if dt == BF16:
            v = v.bitcast(BF16)
        v = v[:, 0:free]
        if len(shape) == 3:
            v = v.rearrange("p (a b) -> p a b", b=shape[2])
        return Buf(v, name)


def build(EXT, OWN, stop=99):
    nc = bass.Bass("TRN2", target_bir_lowering=False)
    P = Prog(nc)

    def done():
        P.finish()
        P.emit()
        return nc
    NT = EXT // 128
    NTO = OWN // 128
    NB = EXT // 256
    PRE = EXT - OWN
    assert OWN % 512 == 0 and EXT % 512 == 0 and NB <= 64

    def din(name, shape, dt=F32):
        return nc.dram_tensor(name, shape, dt, kind="ExternalInput").ap()

    x = din("x", [EXT, D])
    w_in = din("w_in", [D, PROJ])
    w_bg = din("w_bg", [1024, D])
    w_bm = din("w_bm", [1024, D])
    w_o = din("w_o", [D, D])
    c_ng = din("c_ng", [128, NCH])
    c_wga = din("c_wga", [17, 512])
    c_gout = din("c_gout", [128, 1024])
    c_qg = din("c_qg", [128, 2])
    c_bval = din("c_bval", [128, 64])
    c_id = din("c_id", [128, 128])
    c_u2 = din("c_u2", [128, 128])
    c_msk = din("c_msk", [128, 128])
    c_tri = din("c_tri", [128, 128])
    c_jrow = din("c_jrow", [128, 64])
    c_sc = din("c_sc", [128, 16])
    c_negt = din("c_negt", [128, 8 * NTO])
    y = nc.dram_tensor("y", [OWN, D], F32, kind="ExternalOutput").ap()

    import os as _os0
    _dbg = _os0.environ.get("KDEBUG", "0") == "1"

    def dscr(name, shape, dt=BF16):
        if _dbg:
            return Buf(nc.dram_tensor(name, shape, dt, kind="ExternalOutput").ap(), name)
        return Buf(nc.dram_tensor(name, shape, dt).ap(), name)

    HT = dscr("s_ht", [NCH, 128, EXT])
    KT = dscr("s_kt", [8, 128, EXT])
    VV = dscr("s_v", [EXT, 1024])
    GKV = dscr("s_gkv", [EXT, 1536])
    GKT = dscr("s_gkt", [4, 128, EXT])
    LRT = dscr("s_lrt", [16, EXT], F32)
    GQT = dscr("s_gqt", [4, 128, OWN])
    MQT = dscr("s_mqt", [8, 128, OWN])
    SGT = dscr("s_sgt", [32, 128, OWN])
    GS = dscr("s_gs", [OWN, 2048])
    OT = dscr("s_ot", [16, 128, OWN])
    MT = dscr("s_mt", [16, 128, OWN])
    dbg_outs = {}

    def csb(name, shape, dt):
        return Buf(nc.alloc_sbuf_tensor(name, shape, dt), name)
    uniq = [0]

    def sb(name, shape, dt):
        uniq[0] += 1
        return AR.alloc("%s_%d" % (name, uniq[0]), shape, dt)

    def ps(name, shape, dt=F32):
        return Buf(nc.alloc_psum_tensor(name, shape, dt), name)

    BIG0 = ps("big0", [128, 1024])
    BIG1 = ps("big1", [128, 1024])
    PA0 = ps("pa0", [128, 512])
    PA1 = ps("pa1", [128, 512])
    PTB = [ps("ptb0", [128, 1024], BF16), ps("ptb1", [128, 1024], BF16)]
    PTk = [PTB[0].k, PTB[1].k]

    class Bank:
        def __init__(self, ap, k):
            self.ap = ap
            self.k = k
    b0a, b0b, b1a, b1b = Tok("b0a"), Tok("b0b"), Tok("b1a"), Tok("b1b")
    banks = [Bank(PA0[:, :], PA0.k), Bank(PA1[:, :], PA1.k),
             Bank(BIG0[:, 0:512], b0a), Bank(BIG0[:, 512:1024], b0b),
             Bank(BIG1[:, 0:512], b1a), Bank(BIG1[:, 512:1024], b1b)]

    def const(name, src, shape, dt=F32, q="sp"):
        b = csb(name, shape, dt)
        P.dma(q, b[:], src, writes=[b.k])
        return b
    NG = const("ng", c_ng, [128, NCH])
    WGA = const("wga", c_wga, [17, 512])
    GOUT = const("gout", c_gout, [128, 1024])
    QG = const("qg", c_qg, [128, 2])
    BVAL = const("bval", c_bval, [128, 64])
    IDF = const("idf", c_id, [128, 128])
    U2 = const("u2", c_u2, [128, 128])
    MSK = const("msk", c_msk, [128, 128])
    TRIF = const("trif", c_tri, [128, 128])
    JROW = const("jrow", c_jrow, [128, 64])
    SC = const("sc", c_sc, [128, 16])
    NEGT = const("negt", c_negt, [128, 8 * NTO])
    IDB = csb("idb", [128, 128], BF16)
    TRIB = csb("trib", [128, 128], BF16)
    ONESB = csb("onesb", [128, 128], BF16)
    KM = csb("km", [128, 8, 64], F32)
    KMB = csb("kmb", [128, 8, 64], BF16)
    AR = Arena(nc, 47000)
    P.op("dve", lambda e: e.tensor_copy(out=IDB[:], in_=IDF[:]), reads=[IDF.k], writes=[IDB.k])
    P.op("dve", lambda e: e.tensor_copy(out=TRIB[:], in_=TRIF[:]), reads=[TRIF.k], writes=[TRIB.k])
    P.op("pool", lambda e: e.memset(ONESB[:], 1.0), writes=[ONESB.k])

    R = {}

    def alloc_gemm():
        P.barrier()
        AR.reset()
        R["WBIG"] = sb("wbig", [128, NCH, 2560], BF16)
        R["WSTG"] = [sb("wstg%d" % i, [128, 2560], F32) for i in range(2)]
        R["HTG"] = [sb("htg%d" % i, [128, NCH, 512], BF16) for i in range(2)]
    wstg_n = [0]

    def load_w(src, col_list, dst=None, nch=NCH, scale=True):
        dst = dst or R["WBIG"]
        WSTG = R["WSTG"]
        tot = sum(n for _, n in col_list)
        first = True
        for c in range(nch):
            st = WSTG[wstg_n[0] % 2]
            wstg_n[0] += 1
            off = 0
            for (c0, n) in col_list:
                P.dma("sp", st[:, off:off + n], src[c * 128:(c + 1) * 128, c0:c0 + n],
                      **({"writes": [st.k]} if off == 0 else {"wadd": [st.k]}))
                off += n
            if scale:
                fn = (lambda e, st=st, c=c: e.tensor_scalar(out=dst[:, c, 0:tot], in0=st[:, 0:tot],
                                                             scalar1=NG[:, c:c + 1], scalar2=None, op0=ALU.mult))
                rd = [st.k, NG.k]
            else:
                fn = (lambda e, st=st, c=c: e.tensor_copy(out=dst[:, c, 0:tot], in_=st[:, 0:tot]))
                rd = [st.k]
            if first:
                P.op("pool", fn, reads=rd, writes=[dst.k])
                first = False
            else:
                P.op("pool", fn, reads=rd, wadd=[dst.k])

    if stop == 0:
        return done()
    XT = [sb("xt%d" % i, [128, D], F32) for i in range(2)]
    JUNK = sb("junk", [128, D], BF16)
    HB = [sb("hb%d" % i, [128, D], BF16) for i in range(2)]
    SSQ = [sb("ssq%d" % i, [128, 1], F32) for i in range(2)]
    HTT = [sb("htt%d" % i, [128, NCH, 128], BF16) for i in range(2)]
    for i in range(NT):
        xt, hb, ssq, htt = XT[i % 2], HB[i % 2], SSQ[i % 2], HTT[i % 2]
        P.dma("sp", xt[:], x[i * 128:(i + 1) * 128, :], writes=[xt.k])
        P.op("act", lambda e, xt=xt, ssq=ssq: e.activation(out=JUNK[:], in_=xt[:], func=AF.Square, accum_out=ssq[:]),
             reads=[xt.k], writes=[JUNK.k, ssq.k])
        import os as _os
        _lv = int(_os.environ.get("PH1", "9"))
        if _lv < 2:
            continue
        P.op("dve", lambda e, ssq=ssq: e.tensor_scalar(out=ssq[:], in0=ssq[:], scalar1=1.0 / D, scalar2=EPS,
                                                       op0=ALU.mult, op1=ALU.add), reads=[ssq.k], writes=[ssq.k])
        P.op("act", lambda e, ssq=ssq: e.activation(out=ssq[:], in_=ssq[:], func=AF.Sqrt), reads=[ssq.k], writes=[ssq.k])
        P.op("dve", lambda e, ssq=ssq: e.reciprocal(out=ssq[:], in_=ssq[:]), reads=[ssq.k], writes=[ssq.k])
        P.op("dve", lambda e, xt=xt, hb=hb, ssq=ssq: e.tensor_scalar(out=hb[:], in0=xt[:], scalar1=ssq[:, 0:1],
                                                                     scalar2=None, op0=ALU.mult),
             reads=[xt.k, ssq.k], writes=[hb.k])
        if _lv < 3:
            continue
        for g4 in range(4):
            half = g4 % 2
            for j in range(4):
                c = g4 * 4 + j
                P.op("pe", lambda e, hb=hb, c=c, half=half, j=j: e.transpose(
                    out=PTB[half][:, j * 128:(j + 1) * 128], in_=hb[:, c * 128:(c + 1) * 128],
                    identity=IDB[:]), reads=[hb.k, IDB.k],
                    **({"writes": [PTk[half]]} if j == 0 else {"wadd": [PTk[half]]}))
            if _lv < 4:
                continue
            eng = "act" if g4 % 2 == 0 else "dve"
            if eng == "act":
                fn = lambda e, htt=htt, g4=g4, half=half: e.copy(
                    out=htt[:, g4 * 4:(g4 + 1) * 4, :], in_=PTB[half][:, 0:512].rearrange("p (a b) -> p a b", b=128))
            else:
                fn = lambda e, htt=htt, g4=g4, half=half: e.tensor_copy(
                    out=htt[:, g4 * 4:(g4 + 1) * 4, :], in_=PTB[half][:, 0:512].rearrange("p (a b) -> p a b", b=128))
            P.op(eng, fn, reads=[PTk[half]], **({"writes": [htt.k]} if g4 == 0 else {"wadd": [htt.k]}))
        import os as _os
        _m = _os.environ.get("HTMODE", "split")
        if _m == "one":
            P.dma("pool", HT[:, :, i * 128:(i + 1) * 128].rearrange("c p t -> p c t"), htt[:], reads=[htt.k],
                  wadd=[HT.k], st=htt.k)
        elif _m == "split":
            for c4 in range(4):
                P.dma("pool", HT[c4 * 4:(c4 + 1) * 4, :, i * 128:(i + 1) * 128].rearrange("c p t -> p c t"),
                      htt[:, c4 * 4:(c4 + 1) * 4, :], reads=[htt.k], wadd=[HT.k], st=htt.k)
        elif _m == "sp":
            for c4 in range(4):
                P.dma("sp", HT[c4 * 4:(c4 + 1) * 4, :, i * 128:(i + 1) * 128].rearrange("c p t -> p c t"),
                      htt[:, c4 * 4:(c4 + 1) * 4, :], reads=[htt.k], wadd=[HT.k], st=htt.k)

    if stop == 1:
        return done()
    bank_n = [0]

    def next_bank():
        b = banks[bank_n[0] % len(banks)]
        bank_n[0] += 1
        return b

    def load_htg(tok0, gi):
        htg = R["HTG"][gi % 2]
        P.dma("sp", htg[:], HT[:, :, tok0:tok0 + 512].rearrange("c p t -> p c t"), reads=[HT.k], writes=[htg.k])
        return htg

    def pass_fm(tok0, ntok, jobs):
        pend = None
        WBIG = R["WBIG"]
        ng_ = ntok // 512
        nxt = load_htg(tok0, 0)
        for g in range(ng_):
            htg = nxt
            if g + 1 < ng_:
                nxt = load_htg(tok0 + (g + 1) * 512, g + 1)
            for (woff, M, epi) in jobs:
                b = next_bank()
                for c in range(NCH):
                    P.op("pe", lambda e, b=b, c=c, htg=htg, woff=woff, M=M: e.matmul(
                        b.ap[0:M, :], WBIG[:, c, woff:woff + M], htg[:, c, :], start=(c == 0), stop=(c == NCH - 1)),
                        reads=[WBIG.k, htg.k], **({"writes": [b.k]} if c == 0 else {"wadd": [b.k]}))
                if pend is not None:
                    pend()
                pend = epi(g, b)
        if pend is not None:
            pend()

    def pass_tm(tok0, ntok, jobs):
        WBIG = R["WBIG"]
        ng_ = ntok // 512
        nxt = load_htg(tok0, 0)
        for g in range(ng_):
            htg = nxt
            if g + 1 < ng_:
                nxt = load_htg(tok0 + (g + 1) * 512, g + 1)
            for t4 in range(4):
                for (woff, N, epi) in jobs:
                    b = next_bank()
                    for c in range(NCH):
                        P.op("pe", lambda e, b=b, c=c, htg=htg, woff=woff, N=N, t4=t4: e.matmul(
                            b.ap[:, 0:N], htg[:, c, t4 * 128:(t4 + 1) * 128], WBIG[:, c, woff:woff + N],
                            start=(c == 0), stop=(c == NCH - 1)),
                            reads=[WBIG.k, htg.k], **({"writes": [b.k]} if c == 0 else {"wadd": [b.k]}))
                    epi(g * 4 + t4, b)

    cp_n = [0]

    def evac_copy(out_ap, in_ap, reads, writes=(), wadd=()):
        cp_n[0] += 1
        if cp_n[0] % 2 == 0:
            P.op("act", lambda e: e.copy(out=out_ap, in_=in_ap), reads=reads, writes=writes, wadd=wadd)
        else:
            P.op("dve", lambda e: e.tensor_copy(out=out_ap, in_=in_ap), reads=reads, writes=writes, wadd=wadd)

    alloc_gemm()
    FST = [sb("fst%d" % i, [128, 512], BF16) for i in range(4)]
    fst_n = [0]
    SQB = [sb("sqb%d" % i, [128, 512], BF16) for i in range(2)]
    RINV = [sb("rinv%d" % i, [128, 512], F32) for i in range(2)]
    KNF = [sb("knf%d" % i, [128, 512], F32) for i in range(2)]
    LRS = [sb("lrs%d" % i, [16, 512], F32) for i in range(2)]
    TST = [sb("tst%d" % i, [128, 2560], BF16) for i in range(2)]
    qk_n = [0]

    def epi_store_fm(dst, chunk, ntok_total, func=None):
        def epi(g, b):
            st = FST[fst_n[0] % 4]
            fst_n[0] += 1
            if func is None:
                evac_copy(st[:], b.ap, [b.k], writes=[st.k])
            else:
                P.op("act", lambda e: e.activation(out=st[:], in_=b.ap, func=func), reads=[b.k], writes=[st.k])
            P.dma("pool", dst[chunk, :, g * 512:(g + 1) * 512], st[:], reads=[st.k], wadd=[dst.k], st=st.k)
            return None
        return epi

    def epi_qknorm(dst, head, gcol, want_mean):
        def epi(g, b):
            i = qk_n[0] % 2
            qk_n[0] += 1
            sqb, rinv, knf = SQB[i], RINV[i], KNF[i]
            P.op("act", lambda e: e.activation(out=sqb[:], in_=b.ap, func=AF.Square), reads=[b.k], writes=[sqb.k])

            def deferred():
                b2 = next_bank()
                P.op("pe", lambda e: e.matmul(b2.ap, ONESB[:], sqb[:], start=True, stop=True),
                     reads=[ONESB.k, sqb.k], writes=[b2.k])
                P.op("dve", lambda e: e.tensor_scalar(out=rinv[:], in0=b2.ap, scalar1=1.0 / 128, scalar2=EPS,
                                                      op0=ALU.mult, op1=ALU.add), reads=[b2.k], writes=[rinv.k])
                P.op("act", lambda e: e.activation(out=rinv[:], in_=rinv[:], func=AF.Sqrt), reads=[rinv.k], writes=[rinv.k])
                P.op("dve", lambda e: e.reciprocal(out=rinv[:], in_=rinv[:]), reads=[rinv.k], writes=[rinv.k])
                st = FST[fst_n[0] % 4]
                fst_n[0] += 1
                if want_mean:
                    P.op("dve", lambda e: e.scalar_tensor_tensor(out=knf[:], in0=b.ap, scalar=QG[:, gcol:gcol + 1],
                                                                  in1=rinv[:], op0=ALU.mult, op1=ALU.mult),
                         reads=[b.k, QG.k, rinv.k], writes=[knf.k])
                    P.op("dve", lambda e: e.tensor_reduce(out=KM[:, head, 2 * g:2 * g + 2],
                                                          in_=knf[:].rearrange("p (a b) -> p a b", b=256),
                                                          axis=AX.X, op=ALU.add), reads=[knf.k], wadd=[KM.k])
                    P.op("pool", lambda e: e.tensor_copy(out=st[:], in_=knf[:]), reads=[knf.k], writes=[st.k])
                else:
                    P.op("dve", lambda e: e.scalar_tensor_tensor(out=st[:], in0=b.ap, scalar=QG[:, gcol:gcol + 1],
                                                                  in1=rinv[:], op0=ALU.mult, op1=ALU.mult),
                         reads=[b.k, QG.k, rinv.k], writes=[st.k])
                P.dma("pool", dst[head, :, g * 512:(g + 1) * 512], st[:], reads=[st.k], wadd=[dst.k], st=st.k)
            return deferred
        return epi

    load_w(w_in, [(C_MK, 1024), (C_GK, 512), (C_LR, 16)])
    lr_n = [0]

    def epi_lr(g, b):
        st = LRS[lr_n[0] % 2]
        lr_n[0] += 1
        evac_copy(st[:], b.ap[0:16, :], [b.k], writes=[st.k])
        P.dma("pool", LRT[:, g * 512:(g + 1) * 512], st[:], reads=[st.k], wadd=[LRT.k], st=st.k)
        return None
    jobsA = [(h * 128, 128, epi_qknorm(KT, h, 1, True)) for h in range(8)]
    jobsA += [(1024 + h * 128, 128, epi_store_fm(GKT, h, EXT)) for h in range(4)]
    jobsA += [(1536, 16, epi_lr)]
    pass_fm(0, EXT, jobsA)
    if stop == 2:
        return done()
    P.op("dve", lambda e: e.tensor_scalar(out=KMB[:], in0=KM[:], scalar1=1.0 / 256, scalar2=None, op0=ALU.mult),
         reads=[KM.k], writes=[KMB.k])

    load_w(w_in, [(C_MV, 1024), (C_GK, 512), (C_GV, 1024)])

    def mk_epi_tm(col0, N, last, dsts):
        def epi(ti, b):
            st = TST[ti % 2]
            evac_copy(st[:, col0:col0 + N], b.ap[:, 0:N], [b.k], **({"writes": [st.k]} if col0 == 0 else {"wadd": [st.k]}))
            if last:
                for (dst, s0, n) in dsts:
                    P.dma("pool", dst[ti * 128:(ti + 1) * 128, :], st[:, s0:s0 + n], reads=[st.k], wadd=[dst.k], st=st.k)
        return epi
    dstsB = [(VV, 0, 1024), (GKV, 1024, 1536)]
    jobsB = [(i * 512, 512, mk_epi_tm(i * 512, 512, i == 4, dstsB)) for i in range(5)]
    pass_tm(0, EXT, jobsB)

    if stop == 3:
        return done()
    load_w(w_in, [(C_MQ, 1024), (C_GQ, 512)])
    jobsD = [(h * 128, 128, epi_qknorm(MQT, h, 0, False)) for h in range(8)]
    jobsD += [(1024 + h * 128, 128, epi_store_fm(GQT, h, OWN)) for h in range(4)]
    pass_fm(PRE, OWN, jobsD)
    for gi, c0 in enumerate((C_GA, C_GB)):
        load_w(w_in, [(c0, 2048)])
        pass_fm(PRE, OWN, [(n * 128, 128, epi_store_fm(SGT, gi * 16 + n, OWN, func=AF.Sigmoid)) for n in range(16)])
    load_w(w_in, [(C_GS, 1024), (C_MS, 1024)])

    def mk_epi_silu(col0, last):
        def epi(ti, b):
            st = TST[ti % 2]
            P.op("act", lambda e: e.activation(out=st[:, col0:col0 + 512], in_=b.ap, func=AF.Silu), reads=[b.k],
                 **({"writes": [st.k]} if col0 == 0 else {"wadd": [st.k]}))
            if last:
                P.dma("pool", GS[ti * 128:(ti + 1) * 128, :], st[:, 0:2048], reads=[st.k], wadd=[GS.k], st=st.k)
        return epi
    pass_tm(PRE, OWN, [(i * 512, 512, mk_epi_silu(i * 512, i == 3)) for i in range(4)])

    if stop == 4:
        return done()
    P.barrier()
    AR.reset()
    KTS = sb("kts", [128, EXT], BF16)
    VP = sb("vp", [128, NT, 129], BF16)
    QS = sb("qs", [128, OWN], BF16)
    GSH = sb("gsh", [128, NTO, 128], BF16)
    G = sb("gate", [128, 64], F32)
    T8 = sb("t8", [128, 8], F32)
    MSEL = sb("msel", [128, 64], F32)
    DEX = sb("dex", [128, 64], F32)
    DM = sb("dm", [128, 64], F32)
    PTS = [sb("pts%d" % i, [128, 512], BF16) for i in range(3)]
    ACC = sb("acc", [128, 129], F32)
    RDEN = sb("rden", [128, 1], F32)
    OB = [sb("ob%d" % i, [128, 128], BF16) for i in range(2)]
    OST = [sb("ost%d" % i, [128, 512], BF16) for i in range(2)]
    SBANKS = [banks[0], banks[1]]
    OBANKS = [banks[3], banks[4], banks[5]]
    GBANK = banks[2]
    sct = [0, 0, 0, 0]
    for h in range(8):
        slope = float(2.0 ** (-(h + 1)))
        P.dma("sp", KTS[:], KT[h, :, :], reads=[KT.k], writes=[KTS.k])
        vsrc = VV[:, h * 128:(h + 1) * 128].rearrange("(t p) d -> p t d", p=128)
        for t0_ in range(0, NT, 16):
            t1_ = min(NT, t0_ + 16)
            P.dma("sp", VP[:, t0_:t1_, 0:128], vsrc[:, t0_:t1_, :], reads=[VV.k],
                  **({"writes": [VP.k]} if t0_ == 0 else {"wadd": [VP.k]}))
        P.op("pool", lambda e: e.memset(VP[:, :, 128:129], 1.0), wadd=[VP.k])
        vpv = VP[:].rearrange("p (a two) d -> p a two d", two=2)
        for par in range(2):
            P.op("dve", lambda e, par=par, h=h: e.tensor_scalar(
                out=vpv[:, :, par, :], in0=vpv[:, :, par, :], scalar1=SC[:, 2 * h + par:2 * h + par + 1],
                scalar2=None, op0=ALU.mult), reads=[VP.k, SC.k], writes=[VP.k])
        P.dma("sp", QS[:], MQT[h, :, :], reads=[MQT.k], writes=[QS.k])
        gsrc = GS[:, 1024 + h * 128:1024 + (h + 1) * 128].rearrange("(t p) d -> p t d", p=128)
        for t0_ in range(0, NTO, 16):
            t1_ = min(NTO, t0_ + 16)
            P.dma("sp", GSH[:, t0_:t1_, :], gsrc[:, t0_:t1_, :], reads=[GS.k],
                  **({"writes": [GSH.k]} if t0_ == 0 else {"wadd": [GSH.k]}))
        for qt in range(NTO):
            eq = PRE // 128 + qt
            ob_ = eq // 2
            nblk = ob_ + 1
            qsl = QS[:, qt * 128:(qt + 1) * 128]
            P.op("pool", lambda e: e.memset(G[:], NEG), writes=[G.k])
            P.op("pool", lambda e: e.memset(MSEL[:], 1.0), writes=[MSEL.k])
            if ob_ > 0:
                P.op("pe", lambda e, h=h, ob_=ob_, qsl=qsl: e.matmul(GBANK.ap[:, 0:ob_], qsl, KMB[:, h, 0:ob_],
                                                                   start=True, stop=True),
                     reads=[QS.k, KMB.k], writes=[GBANK.k])
                P.op("dve", lambda e, ob_=ob_: e.tensor_tensor(out=G[:, 0:ob_], in0=GBANK.ap[:, 0:ob_],
                                                              in1=BVAL[:, 0:ob_], op=ALU.add),
                     reads=[GBANK.k, BVAL.k], wadd=[G.k])
                P.op("dve", lambda e: e.max(out=T8[:], in_=G[:]), reads=[G.k], writes=[T8.k])
                P.op("dve", lambda e: e.tensor_scalar_max(out=T8[:, 2:3], in0=T8[:, 2:3], scalar1=-1.0e29),
                     reads=[T8.k], writes=[T8.k])
                P.op("dve", lambda e, ob_=ob_: e.tensor_scalar(out=MSEL[:, 0:ob_], in0=G[:, 0:ob_],
                                                              scalar1=T8[:, 2:3], scalar2=None, op0=ALU.is_ge),
                     reads=[G.k, T8.k], wadd=[MSEL.k])
            P.op("act", lambda e, nblk=nblk, h=h, qt=qt, slope=slope: e.activation(
                out=DEX[:, 0:nblk], in_=JROW[:, 0:nblk], func=AF.Exp, scale=slope,
                bias=NEGT[:, h * NTO + qt:h * NTO + qt + 1]), reads=[JROW.k, NEGT.k], writes=[DEX.k])
            P.op("dve", lambda e, nblk=nblk: e.tensor_tensor(out=DM[:, 0:nblk], in0=DEX[:, 0:nblk],
                                                            in1=MSEL[:, 0:nblk], op=ALU.mult),
                 reads=[DEX.k, MSEL.k], writes=[DM.k])
            last_kt = 2 * ob_ + (1 if eq % 2 == 1 else 0)
            kts = list(range(last_kt + 1))
            first_acc = True
            for g0 in range(0, len(kts), 4):
                grp = kts[g0:g0 + 4]
                sbk = SBANKS[sct[0] % 2]
                sct[0] += 1
                for i_, kt in enumerate(grp):
                    P.op("pe", lambda e, sbk=sbk, i_=i_, kt=kt, qsl=qsl: e.matmul(
                        sbk.ap[:, i_ * 128:(i_ + 1) * 128], KTS[:, kt * 128:(kt + 1) * 128], qsl, start=True, stop=True),
                        reads=[KTS.k, QS.k], **({"writes": [sbk.k]} if i_ == 0 else {"wadd": [sbk.k]}))
                pts = PTS[sct[1] % 3]
                sct[1] += 1
                n = len(grp)
                P.op("act", lambda e, pts=pts, sbk=sbk, n=n: e.activation(
                    out=pts[:, 0:n * 128], in_=sbk.ap[:, 0:n * 128], func=AF.Exp, scale=float(128 ** -0.5)),
                    reads=[sbk.k], writes=[pts.k])
                if last_kt in grp:
                    i_ = grp.index(last_kt)
                    P.op("pool", lambda e, pts=pts, i_=i_: e.tensor_tensor(
                        out=pts[:, i_ * 128:(i_ + 1) * 128], in0=pts[:, i_ * 128:(i_ + 1) * 128], in1=TRIB[:],
                        op=ALU.mult), reads=[pts.k, TRIB.k], writes=[pts.k])
                blks = sorted(set(kt // 2 for kt in grp))
                for j in blks:
                    obk = OBANKS[sct[2] % 3]
                    sct[2] += 1
                    jk = [kt for kt in grp if kt // 2 == j]
                    for ii, kt in enumerate(jk):
                        i_ = grp.index(kt)
                        P.op("pe", lambda e, obk=obk, pts=pts, i_=i_, kt=kt, ii=ii, jk=jk: e.matmul(
                            obk.ap[:, 0:129], pts[:, i_ * 128:(i_ + 1) * 128], VP[:, kt, :],
                            start=(ii == 0), stop=(ii == len(jk) - 1)),
                            reads=[pts.k, VP.k], **({"writes": [obk.k]} if ii == 0 else {"wadd": [obk.k]}))
                    if first_acc:
                        P.op("dve", lambda e, obk=obk, j=j: e.tensor_scalar(
                            out=ACC[:], in0=obk.ap[:, 0:129], scalar1=DM[:, j:j + 1], scalar2=None, op0=ALU.mult),
                            reads=[obk.k, DM.k], writes=[ACC.k])
                        first_acc = False
                    else:
                        P.op("dve", lambda e, obk=obk, j=j: e.scalar_tensor_tensor(
                            out=ACC[:], in0=obk.ap[:, 0:129], scalar=DM[:, j:j + 1], in1=ACC[:],
                            op0=ALU.mult, op1=ALU.add), reads=[obk.k, DM.k, ACC.k], writes=[ACC.k])
            ob16 = OB[qt % 2]
            P.op("dve", lambda e: e.reciprocal(out=RDEN[:], in_=ACC[:, 128:129]), reads=[ACC.k], writes=[RDEN.k])
            P.op("dve", lambda e, ob16=ob16, qt=qt: e.scalar_tensor_tensor(
                out=ob16[:], in0=ACC[:, 0:128], scalar=RDEN[:, 0:1], in1=GSH[:, qt, :], op0=ALU.mult, op1=ALU.mult),
                reads=[ACC.k, RDEN.k, GSH.k], writes=[ob16.k])
            half = (qt // 4) % 2
            j4 = qt % 4
            P.op("pe", lambda e, ob16=ob16, half=half, j4=j4: e.transpose(
                out=PTB[half][:, j4 * 128:(j4 + 1) * 128], in_=ob16[:], identity=IDB[:]),
                reads=[ob16.k, IDB.k], **({"writes": [PTk[half]]} if j4 == 0 else {"wadd": [PTk[half]]}))
            if j4 == 3:
                ost = OST[(qt // 4) % 2]
                evac_copy(ost[:], PTB[half][:, 0:512], [PTk[half]], writes=[ost.k])
                P.dma("pool", OT[8 + h, :, (qt - 3) * 128:(qt + 1) * 128], ost[:], reads=[ost.k], wadd=[OT.k], st=ost.k)

    if stop == 5:
        return done()
    P.barrier()
    AR.reset()
    JUNK2 = sb("junk2", [128, 256], BF16)
    GKVT = [sb("gkvt%d" % i, [128, 1536], BF16) for i in range(2)]
    LRA = [sb("lra%d" % i, [17, 128], F32) for i in range(2)]
    KQT = [sb("kqt%d" % i, [128, 8, 128], BF16) for i in range(2)]
    GSL = [sb("gsl%d" % i, [128, 1024], BF16) for i in range(2)]
    E1 = sb("e1", [128, 512], F32)
    SP_ = sb("sp", [128, 512], F32)
    EKTM = sb("ektm", [128, 512], F32)
    KTT = sb("ktt", [128, 512], BF16)
    EQT = sb("eqt", [128, 512], F32)
    EKT = sb("ekt", [128, 512], F32)
    KTF = sb("ktf", [128, 4, 128], BF16)
    QZ = sb("qz", [128, 4, 192], BF16)
    S = sb("S", [128, 1024], F32)
    TS_ = sb("Ts", [128, 1024], F32)
    SBF = [sb("sbf%d" % i, [128, 1024], BF16) for i in range(2)]
    ATB = sb("atb", [128, 512], BF16)
    SSG = sb("ssg", [128, 4], F32)
    GG = sb("gg", [128, 1024], F32)
    OG = sb("og", [128, 1024], BF16)
    OGT = [sb("ogt%d" % i, [128, 8, 128], BF16) for i in range(2)]
    P.op("pool", lambda e: e.memset(S[:], 0.0), writes=[S.k])
    P.op("pool", lambda e: e.memset(SBF[0][:], 0.0), writes=[SBF[0].k])
    P.op("pool", lambda e: e.memset(QZ[:], 0.0), writes=[QZ.k])
    for i in range(2):
        P.op("pool", lambda e, i=i: e.memset(LRA[i][:], 1.0), writes=[LRA[i].k])
    for ti in range(NT):
        own = ti >= PRE // 128
        to = ti - PRE // 128
        gkv, lra = GKVT[ti % 2], LRA[ti % 2]
        P.dma("sp", gkv[:], GKV[ti * 128:(ti + 1) * 128, :], reads=[GKV.k], writes=[gkv.k])
        P.dma("sp", lra[0:16, :], LRT[:, ti * 128:(ti + 1) * 128], reads=[LRT.k], wadd=[lra.k])
        if own:
            kqt, gsl = KQT[ti % 2], GSL[ti % 2]
            P.dma("sp", kqt[:, 0:4, :], GKT[:, :, ti * 128:(ti + 1) * 128].rearrange("c p t -> p c t"),
                  reads=[GKT.k], writes=[kqt.k])
            P.dma("sp", kqt[:, 4:8, :], GQT[:, :, to * 128:(to + 1) * 128].rearrange("c p t -> p c t"),
                  reads=[GQT.k], wadd=[kqt.k])
            P.dma("sp", gsl[:], GS[to * 128:(to + 1) * 128, 0:1024], reads=[GS.k], writes=[gsl.k])
        zb = banks[0]
        P.op("pe", lambda e, lra=lra: e.matmul(zb.ap, lra[:], WGA[:], start=True, stop=True),
             reads=[lra.k, WGA.k], writes=[zb.k])
        P.op("act", lambda e: e.activation(out=E1[:], in_=zb.ap, func=AF.Exp, scale=-1.0), reads=[zb.k], writes=[E1.k])
        P.op("act", lambda e: e.activation(out=SP_[:], in_=E1[:], func=AF.Ln, bias=1.0), reads=[E1.k], writes=[SP_.k])
        cb = banks[1]
        P.op("pe", lambda e: e.matmul(cb.ap, U2[:], SP_[:], start=True, stop=True), reads=[U2.k, SP_.k], writes=[cb.k])
        tb = banks[0]
        for hh in range(4):
            P.op("pe", lambda e, hh=hh: e.matmul(tb.ap[:, hh * 128:(hh + 1) * 128], SP_[:, hh * 128:(hh + 1) * 128], U2[:],
                                                 start=True, stop=True), reads=[SP_.k, U2.k],
                 **({"writes": [tb.k]} if hh == 0 else {"wadd": [tb.k]}))
        P.op("act", lambda e: e.activation(out=EKTM[:], in_=cb.ap, func=AF.Exp), reads=[cb.k], writes=[EKTM.k])
        P.op("dve", lambda e, gkv=gkv: e.tensor_tensor(out=KTT[:], in0=gkv[:, 0:512], in1=EKTM[:], op=ALU.mult),
             reads=[gkv.k, EKTM.k], writes=[KTT.k])
        P.op("act", lambda e: e.activation(out=EQT[:], in_=tb.ap, func=AF.Exp, scale=-1.0), reads=[tb.k], writes=[EQT.k])
        if own:
            P.op("act", lambda e: e.activation(out=EKT[:], in_=tb.ap, func=AF.Exp), reads=[tb.k], writes=[EKT.k])
            P.op("dve", lambda e, kqt=kqt: e.tensor_tensor(
                out=KTF[:], in0=kqt[:, 0:4, :], in1=EKT[:].rearrange("p (a b) -> p a b", b=128), op=ALU.mult),
                reads=[kqt.k, EKT.k], writes=[KTF.k])
            for c in range(2):
                P.op("dve", lambda e, kqt=kqt, c=c: e.scalar_tensor_tensor(
                    out=QZ[:, :, c * 128:c * 128 + 64], in0=kqt[:, 4:8, c * 64:(c + 1) * 64], scalar=float(128 ** -0.5),
                    in1=EQT[:].rearrange("p (a b) -> p a b", b=128)[:, :, c * 64:(c + 1) * 64],
                    op0=ALU.mult, op1=ALU.mult), reads=[kqt.k, EQT.k], wadd=[QZ.k])
            ab = banks[1]
            for hh in range(4):
                P.op("pe", lambda e, hh=hh: e.matmul(
                    ab.ap[:, hh * 128:(hh + 1) * 128].rearrange("p (a b) -> p a b", b=64), KTF[:, hh, :],
                    QZ[:, hh, :].rearrange("p (a b) -> p a b", b=64)[:, 0:3:2, :], start=True, stop=True),
                    reads=[KTF.k, QZ.k], **({"writes": [ab.k]} if hh == 0 else {"wadd": [ab.k]}))
            P.op("dve", lambda e: e.tensor_tensor(
                out=ATB[:].rearrange("p (a b) -> p a b", b=128), in0=ab.ap.rearrange("p (a b) -> p a b", b=128),
                in1=MSK[:].unsqueeze(1).to_broadcast([128, 4, 128]), op=ALU.mult), reads=[ab.k, MSK.k], writes=[ATB.k])
        def state_update(c, gkv=gkv):
            sout = SBF[(c + 1) % 2]
            for hh in range(4):
                P.op("pe", lambda e, hh=hh, c=c, gkv=gkv: e.matmul(
                    BIG1[:, hh * 256:(hh + 1) * 256], KTT[c * 64:(c + 1) * 64, hh * 128:(hh + 1) * 128],
                    gkv[c * 64:(c + 1) * 64, 512 + hh * 256:512 + (hh + 1) * 256], start=True, stop=True),
                    reads=[KTT.k, gkv.k], **({"writes": [b1a, b1b]} if hh == 0 else {"wadd": [b1a, b1b]}))
            P.op("dve", lambda e: e.tensor_tensor(out=TS_[:], in0=BIG1[:, :], in1=S[:], op=ALU.add),
                 reads=[b1a, b1b, S.k], writes=[TS_.k])
            for hh in range(4):
                col = hh * 128 + c * 64 + 63
                P.op("act", lambda e, hh=hh, col=col: e.activation(
                    out=S[:, hh * 256:(hh + 1) * 256], in_=TS_[:, hh * 256:(hh + 1) * 256], func=AF.Copy,
                    scale=EQT[:, col:col + 1]), reads=[TS_.k, EQT.k], **({"writes": [S.k]} if hh == 0 else {"wadd": [S.k]}))
            P.op("pool", lambda e, sout=sout: e.tensor_copy(out=sout[:], in_=S[:]), reads=[S.k], writes=[sout.k])
        state_update(0)
        if own:
            for hh in range(4):
                P.op("pe", lambda e, hh=hh, gkv=gkv: e.matmul(
                    BIG0[:, hh * 256:(hh + 1) * 256], ATB[:, hh * 128:(hh + 1) * 128],
                    gkv[:, 512 + hh * 256:512 + (hh + 1) * 256], start=True, stop=False),
                    reads=[ATB.k, gkv.k], **({"writes": [b0a, b0b]} if hh == 0 else {"wadd": [b0a, b0b]}))
                P.op("pe", lambda e, hh=hh: e.matmul(
                    BIG0[:, hh * 256:(hh + 1) * 256], QZ[:, hh, 0:128], SBF[0][:, hh * 256:(hh + 1) * 256],
                    start=False, stop=False), reads=[QZ.k, SBF[0].k], wadd=[b0a, b0b])
                P.op("pe", lambda e, hh=hh: e.matmul(
                    BIG0[:, hh * 256:(hh + 1) * 256], QZ[:, hh, 64:192], SBF[1][:, hh * 256:(hh + 1) * 256],
                    start=False, stop=True), reads=[QZ.k, SBF[1].k], wadd=[b0a, b0b])
        state_update(1)
        if _dbg and own and to == 0:
            def ddump(name, buf_ap, shape, dt, reads):
                dd = nc.dram_tensor(name, shape, dt, kind="ExternalOutput").ap()
                tk = Tok(name)
                P.dma("sp", dd, buf_ap, reads=reads, st=tk)
            ddump("d_sp", SP_[:], [128, 512], F32, [SP_.k])
            ddump("d_ektm", EKTM[:], [128, 512], F32, [EKTM.k])
            ddump("d_eqt", EQT[:], [128, 512], F32, [EQT.k])
            ddump("d_ktt", KTT[:], [128, 512], BF16, [KTT.k])
            ddump("d_atb", ATB[:], [128, 512], BF16, [ATB.k])
            ddump("d_qz", QZ[:], [128, 4, 192], BF16, [QZ.k])
            ddump("d_ktf", KTF[:], [128, 4, 128], BF16, [KTF.k])
            ddump("d_S", S[:], [128, 1024], F32, [S.k])
            P.op("dve", lambda e: e.tensor_copy(out=GG[:], in_=BIG0[:, :]), reads=[b0a, b0b], writes=[GG.k])
            ddump("d_o", GG[:], [128, 1024], F32, [GG.k])
        if own:
            for hh in range(4):
                P.op("act", lambda e, hh=hh: e.activation(out=JUNK2[:, 0:256], in_=BIG0[:, hh * 256:(hh + 1) * 256],
                                                          func=AF.Square, accum_out=SSG[:, hh:hh + 1]),
                     reads=[b0a, b0b], writes=[JUNK2.k], wadd=[SSG.k])
            P.op("dve", lambda e: e.tensor_scalar(out=SSG[:], in0=SSG[:], scalar1=1.0 / 256, scalar2=EPS,
                                                  op0=ALU.mult, op1=ALU.add), reads=[SSG.k], writes=[SSG.k])
            P.op("act", lambda e: e.activation(out=SSG[:], in_=SSG[:], func=AF.Sqrt), reads=[SSG.k], writes=[SSG.k])
            P.op("dve", lambda e: e.reciprocal(out=SSG[:], in_=SSG[:]), reads=[SSG.k], writes=[SSG.k])
            P.op("pool", lambda e, gsl=gsl: e.tensor_tensor(out=GG[:], in0=gsl[:], in1=GOUT[:], op=ALU.mult),
                 reads=[gsl.k, GOUT.k], writes=[GG.k])
            for hh in range(4):
                P.op("dve", lambda e, hh=hh: e.scalar_tensor_tensor(
                    out=OG[:, hh * 256:(hh + 1) * 256], in0=BIG0[:, hh * 256:(hh + 1) * 256], scalar=SSG[:, hh:hh + 1],
                    in1=GG[:, hh * 256:(hh + 1) * 256], op0=ALU.mult, op1=ALU.mult),
                    reads=[b0a, b0b, SSG.k, GG.k], **({"writes": [OG.k]} if hh == 0 else {"wadd": [OG.k]}))
            ogt = OGT[ti % 2]
            for half in range(2):
                for j in range(4):
                    c8 = half * 4 + j
                    P.op("pe", lambda e, c8=c8, half=half, j=j: e.transpose(
                        out=PTB[half][:, j * 128:(j + 1) * 128], in_=OG[:, c8 * 128:(c8 + 1) * 128],
                        identity=IDB[:]), reads=[OG.k, IDB.k],
                        **({"writes": [PTk[half]]} if j == 0 else {"wadd": [PTk[half]]}))
                evac_copy(ogt[:, half * 4:(half + 1) * 4, :],
                          PTB[half][:, 0:512].rearrange("p (a b) -> p a b", b=128), [PTk[half]],
                          **({"writes": [ogt.k]} if half == 0 else {"wadd": [ogt.k]}))
            P.dma("pool", OT[0:8, :, to * 128:(to + 1) * 128].rearrange("c p t -> p c t"), ogt[:], reads=[ogt.k],
                  wadd=[OT.k], st=ogt.k)

    if stop == 6:
        return done()
    alloc_gemm()
    WZ = R["WBIG"]
    WSTG = R["WSTG"]
    load_w(w_bg, [(0, 2048)], nch=8, scale=False)
    first = True
    for c in range(8):
        st = WSTG[wstg_n[0] % 2]
        wstg_n[0] += 1
        P.dma("sp", st[:, 0:2048], w_bm[c * 128:(c + 1) * 128, :], writes=[st.k])
        P.op("pool", lambda e, st=st, c=c: e.tensor_copy(out=WZ[:, 8 + c, 0:2048], in_=st[:, 0:2048]),
             reads=[st.k], wadd=[WZ.k])
    OTG = R["HTG"]
    SGG = [sb("sgg%d" % i, [128, 32, 512], BF16) for i in range(1)]
    T1 = [sb("t1_%d" % i, [128, 512], BF16) for i in range(2)]
    T2 = [sb("t2_%d" % i, [128, 512], BF16) for i in range(2)]
    MTS = [sb("mts%d" % i, [128, 512], BF16) for i in range(2)]
    for g in range(OWN // 512):
        otg = OTG[g % 2]
        sgg = SGG[0]
        P.dma("sp", otg[:], OT[:, :, g * 512:(g + 1) * 512].rearrange("c p t -> p c t"), reads=[OT.k], writes=[otg.k])
        for c0_ in (0, 16):
            P.dma("sp", sgg[:, c0_:c0_ + 16, :], SGT[c0_:c0_ + 16, :, g * 512:(g + 1) * 512].rearrange("c p t -> p c t"),
                  reads=[SGT.k], **({"writes": [sgg.k]} if c0_ == 0 else {"wadd": [sgg.k]}))
        for n in range(16):
            bg, bm = next_bank(), next_bank()
            for br, bnk in ((0, bg), (1, bm)):
                for c in range(8):
                    P.op("pe", lambda e, bnk=bnk, br=br, c=c, n=n, otg=otg: e.matmul(
                        bnk.ap, WZ[:, br * 8 + c, n * 128:(n + 1) * 128], otg[:, br * 8 + c, :],
                        start=(c == 0), stop=(c == 7)), reads=[WZ.k, otg.k],
                        **({"writes": [bnk.k]} if c == 0 else {"wadd": [bnk.k]}))
            t1, t2, mts = T1[n % 2], T2[n % 2], MTS[n % 2]
            P.op("dve", lambda e, t1=t1, bg=bg, n=n, sgg=sgg: e.tensor_tensor(out=t1[:], in0=bg.ap, in1=sgg[:, n, :], op=ALU.mult),
                 reads=[bg.k, sgg.k], writes=[t1.k])
            P.op("dve", lambda e, t2=t2, bm=bm, n=n, sgg=sgg: e.tensor_tensor(out=t2[:], in0=bm.ap, in1=sgg[:, 16 + n, :], op=ALU.mult),
                 reads=[bm.k, sgg.k], writes=[t2.k])
            P.op("pool", lambda e, t1=t1, t2=t2, mts=mts: e.tensor_tensor(out=mts[:], in0=t1[:], in1=t2[:], op=ALU.add),
                 reads=[t1.k, t2.k], writes=[mts.k])
            P.dma("pool", MT[n, :, g * 512:(g + 1) * 512], mts[:], reads=[mts.k], wadd=[MT.k], st=mts.k)

    if stop == 7:
        return done()
    alloc_gemm()
    WBIG = R["WBIG"]
    load_w(w_o, [(0, 2048)], scale=False)
    XT = [sb("xtz%d" % i, [128, D], F32) for i in range(2)]
    YT = [sb("yt%d" % i, [128, D], F32) for i in range(2)]
    for g in range(OWN // 512):
        mtg = R["HTG"][g % 2]
        P.dma("sp", mtg[:], MT[:, :, g * 512:(g + 1) * 512].rearrange("c p t -> p c t"), reads=[MT.k], writes=[mtg.k])
        for t4 in range(4):
            ti = g * 4 + t4
            xt, yt = XT[ti % 2], YT[ti % 2]
            P.dma("sp", xt[:], x[PRE + ti * 128:PRE + (ti + 1) * 128, :], writes=[xt.k])
            for ng in range(4):
                b = next_bank()
                for c in range(NCH):
                    P.op("pe", lambda e, b=b, c=c, mtg=mtg, t4=t4, ng=ng: e.matmul(
                        b.ap, mtg[:, c, t4 * 128:(t4 + 1) * 128], WBIG[:, c, ng * 512:(ng + 1) * 512],
                        start=(c == 0), stop=(c == NCH - 1)), reads=[mtg.k, WBIG.k],
                        **({"writes": [b.k]} if c == 0 else {"wadd": [b.k]}))
                P.op("dve", lambda e, b=b, yt=yt, xt=xt, ng=ng: e.tensor_tensor(
                    out=yt[:, ng * 512:(ng + 1) * 512], in0=b.ap, in1=xt[:, ng * 512:(ng + 1) * 512], op=ALU.add),
                    reads=[b.k, xt.k], **({"writes": [yt.k]} if ng == 0 else {"wadd": [yt.k]}))
            P.dma("pool", y[ti * 128:(ti + 1) * 128, :], yt[:], reads=[yt.k], st=yt.k)

    P.finish()
    P.emit()
    return nc


def make_consts(EXT, OWN):
    NTO = OWN // 128
    PRE = EXT - OWN
    p = np.arange(128)
    same = (p[:, None] // 64) == (p[None, :] // 64)
    le = p[:, None] <= p[None, :]
    msk = (same & le).astype(np.float32)
    slopes = 2.0 ** (-8.0 * np.arange(1, 9, dtype=np.float64) / 8)
    sc = np.zeros((128, 16), np.float32)
    negt = np.zeros((128, 8 * NTO), np.float32)
    for h in range(8):
        sc[:, 2 * h] = np.exp(slopes[h] * (p - 128.0))
        sc[:, 2 * h + 1] = np.exp(slopes[h] * (p * 1.0))
        for qt in range(NTO):
            negt[:, h * NTO + qt] = -slopes[h] * (PRE + qt * 128 + p)
    return {
        "c_id": np.eye(128, dtype=np.float32),
        "c_u2": (msk / 16.0).astype(np.float32),
        "c_msk": msk,
        "c_tri": le.astype(np.float32),
        "c_jrow": np.broadcast_to((256.0 * np.arange(64) + 128.0).astype(np.float32), (128, 64)).copy(),
        "c_sc": sc,
        "c_negt": negt,
    }


def make_in_maps(inputs, EXT, OWN, nseg, ncores=8):
    x = np.asarray(inputs["x"], np.float32)
    B = x.shape[0]
    consts = make_consts(EXT, OWN)
    shared = dict(consts)
    shared["w_in"] = np.ascontiguousarray(np.asarray(inputs["w_in"], np.float32)[0])
    shared["w_bg"] = np.ascontiguousarray(np.asarray(inputs["w_branch_gla"], np.float32)[0])
    shared["w_bm"] = np.ascontiguousarray(np.asarray(inputs["w_branch_moba"], np.float32)[0])
    shared["w_o"] = np.ascontiguousarray(np.asarray(inputs["w_out"], np.float32)[0])
    shared["c_ng"] = np.ascontiguousarray(np.asarray(inputs["norm_g"], np.float32)[0].reshape(NCH, 128).T)
    shared["c_wga"] = np.concatenate([np.asarray(inputs["w_gla_gate"], np.float32)[0],
                                      np.asarray(inputs["b_gla_gate"], np.float32)[0][None, :]], axis=0)
    shared["c_gout"] = np.ascontiguousarray(np.broadcast_to(
        np.tile(np.asarray(inputs["gla_out_g"], np.float32)[0], 4)[None, :], (128, 1024)))
    shared["c_qg"] = np.ascontiguousarray(np.stack([np.asarray(inputs["q_norm_g"], np.float32)[0],
                                                    np.asarray(inputs["k_norm_g"], np.float32)[0]], axis=1))
    maps = []
    for c in range(ncores):
        b, i = (c // nseg) % B, c % nseg
        end = (i + 1) * OWN
        pad = EXT - end
        xe = np.zeros((EXT, D), np.float32)
        xe[pad:] = x[b, :end]
        bval = np.zeros((128, 64), np.float32)
        bval[:, :pad // 256] = NEG
        m = dict(shared)
        m["x"] = xe
        m["c_bval"] = bval
        maps.append(m)
    return maps


_NC_CACHE = {}


def kernel(**inputs):
    EXT, OWN, nseg = 16384, 4096, 4
    x = np.asarray(inputs["x"])
    B, S, _ = x.shape
    key = (EXT, OWN)
    if key not in _NC_CACHE:
        _NC_CACHE[key] = build(EXT, OWN)
    nc = _NC_CACHE[key]
    maps = make_in_maps(inputs, EXT, OWN, nseg)
    res = run_bass_kernel_spmd(nc, maps, core_ids=list(range(8)))
    out = np.empty((B, S, D), np.float32)
    for c in range(8):
        b, i = c // nseg, c % nseg
        out[b, i * OWN:(i + 1) * OWN] = res.results[c]["y"]
    return out
```

```python
import numpy as np
import ml_dtypes
import concourse.bass as bass
import concourse.mybir as mybir
from concourse.bass_utils import run_bass_kernel_spmd

F32 = mybir.dt.float32
BF16 = mybir.dt.bfloat16
AF = mybir.ActivationFunctionType
ALU = mybir.AluOpType
AX = mybir.AxisListType

D = 2048
NCH = 16
PROJ = 11280
C_GQ, C_GK, C_GV, C_LR, C_GS, C_MQ, C_MK, C_MV, C_MS, C_GA, C_GB = (
    0, 512, 1024, 2048, 2064, 3088, 4112, 5136, 6160, 7184, 9232)
EPS = 1e-6
NEG = -1.0e30


class Tok:
    __slots__ = ("name", "w", "r", "dsem")

    def __init__(self, name):
        self.name = name
        self.w = {}
        self.r = {}
        self.dsem = None


class Prog:
    ENG = ("pe", "act", "dve", "pool", "sp")

    def __init__(self, nc):
        self.nc = nc
        self.q = {e: [] for e in self.ENG}
        self.cnt = {e: 0 for e in self.ENG}
        self.esem = {e: nc.alloc_semaphore("es_" + e) for e in self.ENG}
        self.seen = {e: {} for e in self.ENG}
        self.dsems = []
        self.retired = []
        self.nsem = 0

    def _deps(self, reads, writes):
        deps = {}
        for t in reads:
            for s, v in t.w.items():
                if deps.get(s, 0) < v:
                    deps[s] = v
        for t in writes:
            for s, v in t.w.items():
                if deps.get(s, 0) < v:
                    deps[s] = v
            for s, v in t.r.items():
                if deps.get(s, 0) < v:
                    deps[s] = v
        return deps

    def _waits(self, e, deps, skip_own=False):
        waits = []
        seen = self.seen[e]
        own = self.esem[e]
        for s, v in deps.items():
            if skip_own and s is own:
                continue
            if seen.get(s, 0) < v:
                seen[s] = v
                waits.append((s, v))
        return waits

    def op(self, e, fn, reads=(), writes=(), wadd=()):
        allw = tuple(writes) + tuple(wadd)
        deps = self._deps(reads, allw)
        waits = self._waits(e, deps, skip_own=(e == "pe"))
        if self.cnt[e] >= 30000:
            self.retired.append((self.esem[e], self.cnt[e]))
            self.nsem += 1
            self.esem[e] = self.nc.alloc_semaphore("es_%s_%d" % (e, self.nsem))
            self.cnt[e] = 0
        self.cnt[e] += 1
        c = self.cnt[e]
        sem = self.esem[e]

        def run(eng, waits=waits, fn=fn, sem=sem):
            for s, v in waits:
                eng.wait_ge(s, v)
            fn(eng).then_inc(sem, 1)
        self.q[e].append(run)
        for t in writes:
            t.w = {sem: c}
            t.r = {}
        for t in wadd:
            t.w[sem] = c
        for t in reads:
            t.r[sem] = c

    def dma(self, qe, out, in_, reads=(), writes=(), wadd=(), st=None):
        allw = tuple(writes) + tuple(wadd)
        if st is None:
            st = (allw + tuple(reads))[0]
        if st.dsem is None:
            st.dsem = [self.nc.alloc_semaphore("d_" + st.name), 0]
            self.dsems.append(st)
        sem, tot = st.dsem
        deps = self._deps(reads, allw)
        if tot > 0:
            deps[sem] = max(deps.get(sem, 0), tot)
        waits = self._waits(qe, deps)
        v = tot + 16
        st.dsem[1] = v

        def run(eng, waits=waits, sem=sem, out=out, in_=in_):
            for s, vv in waits:
                eng.wait_ge(s, vv)
            eng.dma_start(out=out, in_=in_).then_inc(sem, 16)
        self.q[qe].append(run)
        for t in writes:
            t.w = {sem: v}
            t.r = {}
        for t in wadd:
            t.w[sem] = v
        for t in reads:
            t.r[sem] = max(t.r.get(sem, 0), v)

    def barrier(self):
        for e in self.ENG:
            deps = {}
            for st in self.dsems:
                sem, tot = st.dsem
                deps[sem] = tot
            for f in self.ENG:
                if f != e and self.cnt[f] > 0:
                    deps[self.esem[f]] = self.cnt[f]
            for rs, rv in self.retired:
                deps[rs] = rv
            waits = self._waits(e, deps)

            def run(eng, waits=waits):
                for s, v in waits:
                    eng.wait_ge(s, v)
            self.q[e].append(run)

    def finish(self):
        deps = {}
        for st in self.dsems:
            sem, tot = st.dsem
            deps[sem] = tot
        for e in self.ENG:
            if e != "sp" and self.cnt[e] > 0:
                deps[self.esem[e]] = self.cnt[e]
        for rs, rv in self.retired:
            deps[rs] = rv
        waits = self._waits("sp", deps)

        def run(eng, waits=waits):
            for s, v in waits:
                eng.wait_ge(s, v)
        self.q["sp"].append(run)

    def emit(self):
        nc = self.nc
        q = self.q
        with nc.Block() as block:
            @block.tensor
            def _(eng):
                for f in q["pe"]:
                    f(eng)

            @block.scalar
            def _(eng):
                for f in q["act"]:
                    f(eng)

            @block.vector
            def _(eng):
                for f in q["dve"]:
                    f(eng)

            @block.gpsimd
            def _(eng):
                for f in q["pool"]:
                    f(eng)

            @block.sync
            def _(eng):
                for f in q["sp"]:
                    f(eng)


class Buf:
    def __init__(self, t, name):
        self.t = t
        self.k = Tok(name)

    def __getitem__(self, key):
        return self.t[key]


class Arena:
    def __init__(self, nc, words):
        self.t = nc.alloc_sbuf_tensor("arena", [128, words], F32)
        self.words = words
        self.off = 0

    def reset(self):
        self.off = 0

    def alloc(self, name, shape, dt):
        nb = 2 if dt == BF16 else 4
        free = int(np.prod(shape[1:]))
        w = (free * nb + 3) // 4
        w = (w + 7) // 8 * 8
        assert self.off + w <= self.words, (name, self.off, w, self.words)
        v = self.t[0:shape[0], self.off:self.off + w]
        self.off += w
        if dt == BF16:
            v = v.bitcast(BF16)
        v = v[:, 0:free]
        if len(shape) == 3:
            v = v.rearrange("p (a b) -> p a b", b=shape[2])
        return Buf(v, name)


def build(EXT, OWN, stop=99):
    nc = bass.Bass("TRN2", target_bir_lowering=False)
    P = Prog(nc)

    def done():
        P.finish()
        P.emit()
        return nc
    NT = EXT // 128
    NTO = OWN // 128
    NB = EXT // 256
    PRE = EXT - OWN
    assert OWN % 512 == 0 and EXT % 512 == 0 and NB <= 64

    def din(name, shape, dt=F32):
        return nc.dram_tensor(name, shape, dt, kind="ExternalInput").ap()

    x = din("x", [EXT, D])
    w_in = din("w_in", [D, PROJ])
    w_bg = din("w_bg", [1024, D])
    w_bm = din("w_bm", [1024, D])
    w_o = din("w_o", [D, D])
    c_ng = din("c_ng", [128, NCH])
    c_wga = din("c_wga", [17, 512])
    c_gout = din("c_gout", [128, 1024])
    c_qg = din("c_qg", [128, 2])
    c_bval = din("c_bval", [128, 64])
    c_id = din("c_id", [128, 128])
    c_u2 = din("c_u2", [128, 128])
    c_msk = din("c_msk", [128, 128])
    c_tri = din("c_tri", [128, 128])
    c_jrow = din("c_jrow", [128, 64])
    c_sc = din("c_sc", [128, 16])
    c_negt = din("c_negt", [128, 8 * NTO])
    y = nc.dram_tensor("y", [OWN, D], F32, kind="ExternalOutput").ap()

    import os as _os0
    _dbg = _os0.environ.get("KDEBUG", "0") == "1"

    def dscr(name, shape, dt=BF16):
        if _dbg:
            return Buf(nc.dram_tensor(name, shape, dt, kind="ExternalOutput").ap(), name)
        return Buf(nc.dram_tensor(name, shape, dt).ap(), name)

    HT = dscr("s_ht", [NCH, 128, EXT])
    KT = dscr("s_kt", [8, 128, EXT])
    VV = dscr("s_v", [EXT, 1024])
    GKV = dscr("s_gkv", [EXT, 1536])
    GKT = dscr("s_gkt", [4, 128, EXT])
    LRT = dscr("s_lrt", [16, EXT], F32)
    GQT = dscr("s_gqt", [4, 128, OWN])
    MQT = dscr("s_mqt", [8, 128, OWN])
    SGT = dscr("s_sgt", [32, 128, OWN])
    GS = dscr("s_gs", [OWN, 2048])
    OT = dscr("s_ot", [16, 128, OWN])
    MT = dscr("s_mt", [16, 128, OWN])
    dbg_outs = {}

    def csb(name, shape, dt):
        return Buf(nc.alloc_sbuf_tensor(name, shape, dt), name)
    uniq = [0]

    def sb(name, shape, dt):
        uniq[0] += 1
        return AR.alloc("%s_%d" % (name, uniq[0]), shape, dt)

    def ps(name, shape, dt=F32):
        return Buf(nc.alloc_psum_tensor(name, shape, dt), name)

    BIG0 = ps("big0", [128, 1024])
    BIG1 = ps("big1", [128, 1024])
    PA0 = ps("pa0", [128, 512])
    PA1 = ps("pa1", [128, 512])
    PTB = [ps("ptb0", [128, 1024], BF16), ps("ptb1", [128, 1024], BF16)]
    PTk = [PTB[0].k, PTB[1].k]

    class Bank:
        def __init__(self, ap, k):
            self.ap = ap
            self.k = k
    b0a, b0b, b1a, b1b = Tok("b0a"), Tok("b0b"), Tok("b1a"), Tok("b1b")
    banks = [Bank(PA0[:, :], PA0.k), Bank(PA1[:, :], PA1.k),
             Bank(BIG0[:, 0:512], b0a), Bank(BIG0[:, 512:1024], b0b),
             Bank(BIG1[:, 0:512], b1a), Bank(BIG1[:, 512:1024], b1b)]

    def const(name, src, shape, dt=F32, q="sp"):
        b = csb(name, shape, dt)
        P.dma(q, b[:], src, writes=[b.k])
        return b
    NG = const("ng", c_ng, [128, NCH])
    WGA = const("wga", c_wga, [17, 512])
    GOUT = const("gout", c_gout, [128, 1024])
    QG = const("qg", c_qg, [128, 2])
    BVAL = const("bval", c_bval, [128, 64])
    IDF = const("idf", c_id, [128, 128])
    U2 = const("u2", c_u2, [128, 128])
    MSK = const("msk", c_msk, [128, 128])
    TRIF = const("trif", c_tri, [128, 128])
    JROW = const("jrow", c_jrow, [128, 64])
    SC = const("sc", c_sc, [128, 16])
    NEGT = const("negt", c_negt, [128, 8 * NTO])
    IDB = csb("idb", [128, 128], BF16)
    TRIB = csb("trib", [128, 128], BF16)
    ONESB = csb("onesb", [128, 128], BF16)
    KM = csb("km", [128, 8, 64], F32)
    KMB = csb("kmb", [128, 8, 64], BF16)
    AR = Arena(nc, 47000)
    P.op("dve", lambda e: e.tensor_copy(out=IDB[:], in_=IDF[:]), reads=[IDF.k], writes=[IDB.k])
    P.op("dve", lambda e: e.tensor_copy(out=TRIB[:], in_=TRIF[:]), reads=[TRIF.k], writes=[TRIB.k])
    P.op("pool", lambda e: e.memset(ONESB[:], 1.0), writes=[ONESB.k])

    R = {}

    def alloc_gemm():
        P.barrier()
        AR.reset()
        R["WBIG"] = sb("wbig", [128, NCH, 2560], BF16)
        R["WSTG"] = [sb("wstg%d" % i, [128, 2560], F32) for i in range(3)]
        R["HTG"] = [sb("htg%d" % i, [128, NCH, 512], BF16) for i in range(2)]
    wstg_n = [0]

    def load_w(src, col_list, dst=None, nch=NCH, scale=True):
        dst = dst or R["WBIG"]
        WSTG = R["WSTG"]
        tot = sum(n for _, n in col_list)
        first = True
        for c in range(nch):
            st = WSTG[wstg_n[0] % 3]
            wstg_n[0] += 1
            off = 0
            for (c0, n) in col_list:
                P.dma("sp", st[:, off:off + n], src[c * 128:(c + 1) * 128, c0:c0 + n],
                      **({"writes": [st.k]} if off == 0 else {"wadd": [st.k]}))
                off += n
            ceng = ("pool", "act", "dve")[c % 3]
            if scale:
                if ceng == "act":
                    fn = (lambda e, st=st, c=c: e.activation(out=dst[:, c, 0:tot], in_=st[:, 0:tot], func=AF.Copy,
                                                             scale=NG[:, c:c + 1]))
                else:
                    fn = (lambda e, st=st, c=c: e.tensor_scalar(out=dst[:, c, 0:tot], in0=st[:, 0:tot],
                                                                 scalar1=NG[:, c:c + 1], scalar2=None, op0=ALU.mult))
                rd = [st.k, NG.k]
            else:
                if ceng == "act":
                    fn = (lambda e, st=st, c=c: e.copy(out=dst[:, c, 0:tot], in_=st[:, 0:tot]))
                else:
                    fn = (lambda e, st=st, c=c: e.tensor_copy(out=dst[:, c, 0:tot], in_=st[:, 0:tot]))
                rd = [st.k]
            if first:
                P.op(ceng, fn, reads=rd, writes=[dst.k])
                first = False
            else:
                P.op(ceng, fn, reads=rd, wadd=[dst.k])

    cp_n = [0]

    def evac_copy(out_ap, in_ap, reads, writes=(), wadd=()):
        cp_n[0] += 1
        if cp_n[0] % 2 == 0:
            P.op("act", lambda e: e.copy(out=out_ap, in_=in_ap), reads=reads, writes=writes, wadd=wadd)
        else:
            P.op("dve", lambda e: e.tensor_copy(out=out_ap, in_=in_ap), reads=reads, writes=writes, wadd=wadd)

    if stop == 0:
        return done()
    XT = [sb("xt%d" % i, [128, D], F32) for i in range(2)]
    JUNK = sb("junk", [128, D], BF16)
    HB = [sb("hb%d" % i, [128, D], BF16) for i in range(2)]
    SSQ = [sb("ssq%d" % i, [128, 1], F32) for i in range(2)]
    HTT = [sb("htt%d" % i, [128, NCH, 128], BF16) for i in range(2)]
    def ph1_a(i):
        xt, hb, ssq = XT[i % 2], HB[i % 2], SSQ[i % 2]
        P.dma("sp", xt[:], x[i * 128:(i + 1) * 128, :], writes=[xt.k])
        P.op("act", lambda e: e.activation(out=JUNK[:], in_=xt[:], func=AF.Square, accum_out=ssq[:]),
             reads=[xt.k], writes=[JUNK.k, ssq.k])
        P.op("dve", lambda e: e.tensor_scalar(out=ssq[:], in0=ssq[:], scalar1=1.0 / D, scalar2=EPS,
                                              op0=ALU.mult, op1=ALU.add), reads=[ssq.k], writes=[ssq.k])
        P.op("act", lambda e: e.activation(out=ssq[:], in_=ssq[:], func=AF.Sqrt), reads=[ssq.k], writes=[ssq.k])
        P.op("dve", lambda e: e.reciprocal(out=ssq[:], in_=ssq[:]), reads=[ssq.k], writes=[ssq.k])
        P.op("dve", lambda e: e.tensor_scalar(out=hb[:], in0=xt[:], scalar1=ssq[:, 0:1], scalar2=None, op0=ALU.mult),
             reads=[xt.k, ssq.k], writes=[hb.k])

    def ph1_b(i):
        hb, htt = HB[i % 2], HTT[i % 2]
        for g4 in range(4):
            half = g4 % 2
            for j in range(4):
                c = g4 * 4 + j
                P.op("pe", lambda e, c=c, half=half, j=j: e.transpose(
                    out=PTB[half][:, j * 128:(j + 1) * 128], in_=hb[:, c * 128:(c + 1) * 128],
                    identity=IDB[:]), reads=[hb.k, IDB.k],
                    **({"writes": [PTk[half]]} if j == 0 else {"wadd": [PTk[half]]}))
            evac_copy(htt[:, g4 * 4:(g4 + 1) * 4, :], PTB[half][:, 0:512].rearrange("p (a b) -> p a b", b=128),
                      [PTk[half]], **({"writes": [htt.k]} if g4 == 0 else {"wadd": [htt.k]}))
        for c4 in range(4):
            P.dma("pool", HT[c4 * 4:(c4 + 1) * 4, :, i * 128:(i + 1) * 128].rearrange("c p t -> p c t"),
                  htt[:, c4 * 4:(c4 + 1) * 4, :], reads=[htt.k], wadd=[HT.k], st=htt.k)
    ph1_a(0)
    for i in range(NT):
        if i + 1 < NT:
            ph1_a(i + 1)
        ph1_b(i)

    if stop == 1:
        return done()
    bank_n = [0]

    def next_bank():
        b = banks[bank_n[0] % len(banks)]
        bank_n[0] += 1
        return b

    def load_htg(tok0, gi):
        htg = R["HTG"][gi % 2]
        P.dma("sp", htg[:], HT[:, :, tok0:tok0 + 512].rearrange("c p t -> p c t"), reads=[HT.k], writes=[htg.k])
        return htg

    def pass_fm(tok0, ntok, jobs):
        pend = None
        WBIG = R["WBIG"]
        ng_ = ntok // 512
        nxt = load_htg(tok0, 0)
        for g in range(ng_):
            htg = nxt
            if g + 1 < ng_:
                nxt = load_htg(tok0 + (g + 1) * 512, g + 1)
            for (woff, M, epi) in jobs:
                b = next_bank()
                for c in range(NCH):
                    P.op("pe", lambda e, b=b, c=c, htg=htg, woff=woff, M=M: e.matmul(
                        b.ap[0:M, :], WBIG[:, c, woff:woff + M], htg[:, c, :], start=(c == 0), stop=(c == NCH - 1)),
                        reads=[WBIG.k, htg.k], **({"writes": [b.k]} if c == 0 else {"wadd": [b.k]}))
                if pend is not None:
                    pend()
                pend = epi(g, b)
        if pend is not None:
            pend()

    def pass_tm(tok0, ntok, jobs):
        WBIG = R["WBIG"]
        ng_ = ntok // 512
        nxt = load_htg(tok0, 0)
        for g in range(ng_):
            htg = nxt
            if g + 1 < ng_:
                nxt = load_htg(tok0 + (g + 1) * 512, g + 1)
            for t4 in range(4):
                for (woff, N, epi) in jobs:
                    b = next_bank()
                    for c in range(NCH):
                        P.op("pe", lambda e, b=b, c=c, htg=htg, woff=woff, N=N, t4=t4: e.matmul(
                            b.ap[:, 0:N], htg[:, c, t4 * 128:(t4 + 1) * 128], WBIG[:, c, woff:woff + N],
                            start=(c == 0), stop=(c == NCH - 1)),
                            reads=[WBIG.k, htg.k], **({"writes": [b.k]} if c == 0 else {"wadd": [b.k]}))
                    epi(g * 4 + t4, b)

    alloc_gemm()
    FST = [sb("fst%d" % i, [128, 512], BF16) for i in range(4)]
    fst_n = [0]
    SQB = [sb("sqb%d" % i, [128, 512], BF16) for i in range(2)]
    RINV = [sb("rinv%d" % i, [128, 512], F32) for i in range(2)]
    KNF = [sb("knf%d" % i, [128, 512], F32) for i in range(2)]
    LRS = [sb("lrs%d" % i, [16, 512], F32) for i in range(2)]
    TST = [sb("tst%d" % i, [128, 2560], BF16) for i in range(2)]
    qk_n = [0]

    def epi_store_fm(dst, chunk, ntok_total, func=None):
        def epi(g, b):
            st = FST[fst_n[0] % 4]
            fst_n[0] += 1
            if func is None:
                evac_copy(st[:], b.ap, [b.k], writes=[st.k])
            else:
                P.op("act", lambda e: e.activation(out=st[:], in_=b.ap, func=func), reads=[b.k], writes=[st.k])
            P.dma("pool", dst[chunk, :, g * 512:(g + 1) * 512], st[:], reads=[st.k], wadd=[dst.k], st=st.k)
            return None
        return epi

    def epi_qknorm(dst, head, gcol, want_mean):
        def epi(g, b):
            i = qk_n[0] % 2
            qk_n[0] += 1
            sqb, rinv, knf = SQB[i], RINV[i], KNF[i]
            P.op("act", lambda e: e.activation(out=sqb[:], in_=b.ap, func=AF.Square), reads=[b.k], writes=[sqb.k])

            def deferred():
                b2 = next_bank()
                P.op("pe", lambda e: e.matmul(b2.ap, ONESB[:], sqb[:], start=True, stop=True),
                     reads=[ONESB.k, sqb.k], writes=[b2.k])
                P.op("dve", lambda e: e.tensor_scalar(out=rinv[:], in0=b2.ap, scalar1=1.0 / 128, scalar2=EPS,
                                                      op0=ALU.mult, op1=ALU.add), reads=[b2.k], writes=[rinv.k])
                P.op("act", lambda e: e.activation(out=rinv[:], in_=rinv[:], func=AF.Sqrt), reads=[rinv.k], writes=[rinv.k])
                P.op("dve", lambda e: e.reciprocal(out=rinv[:], in_=rinv[:]), reads=[rinv.k], writes=[rinv.k])
                st = FST[fst_n[0] % 4]
                fst_n[0] += 1
                if want_mean:
                    P.op("dve", lambda e: e.scalar_tensor_tensor(out=knf[:], in0=b.ap, scalar=QG[:, gcol:gcol + 1],
                                                                  in1=rinv[:], op0=ALU.mult, op1=ALU.mult),
                         reads=[b.k, QG.k, rinv.k], writes=[knf.k])
                    P.op("dve", lambda e: e.tensor_reduce(out=KM[:, head, 2 * g:2 * g + 2],
                                                          in_=knf[:].rearrange("p (a b) -> p a b", b=256),
                                                          axis=AX.X, op=ALU.add), reads=[knf.k], wadd=[KM.k])
                    P.op("pool", lambda e: e.tensor_copy(out=st[:], in_=knf[:]), reads=[knf.k], writes=[st.k])
                else:
                    P.op("dve", lambda e: e.scalar_tensor_tensor(out=st[:], in0=b.ap, scalar=QG[:, gcol:gcol + 1],
                                                                  in1=rinv[:], op0=ALU.mult, op1=ALU.mult),
                         reads=[b.k, QG.k, rinv.k], writes=[st.k])
                P.dma("pool", dst[head, :, g * 512:(g + 1) * 512], st[:], reads=[st.k], wadd=[dst.k], st=st.k)
            return deferred
        return epi

    load_w(w_in, [(C_MK, 1024), (C_GK, 512), (C_LR, 16)])
    lr_n = [0]

    def epi_lr(g, b):
        st = LRS[lr_n[0] % 2]
        lr_n[0] += 1
        evac_copy(st[:], b.ap[0:16, :], [b.k], writes=[st.k])
        P.dma("pool", LRT[:, g * 512:(g + 1) * 512], st[:], reads=[st.k], wadd=[LRT.k], st=st.k)
        return None
    jobsA = [(h * 128, 128, epi_qknorm(KT, h, 1, True)) for h in range(8)]
    jobsA += [(1024 + h * 128, 128, epi_store_fm(GKT, h, EXT)) for h in range(4)]
    jobsA += [(1536, 16, epi_lr)]
    pass_fm(0, EXT, jobsA)
    if stop == 2:
        return done()
    P.op("dve", lambda e: e.tensor_scalar(out=KMB[:], in0=KM[:], scalar1=1.0 / 256, scalar2=None, op0=ALU.mult),
         reads=[KM.k], writes=[KMB.k])

    load_w(w_in, [(C_MV, 1024), (C_GK, 512), (C_GV, 1024)])

    def mk_epi_tm(col0, N, last, dsts):
        def epi(ti, b):
            st = TST[ti % 2]
            evac_copy(st[:, col0:col0 + N], b.ap[:, 0:N], [b.k], **({"writes": [st.k]} if col0 == 0 else {"wadd": [st.k]}))
            if last:
                for (dst, s0, n) in dsts:
                    P.dma("pool", dst[ti * 128:(ti + 1) * 128, :], st[:, s0:s0 + n], reads=[st.k], wadd=[dst.k], st=st.k)
        return epi
    dstsB = [(VV, 0, 1024), (GKV, 1024, 1536)]
    jobsB = [(i * 512, 512, mk_epi_tm(i * 512, 512, i == 4, dstsB)) for i in range(5)]
    pass_tm(0, EXT, jobsB)

    if stop == 3:
        return done()
    load_w(w_in, [(C_MQ, 1024), (C_GQ, 512)])
    jobsD = [(h * 128, 128, epi_qknorm(MQT, h, 0, False)) for h in range(8)]
    jobsD += [(1024 + h * 128, 128, epi_store_fm(GQT, h, OWN)) for h in range(4)]
    pass_fm(PRE, OWN, jobsD)
    for gi, c0 in enumerate((C_GA, C_GB)):
        load_w(w_in, [(c0, 2048)])
        pass_fm(PRE, OWN, [(n * 128, 128, epi_store_fm(SGT, gi * 16 + n, OWN, func=AF.Sigmoid)) for n in range(16)])
    load_w(w_in, [(C_GS, 1024), (C_MS, 1024)])

    def mk_epi_silu(col0, last):
        def epi(ti, b):
            st = TST[ti % 2]
            P.op("act", lambda e: e.activation(out=st[:, col0:col0 + 512], in_=b.ap, func=AF.Silu), reads=[b.k],
                 **({"writes": [st.k]} if col0 == 0 else {"wadd": [st.k]}))
            if last:
                P.dma("pool", GS[ti * 128:(ti + 1) * 128, :], st[:, 0:2048], reads=[st.k], wadd=[GS.k], st=st.k)
        return epi
    pass_tm(PRE, OWN, [(i * 512, 512, mk_epi_silu(i * 512, i == 3)) for i in range(4)])

    if stop == 4:
        return done()
    P.barrier()
    AR.reset()
    KTS = sb("kts", [128, EXT], BF16)
    VP = sb("vp", [128, NT, 129], BF16)
    QS = sb("qs", [128, OWN], BF16)
    GSH = sb("gsh", [128, NTO, 128], BF16)
    SELB = [(sb("gate", [128, 64], F32), sb("t8", [128, 8], F32), sb("msel", [128, 64], F32),
             sb("dex", [128, 64], F32), sb("dm", [128, 64], F32)) for _ in range(2)]
    PTS = [sb("pts%d" % i, [128, 512], BF16) for i in range(3)]
    ACC = sb("acc", [128, 129], F32)
    RDEN = sb("rden", [128, 1], F32)
    OB = [sb("ob%d" % i, [128, 128], BF16) for i in range(2)]
    OST = [sb("ost%d" % i, [128, 512], BF16) for i in range(2)]
    SBANKS = [banks[0], banks[1]]
    OBANKS = [banks[3], banks[4], banks[5]]
    GBANK = banks[2]
    sct = [0, 0, 0, 0]
    pend_fin = [None]
    for h in range(8):
        slope = float(2.0 ** (-(h + 1)))
        P.dma("sp", KTS[:], KT[h, :, :], reads=[KT.k], writes=[KTS.k])
        vsrc = VV[:, h * 128:(h + 1) * 128].rearrange("(t p) d -> p t d", p=128)
        for t0_ in range(0, NT, 16):
            t1_ = min(NT, t0_ + 16)
            P.dma("sp", VP[:, t0_:t1_, 0:128], vsrc[:, t0_:t1_, :], reads=[VV.k],
                  **({"writes": [VP.k]} if t0_ == 0 else {"wadd": [VP.k]}))
        P.op("pool", lambda e: e.memset(VP[:, :, 128:129], 1.0), wadd=[VP.k])
        vpv = VP[:].rearrange("p (a two) d -> p a two d", two=2)
        for par in range(2):
            P.op("dve", lambda e, par=par, h=h: e.tensor_scalar(
                out=vpv[:, :, par, :], in0=vpv[:, :, par, :], scalar1=SC[:, 2 * h + par:2 * h + par + 1],
                scalar2=None, op0=ALU.mult), reads=[VP.k, SC.k], writes=[VP.k])
        P.dma("sp", QS[:], MQT[h, :, :], reads=[MQT.k], writes=[QS.k])
        gsrc = GS[:, 1024 + h * 128:1024 + (h + 1) * 128].rearrange("(t p) d -> p t d", p=128)
        for t0_ in range(0, NTO, 16):
            t1_ = min(NTO, t0_ + 16)
            P.dma("sp", GSH[:, t0_:t1_, :], gsrc[:, t0_:t1_, :], reads=[GS.k],
                  **({"writes": [GSH.k]} if t0_ == 0 else {"wadd": [GSH.k]}))
        def prologue(qt, bufs, h=h, slope=slope):
            G, T8, MSEL, DEX, DM = bufs
            eq = PRE // 128 + qt
            ob_ = eq // 2
            nblk = ob_ + 1
            qsl = QS[:, qt * 128:(qt + 1) * 128]
            P.op("pool", lambda e: e.memset(G[:], NEG), writes=[G.k])
            P.op("pool", lambda e: e.memset(MSEL[:], 1.0), writes=[MSEL.k])
            if ob_ > 0:
                P.op("pe", lambda e: e.matmul(GBANK.ap[:, 0:ob_], qsl, KMB[:, h, 0:ob_], start=True, stop=True),
                     reads=[QS.k, KMB.k], writes=[GBANK.k])
                P.op("dve", lambda e: e.tensor_tensor(out=G[:, 0:ob_], in0=GBANK.ap[:, 0:ob_],
                                                      in1=BVAL[:, 0:ob_], op=ALU.add),
                     reads=[GBANK.k, BVAL.k], wadd=[G.k])
                P.op("dve", lambda e: e.max(out=T8[:], in_=G[:]), reads=[G.k], writes=[T8.k])
                P.op("dve", lambda e: e.tensor_scalar_max(out=T8[:, 2:3], in0=T8[:, 2:3], scalar1=-1.0e29),
                     reads=[T8.k], writes=[T8.k])
                P.op("dve", lambda e: e.tensor_scalar(out=MSEL[:, 0:ob_], in0=G[:, 0:ob_],
                                                      scalar1=T8[:, 2:3], scalar2=None, op0=ALU.is_ge),
                     reads=[G.k, T8.k], wadd=[MSEL.k])
            P.op("act", lambda e: e.activation(
                out=DEX[:, 0:nblk], in_=JROW[:, 0:nblk], func=AF.Exp, scale=slope,
                bias=NEGT[:, h * NTO + qt:h * NTO + qt + 1]), reads=[JROW.k, NEGT.k], writes=[DEX.k])
            P.op("dve", lambda e: e.tensor_tensor(out=DM[:, 0:nblk], in0=DEX[:, 0:nblk],
                                                  in1=MSEL[:, 0:nblk], op=ALU.mult),
                 reads=[DEX.k, MSEL.k], writes=[DM.k])

        prologue(0, SELB[0])
        for qt in range(NTO):
            eq = PRE // 128 + qt
            ob_ = eq // 2
            qsl = QS[:, qt * 128:(qt + 1) * 128]
            DM = SELB[qt % 2][4]
            last_kt = 2 * ob_ + (1 if eq % 2 == 1 else 0)
            kts = list(range(last_kt + 1))
            groups = [kts[g0:g0 + 4] for g0 in range(0, len(kts), 4)]

            def emit_qk(gi, groups=groups, qsl=qsl):
                grp = groups[gi]
                sbk = SBANKS[sct[0] % 2]
                sct[0] += 1
                for i_, kt in enumerate(grp):
                    P.op("pe", lambda e, sbk=sbk, i_=i_, kt=kt: e.matmul(
                        sbk.ap[:, i_ * 128:(i_ + 1) * 128], KTS[:, kt * 128:(kt + 1) * 128], qsl, start=True, stop=True),
                        reads=[KTS.k, QS.k], **({"writes": [sbk.k]} if i_ == 0 else {"wadd": [sbk.k]}))
                return sbk
            first_acc = True
            sb_next = emit_qk(0)
            for gi, grp in enumerate(groups):
                sbk = sb_next
                if gi + 1 < len(groups):
                    sb_next = emit_qk(gi + 1)
                if gi == min(1, len(groups) - 1) and qt + 1 < NTO:
                    prologue(qt + 1, SELB[(qt + 1) % 2])
                if gi == min(2, len(groups) - 1) and pend_fin[0] is not None:
                    pend_fin[0]()
                    pend_fin[0] = None
                pts = PTS[sct[1] % 3]
                sct[1] += 1
                n = len(grp)
                P.op("act", lambda e, pts=pts, sbk=sbk, n=n: e.activation(
                    out=pts[:, 0:n * 128], in_=sbk.ap[:, 0:n * 128], func=AF.Exp, scale=float(128 ** -0.5)),
                    reads=[sbk.k], writes=[pts.k])
                if last_kt in grp:
                    i_ = grp.index(last_kt)
                    P.op("pool", lambda e, pts=pts, i_=i_: e.tensor_tensor(
                        out=pts[:, i_ * 128:(i_ + 1) * 128], in0=pts[:, i_ * 128:(i_ + 1) * 128], in1=TRIB[:],
                        op=ALU.mult), reads=[pts.k, TRIB.k], writes=[pts.k])
                blks = sorted(set(kt // 2 for kt in grp))
                for j in blks:
                    obk = OBANKS[sct[2] % 3]
                    sct[2] += 1
                    jk = [kt for kt in grp if kt // 2 == j]
                    for ii, kt in enumerate(jk):
                        i_ = grp.index(kt)
                        P.op("pe", lambda e, obk=obk, pts=pts, i_=i_, kt=kt, ii=ii, jk=jk: e.matmul(
                            obk.ap[:, 0:129], pts[:, i_ * 128:(i_ + 1) * 128], VP[:, kt, :],
                            start=(ii == 0), stop=(ii == len(jk) - 1)),
                            reads=[pts.k, VP.k], **({"writes": [obk.k]} if ii == 0 else {"wadd": [obk.k]}))
                    if first_acc:
                        P.op("dve", lambda e, obk=obk, j=j, DM=DM: e.tensor_scalar(
                            out=ACC[:], in0=obk.ap[:, 0:129], scalar1=DM[:, j:j + 1], scalar2=None, op0=ALU.mult),
                            reads=[obk.k, DM.k], writes=[ACC.k])
                        first_acc = False
                    else:
                        P.op("dve", lambda e, obk=obk, j=j, DM=DM: e.scalar_tensor_tensor(
                            out=ACC[:], in0=obk.ap[:, 0:129], scalar=DM[:, j:j + 1], in1=ACC[:],
                            op0=ALU.mult, op1=ALU.add), reads=[obk.k, DM.k, ACC.k], writes=[ACC.k])
            ob16 = OB[qt % 2]
            P.op("dve", lambda e: e.reciprocal(out=RDEN[:], in_=ACC[:, 128:129]), reads=[ACC.k], writes=[RDEN.k])
            P.op("dve", lambda e, ob16=ob16, qt=qt: e.scalar_tensor_tensor(
                out=ob16[:], in0=ACC[:, 0:128], scalar=RDEN[:, 0:1], in1=GSH[:, qt, :], op0=ALU.mult, op1=ALU.mult),
                reads=[ACC.k, RDEN.k, GSH.k], writes=[ob16.k])

            def fin_pe(ob16=ob16, qt=qt, h=h):
                half = (qt // 4) % 2
                j4 = qt % 4
                P.op("pe", lambda e: e.transpose(
                    out=PTB[half][:, j4 * 128:(j4 + 1) * 128], in_=ob16[:], identity=IDB[:]),
                    reads=[ob16.k, IDB.k], **({"writes": [PTk[half]]} if j4 == 0 else {"wadd": [PTk[half]]}))
                if j4 == 3:
                    ost = OST[(qt // 4) % 2]
                    evac_copy(ost[:], PTB[half][:, 0:512], [PTk[half]], writes=[ost.k])
                    P.dma("pool", OT[8 + h, :, (qt - 3) * 128:(qt + 1) * 128], ost[:], reads=[ost.k], wadd=[OT.k],
                          st=ost.k)
            pend_fin[0] = fin_pe
        if pend_fin[0] is not None:
            pend_fin[0]()
            pend_fin[0] = None

    if stop == 5:
        return done()
    P.barrier()
    AR.reset()
    JUNK2 = sb("junk2", [128, 256], BF16)
    GKVT = [sb("gkvt%d" % i, [128, 1536], BF16) for i in range(2)]
    LRA = [sb("lra%d" % i, [17, 128], F32) for i in range(2)]
    KQT = [sb("kqt%d" % i, [128, 8, 128], BF16) for i in range(2)]
    GSL = [sb("gsl%d" % i, [128, 1024], BF16) for i in range(2)]
    E1 = sb("e1", [128, 512], F32)
    SP_ = sb("sp", [128, 512], F32)
    EKTM = sb("ektm", [128, 512], F32)
    KTT = sb("ktt", [128, 512], BF16)
    EQT = sb("eqt", [128, 512], F32)
    EKT = sb("ekt", [128, 512], F32)
    KTF = sb("ktf", [128, 4, 128], BF16)
    QZ = sb("qz", [128, 4, 192], BF16)
    S = sb("S", [128, 1024], F32)
    TS_ = sb("Ts", [128, 1024], F32)
    SBF = [sb("sbf%d" % i, [128, 1024], BF16) for i in range(2)]
    ATB = sb("atb", [128, 512], BF16)
    SSG = sb("ssg", [128, 4], F32)
    GG = sb("gg", [128, 1024], F32)
    OG = sb("og", [128, 1024], BF16)
    OGT = [sb("ogt%d" % i, [128, 8, 128], BF16) for i in range(2)]
    P.op("pool", lambda e: e.memset(S[:], 0.0), writes=[S.k])
    P.op("pool", lambda e: e.memset(SBF[0][:], 0.0), writes=[SBF[0].k])
    P.op("pool", lambda e: e.memset(QZ[:], 0.0), writes=[QZ.k])
    for i in range(2):
        P.op("pool", lambda e, i=i: e.memset(LRA[i][:], 1.0), writes=[LRA[i].k])
    for ti in range(NT):
        own = ti >= PRE // 128
        to = ti - PRE // 128
        gkv, lra = GKVT[ti % 2], LRA[ti % 2]
        P.dma("sp", gkv[:], GKV[ti * 128:(ti + 1) * 128, :], reads=[GKV.k], writes=[gkv.k])
        P.dma("sp", lra[0:16, :], LRT[:, ti * 128:(ti + 1) * 128], reads=[LRT.k], wadd=[lra.k])
        if own:
            kqt, gsl = KQT[ti % 2], GSL[ti % 2]
            P.dma("sp", kqt[:, 0:4, :], GKT[:, :, ti * 128:(ti + 1) * 128].rearrange("c p t -> p c t"),
                  reads=[GKT.k], writes=[kqt.k])
            P.dma("sp", kqt[:, 4:8, :], GQT[:, :, to * 128:(to + 1) * 128].rearrange("c p t -> p c t"),
                  reads=[GQT.k], wadd=[kqt.k])
            P.dma("sp", gsl[:], GS[to * 128:(to + 1) * 128, 0:1024], reads=[GS.k], writes=[gsl.k])
        zb = banks[0]
        P.op("pe", lambda e, lra=lra: e.matmul(zb.ap, lra[:], WGA[:], start=True, stop=True),
             reads=[lra.k, WGA.k], writes=[zb.k])
        P.op("act", lambda e: e.activation(out=E1[:], in_=zb.ap, func=AF.Exp, scale=-1.0), reads=[zb.k], writes=[E1.k])
        P.op("act", lambda e: e.activation(out=SP_[:], in_=E1[:], func=AF.Ln, bias=1.0), reads=[E1.k], writes=[SP_.k])
        cb = banks[1]
        P.op("pe", lambda e: e.matmul(cb.ap, U2[:], SP_[:], start=True, stop=True), reads=[U2.k, SP_.k], writes=[cb.k])
        tb = banks[0]
        for hh in range(4):
            P.op("pe", lambda e, hh=hh: e.matmul(tb.ap[:, hh * 128:(hh + 1) * 128], SP_[:, hh * 128:(hh + 1) * 128], U2[:],
                                                 start=True, stop=True), reads=[SP_.k, U2.k],
                 **({"writes": [tb.k]} if hh == 0 else {"wadd": [tb.k]}))
        P.op("act", lambda e: e.activation(out=EKTM[:], in_=cb.ap, func=AF.Exp), reads=[cb.k], writes=[EKTM.k])
        P.op("dve", lambda e, gkv=gkv: e.tensor_tensor(out=KTT[:], in0=gkv[:, 0:512], in1=EKTM[:], op=ALU.mult),
             reads=[gkv.k, EKTM.k], writes=[KTT.k])
        P.op("act", lambda e: e.activation(out=EQT[:], in_=tb.ap, func=AF.Exp, scale=-1.0), reads=[tb.k], writes=[EQT.k])
        if own:
            P.op("act", lambda e: e.activation(out=EKT[:], in_=tb.ap, func=AF.Exp), reads=[tb.k], writes=[EKT.k])
            P.op("dve", lambda e, kqt=kqt: e.tensor_tensor(
                out=KTF[:], in0=kqt[:, 0:4, :], in1=EKT[:].rearrange("p (a b) -> p a b", b=128), op=ALU.mult),
                reads=[kqt.k, EKT.k], writes=[KTF.k])
            for c in range(2):
                P.op("dve", lambda e, kqt=kqt, c=c: e.scalar_tensor_tensor(
                    out=QZ[:, :, c * 128:c * 128 + 64], in0=kqt[:, 4:8, c * 64:(c + 1) * 64], scalar=float(128 ** -0.5),
                    in1=EQT[:].rearrange("p (a b) -> p a b", b=128)[:, :, c * 64:(c + 1) * 64],
                    op0=ALU.mult, op1=ALU.mult), reads=[kqt.k, EQT.k], wadd=[QZ.k])
            ab = banks[1]
            for hh in range(4):
                P.op("pe", lambda e, hh=hh: e.matmul(
                    ab.ap[:, hh * 128:(hh + 1) * 128].rearrange("p (a b) -> p a b", b=64), KTF[:, hh, :],
                    QZ[:, hh, :].rearrange("p (a b) -> p a b", b=64)[:, 0:3:2, :], start=True, stop=True),
                    reads=[KTF.k, QZ.k], **({"writes": [ab.k]} if hh == 0 else {"wadd": [ab.k]}))
            P.op("dve", lambda e: e.tensor_tensor(
                out=ATB[:].rearrange("p (a b) -> p a b", b=128), in0=ab.ap.rearrange("p (a b) -> p a b", b=128),
                in1=MSK[:].unsqueeze(1).to_broadcast([128, 4, 128]), op=ALU.mult), reads=[ab.k, MSK.k], writes=[ATB.k])
        def state_update(c, gkv=gkv):
            sout = SBF[(c + 1) % 2]
            for hh in range(4):
                P.op("pe", lambda e, hh=hh, c=c, gkv=gkv: e.matmul(
                    BIG1[:, hh * 256:(hh + 1) * 256], KTT[c * 64:(c + 1) * 64, hh * 128:(hh + 1) * 128],
                    gkv[c * 64:(c + 1) * 64, 512 + hh * 256:512 + (hh + 1) * 256], start=True, stop=True),
                    reads=[KTT.k, gkv.k], **({"writes": [b1a, b1b]} if hh == 0 else {"wadd": [b1a, b1b]}))
            P.op("dve", lambda e: e.tensor_tensor(out=TS_[:], in0=BIG1[:, :], in1=S[:], op=ALU.add),
                 reads=[b1a, b1b, S.k], writes=[TS_.k])
            for hh in range(4):
                col = hh * 128 + c * 64 + 63
                P.op("act", lambda e, hh=hh, col=col: e.activation(
                    out=S[:, hh * 256:(hh + 1) * 256], in_=TS_[:, hh * 256:(hh + 1) * 256], func=AF.Copy,
                    scale=EQT[:, col:col + 1]), reads=[TS_.k, EQT.k], **({"writes": [S.k]} if hh == 0 else {"wadd": [S.k]}))
            P.op("pool", lambda e, sout=sout: e.tensor_copy(out=sout[:], in_=S[:]), reads=[S.k], writes=[sout.k])
        state_update(0)
        if own:
            for hh in range(4):
                P.op("pe", lambda e, hh=hh, gkv=gkv: e.matmul(
                    BIG0[:, hh * 256:(hh + 1) * 256], ATB[:, hh * 128:(hh + 1) * 128],
                    gkv[:, 512 + hh * 256:512 + (hh + 1) * 256], start=True, stop=False),
                    reads=[ATB.k, gkv.k], **({"writes": [b0a, b0b]} if hh == 0 else {"wadd": [b0a, b0b]}))
                P.op("pe", lambda e, hh=hh: e.matmul(
                    BIG0[:, hh * 256:(hh + 1) * 256], QZ[:, hh, 0:128], SBF[0][:, hh * 256:(hh + 1) * 256],
                    start=False, stop=False), reads=[QZ.k, SBF[0].k], wadd=[b0a, b0b])
                P.op("pe", lambda e, hh=hh: e.matmul(
                    BIG0[:, hh * 256:(hh + 1) * 256], QZ[:, hh, 64:192], SBF[1][:, hh * 256:(hh + 1) * 256],
                    start=False, stop=True), reads=[QZ.k, SBF[1].k], wadd=[b0a, b0b])
        state_update(1)
        if _dbg and own and to == 0:
            def ddump(name, buf_ap, shape, dt, reads):
                dd = nc.dram_tensor(name, shape, dt, kind="ExternalOutput").ap()
                tk = Tok(name)
                P.dma("sp", dd, buf_ap, reads=reads, st=tk)
            ddump("d_sp", SP_[:], [128, 512], F32, [SP_.k])
            ddump("d_ektm", EKTM[:], [128, 512], F32, [EKTM.k])
            ddump("d_eqt", EQT[:], [128, 512], F32, [EQT.k])
            ddump("d_ktt", KTT[:], [128, 512], BF16, [KTT.k])
            ddump("d_atb", ATB[:], [128, 512], BF16, [ATB.k])
            ddump("d_qz", QZ[:], [128, 4, 192], BF16, [QZ.k])
            ddump("d_ktf", KTF[:], [128, 4, 128], BF16, [KTF.k])
            ddump("d_S", S[:], [128, 1024], F32, [S.k])
            P.op("dve", lambda e: e.tensor_copy(out=GG[:], in_=BIG0[:, :]), reads=[b0a, b0b], writes=[GG.k])
            ddump("d_o", GG[:], [128, 1024], F32, [GG.k])
        if own:
            for hh in range(4):
                P.op("act", lambda e, hh=hh: e.activation(out=JUNK2[:, 0:256], in_=BIG0[:, hh * 256:(hh + 1) * 256],
                                                          func=AF.Square, accum_out=SSG[:, hh:hh + 1]),
                     reads=[b0a, b0b], writes=[JUNK2.k], wadd=[SSG.k])
            P.op("dve", lambda e: e.tensor_scalar(out=SSG[:], in0=SSG[:], scalar1=1.0 / 256, scalar2=EPS,
                                                  op0=ALU.mult, op1=ALU.add), reads=[SSG.k], writes=[SSG.k])
            P.op("act", lambda e: e.activation(out=SSG[:], in_=SSG[:], func=AF.Sqrt), reads=[SSG.k], writes=[SSG.k])
            P.op("dve", lambda e: e.reciprocal(out=SSG[:], in_=SSG[:]), reads=[SSG.k], writes=[SSG.k])
            P.op("pool", lambda e, gsl=gsl: e.tensor_tensor(out=GG[:], in0=gsl[:], in1=GOUT[:], op=ALU.mult),
                 reads=[gsl.k, GOUT.k], writes=[GG.k])
            for hh in range(4):
                P.op("dve", lambda e, hh=hh: e.scalar_tensor_tensor(
                    out=OG[:, hh * 256:(hh + 1) * 256], in0=BIG0[:, hh * 256:(hh + 1) * 256], scalar=SSG[:, hh:hh + 1],
                    in1=GG[:, hh * 256:(hh + 1) * 256], op0=ALU.mult, op1=ALU.mult),
                    reads=[b0a, b0b, SSG.k, GG.k], **({"writes": [OG.k]} if hh == 0 else {"wadd": [OG.k]}))
            ogt = OGT[ti % 2]
            for half in range(2):
                for j in range(4):
                    c8 = half * 4 + j
                    P.op("pe", lambda e, c8=c8, half=half, j=j: e.transpose(
                        out=PTB[half][:, j * 128:(j + 1) * 128], in_=OG[:, c8 * 128:(c8 + 1) * 128],
                        identity=IDB[:]), reads=[OG.k, IDB.k],
                        **({"writes": [PTk[half]]} if j == 0 else {"wadd": [PTk[half]]}))
                evac_copy(ogt[:, half * 4:(half + 1) * 4, :],
                          PTB[half][:, 0:512].rearrange("p (a b) -> p a b", b=128), [PTk[half]],
                          **({"writes": [ogt.k]} if half == 0 else {"wadd": [ogt.k]}))
            P.dma("pool", OT[0:8, :, to * 128:(to + 1) * 128].rearrange("c p t -> p c t"), ogt[:], reads=[ogt.k],
                  wadd=[OT.k], st=ogt.k)

    if stop == 6:
        return done()
    alloc_gemm()
    WZ = R["WBIG"]
    WSTG = R["WSTG"]
    load_w(w_bg, [(0, 2048)], nch=8, scale=False)
    first = True
    for c in range(8):
        st = WSTG[wstg_n[0] % 3]
        wstg_n[0] += 1
        P.dma("sp", st[:, 0:2048], w_bm[c * 128:(c + 1) * 128, :], writes=[st.k])
        P.op("pool", lambda e, st=st, c=c: e.tensor_copy(out=WZ[:, 8 + c, 0:2048], in_=st[:, 0:2048]),
             reads=[st.k], wadd=[WZ.k])
    OTG = R["HTG"]
    SGG = [sb("sgg%d" % i, [128, 32, 512], BF16) for i in range(1)]
    T1 = [sb("t1_%d" % i, [128, 512], BF16) for i in range(2)]
    T2 = [sb("t2_%d" % i, [128, 512], BF16) for i in range(2)]
    MTS = [sb("mts%d" % i, [128, 512], BF16) for i in range(2)]
    for g in range(OWN // 512):
        otg = OTG[g % 2]
        sgg = SGG[0]
        P.dma("sp", otg[:], OT[:, :, g * 512:(g + 1) * 512].rearrange("c p t -> p c t"), reads=[OT.k], writes=[otg.k])
        for c0_ in (0, 16):
            P.dma("sp", sgg[:, c0_:c0_ + 16, :], SGT[c0_:c0_ + 16, :, g * 512:(g + 1) * 512].rearrange("c p t -> p c t"),
                  reads=[SGT.k], **({"writes": [sgg.k]} if c0_ == 0 else {"wadd": [sgg.k]}))
        for n in range(16):
            bg, bm = next_bank(), next_bank()
            for br, bnk in ((0, bg), (1, bm)):
                for c in range(8):
                    P.op("pe", lambda e, bnk=bnk, br=br, c=c, n=n, otg=otg: e.matmul(
                        bnk.ap, WZ[:, br * 8 + c, n * 128:(n + 1) * 128], otg[:, br * 8 + c, :],
                        start=(c == 0), stop=(c == 7)), reads=[WZ.k, otg.k],
                        **({"writes": [bnk.k]} if c == 0 else {"wadd": [bnk.k]}))
            t1, t2, mts = T1[n % 2], T2[n % 2], MTS[n % 2]
            P.op("dve", lambda e, t1=t1, bg=bg, n=n, sgg=sgg: e.tensor_tensor(out=t1[:], in0=bg.ap, in1=sgg[:, n, :], op=ALU.mult),
                 reads=[bg.k, sgg.k], writes=[t1.k])
            P.op("dve", lambda e, t2=t2, bm=bm, n=n, sgg=sgg: e.tensor_tensor(out=t2[:], in0=bm.ap, in1=sgg[:, 16 + n, :], op=ALU.mult),
                 reads=[bm.k, sgg.k], writes=[t2.k])
            P.op("pool", lambda e, t1=t1, t2=t2, mts=mts: e.tensor_tensor(out=mts[:], in0=t1[:], in1=t2[:], op=ALU.add),
                 reads=[t1.k, t2.k], writes=[mts.k])
            P.dma("pool", MT[n, :, g * 512:(g + 1) * 512], mts[:], reads=[mts.k], wadd=[MT.k], st=mts.k)

    if stop == 7:
        return done()
    alloc_gemm()
    WBIG = R["WBIG"]
    load_w(w_o, [(0, 2048)], scale=False)
    XT = [sb("xtz%d" % i, [128, D], F32) for i in range(2)]
    YT = [sb("yt%d" % i, [128, D], F32) for i in range(2)]
    for g in range(OWN // 512):
        mtg = R["HTG"][g % 2]
        P.dma("sp", mtg[:], MT[:, :, g * 512:(g + 1) * 512].rearrange("c p t -> p c t"), reads=[MT.k], writes=[mtg.k])
        for t4 in range(4):
            ti = g * 4 + t4
            xt, yt = XT[ti % 2], YT[ti % 2]
            P.dma("sp", xt[:], x[PRE + ti * 128:PRE + (ti + 1) * 128, :], writes=[xt.k])
            for ng in range(4):
                b = next_bank()
                for c in range(NCH):
                    P.op("pe", lambda e, b=b, c=c, mtg=mtg, t4=t4, ng=ng: e.matmul(
                        b.ap, mtg[:, c, t4 * 128:(t4 + 1) * 128], WBIG[:, c, ng * 512:(ng + 1) * 512],
                        start=(c == 0), stop=(c == NCH - 1)), reads=[mtg.k, WBIG.k],
                        **({"writes": [b.k]} if c == 0 else {"wadd": [b.k]}))
                P.op("dve", lambda e, b=b, yt=yt, xt=xt, ng=ng: e.tensor_tensor(
                    out=yt[:, ng * 512:(ng + 1) * 512], in0=b.ap, in1=xt[:, ng * 512:(ng + 1) * 512], op=ALU.add),
                    reads=[b.k, xt.k], **({"writes": [yt.k]} if ng == 0 else {"wadd": [yt.k]}))
            P.dma("pool", y[ti * 128:(ti + 1) * 128, :], yt[:], reads=[yt.k], st=yt.k)

    P.finish()
    P.emit()
    return nc


def make_consts(EXT, OWN):
    NTO = OWN // 128
    PRE = EXT - OWN
    p = np.arange(128)
    same = (p[:, None] // 64) == (p[None, :] // 64)
    le = p[:, None] <= p[None, :]
    msk = (same & le).astype(np.float32)
    slopes = 2.0 ** (-8.0 * np.arange(1, 9, dtype=np.float64) / 8)
    sc = np.zeros((128, 16), np.float32)
    negt = np.zeros((128, 8 * NTO), np.float32)
    for h in range(8):
        sc[:, 2 * h] = np.exp(slopes[h] * (p - 128.0))
        sc[:, 2 * h + 1] = np.exp(slopes[h] * (p * 1.0))
        for qt in range(NTO):
            negt[:, h * NTO + qt] = -slopes[h] * (PRE + qt * 128 + p)
    return {
        "c_id": np.eye(128, dtype=np.float32),
        "c_u2": (msk / 16.0).astype(np.float32),
        "c_msk": msk,
        "c_tri": le.astype(np.float32),
        "c_jrow": np.broadcast_to((256.0 * np.arange(64) + 128.0).astype(np.float32), (128, 64)).copy(),
        "c_sc": sc,
        "c_negt": negt,
    }


def make_in_maps(inputs, EXT, OWN, nseg, ncores=8):
    x = np.asarray(inputs["x"], np.float32)
    B = x.shape[0]
    consts = make_consts(EXT, OWN)
    shared = dict(consts)
    shared["w_in"] = np.ascontiguousarray(np.asarray(inputs["w_in"], np.float32)[0])
    shared["w_bg"] = np.ascontiguousarray(np.asarray(inputs["w_branch_gla"], np.float32)[0])
    shared["w_bm"] = np.ascontiguousarray(np.asarray(inputs["w_branch_moba"], np.float32)[0])
    shared["w_o"] = np.ascontiguousarray(np.asarray(inputs["w_out"], np.float32)[0])
    shared["c_ng"] = np.ascontiguousarray(np.asarray(inputs["norm_g"], np.float32)[0].reshape(NCH, 128).T)
    shared["c_wga"] = np.concatenate([np.asarray(inputs["w_gla_gate"], np.float32)[0],
                                      np.asarray(inputs["b_gla_gate"], np.float32)[0][None, :]], axis=0)
    shared["c_gout"] = np.ascontiguousarray(np.broadcast_to(
        np.tile(np.asarray(inputs["gla_out_g"], np.float32)[0], 4)[None, :], (128, 1024)))
    shared["c_qg"] = np.ascontiguousarray(np.stack([np.asarray(inputs["q_norm_g"], np.float32)[0],
                                                    np.asarray(inputs["k_norm_g"], np.float32)[0]], axis=1))
    maps = []
    for c in range(ncores):
        b, i = (c // nseg) % B, c % nseg
        end = (i + 1) * OWN
        pad = EXT - end
        xe = np.zeros((EXT, D), np.float32)
        xe[pad:] = x[b, :end]
        bval = np.zeros((128, 64), np.float32)
        bval[:, :pad // 256] = NEG
        m = dict(shared)
        m["x"] = xe
        m["c_bval"] = bval
        maps.append(m)
    return maps


_NC_CACHE = {}


def kernel(**inputs):
    EXT, OWN, nseg = 16384, 4096, 4
    x = np.asarray(inputs["x"])
    B, S, _ = x.shape
    key = (EXT, OWN)
    if key not in _NC_CACHE:
        _NC_CACHE[key] = build(EXT, OWN)
    nc = _NC_CACHE[key]
    maps = make_in_maps(inputs, EXT, OWN, nseg)
    res = run_bass_kernel_spmd(nc, maps, core_ids=list(range(8)))
    out = np.empty((B, S, D), np.float32)
    for c in range(8):
        b, i = c // nseg, c % nseg
        out[b, i * OWN:(i + 1) * OWN] = res.results[c]["y"]
    return out
```

```python
import numpy as np
import ml_dtypes
import concourse.bass as bass
import concourse.mybir as mybir
from concourse.bass_utils import run_bass_kernel_spmd

F32 = mybir.dt.float32
BF16 = mybir.dt.bfloat16
AF = mybir.ActivationFunctionType
ALU = mybir.AluOpType
AX = mybir.AxisListType

D = 2048
NCH = 16
PROJ = 11280
C_GQ, C_GK, C_GV, C_LR, C_GS, C_MQ, C_MK, C_MV, C_MS, C_GA, C_GB = (
    0, 512, 1024, 2048, 2064, 3088, 4112, 5136, 6160, 7184, 9232)
EPS = 1e-6
NEG = -1.0e30


class Tok:
    __slots__ = ("name", "w", "r", "dsem")

    def __init__(self, name):
        self.name = name
        self.w = {}
        self.r = {}
        self.dsem = None


class Prog:
    ENG = ("pe", "act", "dve", "pool", "sp")

    def __init__(self, nc):
        self.nc = nc
        self.q = {e: [] for e in self.ENG}
        self.cnt = {e: 0 for e in self.ENG}
        self.esem = {e: nc.alloc_semaphore("es_" + e) for e in self.ENG}
        self.seen = {e: {} for e in self.ENG}
        self.dsems = []
        self.retired = []
        self.nsem = 0

    def _deps(self, reads, writes):
        deps = {}
        for t in reads:
            for s, v in t.w.items():
                if deps.get(s, 0) < v:
                    deps[s] = v
        for t in writes:
            for s, v in t.w.items():
                if deps.get(s, 0) < v:
                    deps[s] = v
            for s, v in t.r.items():
                if deps.get(s, 0) < v:
                    deps[s] = v
        return deps

    def _waits(self, e, deps, skip_own=False):
        waits = []
        seen = self.seen[e]
        own = self.esem[e]
        for s, v in deps.items():
            if skip_own and s is own:
                continue
            if seen.get(s, 0) < v:
                seen[s] = v
                waits.append((s, v))
        return waits

    def op(self, e, fn, reads=(), writes=(), wadd=()):
        allw = tuple(writes) + tuple(wadd)
        deps = self._deps(reads, allw)
        waits = self._waits(e, deps, skip_own=(e == "pe"))
        if self.cnt[e] >= 30000:
            self.retired.append((self.esem[e], self.cnt[e]))
            self.nsem += 1
            self.esem[e] = self.nc.alloc_semaphore("es_%s_%d" % (e, self.nsem))
            self.cnt[e] = 0
        self.cnt[e] += 1
        c = self.cnt[e]
        sem = self.esem[e]

        def run(eng, waits=waits, fn=fn, sem=sem):
            for s, v in waits:
                eng.wait_ge(s, v)
            fn(eng).then_inc(sem, 1)
        self.q[e].append(run)
        for t in writes:
            t.w = {sem: c}
            t.r = {}
        for t in wadd:
            t.w[sem] = c
        for t in reads:
            t.r[sem] = c

    def dma(self, qe, out, in_, reads=(), writes=(), wadd=(), st=None):
        allw = tuple(writes) + tuple(wadd)
        if st is None:
            st = (allw + tuple(reads))[0]
        if st.dsem is None:
            st.dsem = [self.nc.alloc_semaphore("d_" + st.name), 0]
            self.dsems.append(st)
        sem, tot = st.dsem
        deps = self._deps(reads, allw)
        if tot > 0:
            deps[sem] = max(deps.get(sem, 0), tot)
        waits = self._waits(qe, deps)
        v = tot + 16
        st.dsem[1] = v

        def run(eng, waits=waits, sem=sem, out=out, in_=in_):
            for s, vv in waits:
                eng.wait_ge(s, vv)
            eng.dma_start(out=out, in_=in_).then_inc(sem, 16)
        self.q[qe].append(run)
        for t in writes:
            t.w = {sem: v}
            t.r = {}
        for t in wadd:
            t.w[sem] = v
        for t in reads:
            t.r[sem] = max(t.r.get(sem, 0), v)

    def barrier(self):
        for e in self.ENG:
            deps = {}
            for st in self.dsems:
                sem, tot = st.dsem
                deps[sem] = tot
            for f in self.ENG:
                if f != e and self.cnt[f] > 0:
                    deps[self.esem[f]] = self.cnt[f]
            for rs, rv in self.retired:
                deps[rs] = rv
            waits = self._waits(e, deps)

            def run(eng, waits=waits):
                for s, v in waits:
                    eng.wait_ge(s, v)
            self.q[e].append(run)

    def finish(self):
        deps = {}
        for st in self.dsems:
            sem, tot = st.dsem
            deps[sem] = tot
        for e in self.ENG:
            if e != "sp" and self.cnt[e] > 0:
                deps[self.esem[e]] = self.cnt[e]
        for rs, rv in self.retired:
            deps[rs] = rv
        waits = self._waits("sp", deps)

        def run(eng, waits=waits):
            for s, v in waits:
                eng.wait_ge(s, v)
        self.q["sp"].append(run)

    def emit(self):
        nc = self.nc
        q = self.q
        with nc.Block() as block:
            @block.tensor
            def _(eng):
                for f in q["pe"]:
                    f(eng)

            @block.scalar
            def _(eng):
                for f in q["act"]:
                    f(eng)

            @block.vector
            def _(eng):
                for f in q["dve"]:
                    f(eng)

            @block.gpsimd
            def _(eng):
                for f in q["pool"]:
                    f(eng)

            @block.sync
            def _(eng):
                for f in q["sp"]:
                    f(eng)


class Buf:
    def __init__(self, t, name):
        self.t = t
        self.k = Tok(name)

    def __getitem__(self, key):
        return self.t[key]


class Arena:
    def __init__(self, nc, words):
        self.t = nc.alloc_sbuf_tensor("arena", [128, words], F32)
        self.words = words
        self.off = 0

    def reset(self):
        self.off = 0

    def alloc(self, name, shape, dt):
        nb = 2 if dt == BF16 else 4
        free = int(np.prod(shape[1:]))
        w = (free * nb + 3) // 4
        w = (w + 7) // 8 * 8
        assert self.off + w <= self.words, (name, self.off, w, self.words)
        v = self.t[0:shape[0], self.off:self.off + w]
        self.off += w
        if dt == BF16:
            v = v.bitcast(BF16)
        v = v[:, 0:free]
        if len(shape) == 3:
            v = v.rearrange("p (a b) -> p a b", b=shape[2])
        return Buf(v, name)


def build(EXT, OWN, stop=99):
    nc = bass.Bass("TRN2", target_bir_lowering=False)
    P = Prog(nc)

    def done():
        P.finish()
        P.emit()
        return nc
    NT = EXT // 128
    NTO = OWN // 128
    NB = EXT // 256
    PRE = EXT - OWN
    assert OWN % 512 == 0 and EXT % 512 == 0 and NB <= 64

    def din(name, shape, dt=F32):
        return nc.dram_tensor(name, shape, dt, kind="ExternalInput").ap()

    x = din("x", [EXT, D])
    w_in = din("w_in", [D, PROJ])
    w_bg = din("w_bg", [1024, D])
    w_bm = din("w_bm", [1024, D])
    w_o = din("w_o", [D, D])
    c_ng = din("c_ng", [128, NCH])
    c_wga = din("c_wga", [17, 512])
    c_gout = din("c_gout", [128, 1024])
    c_qg = din("c_qg", [128, 2])
    c_bval = din("c_bval", [128, 64])
    c_id = din("c_id", [128, 128])
    c_u2 = din("c_u2", [128, 128])
    c_msk = din("c_msk", [128, 128])
    c_tri = din("c_tri", [128, 128])
    c_jrow = din("c_jrow", [128, 64])
    c_sc = din("c_sc", [128, 16])
    c_negt = din("c_negt", [128, 8 * NTO])
    y = nc.dram_tensor("y", [OWN, D], F32, kind="ExternalOutput").ap()

    import os as _os0
    _dbg = _os0.environ.get("KDEBUG", "0") == "1"

    def dscr(name, shape, dt=BF16):
        if _dbg:
            return Buf(nc.dram_tensor(name, shape, dt, kind="ExternalOutput").ap(), name)
        return Buf(nc.dram_tensor(name, shape, dt).ap(), name)

    HT = dscr("s_ht", [NCH, 128, EXT])
    KT = dscr("s_kt", [8, 128, EXT])
    VV = dscr("s_v", [EXT, 1024])
    GKV = dscr("s_gkv", [EXT, 1536])
    GKT = dscr("s_gkt", [4, 128, EXT])
    LRT = dscr("s_lrt", [16, EXT], F32)
    GQT = dscr("s_gqt", [4, 128, OWN])
    MQT = dscr("s_mqt", [8, 128, OWN])
    SGT = dscr("s_sgt", [32, 128, OWN])
    GS = dscr("s_gs", [OWN, 2048])
    OT = dscr("s_ot", [16, 128, OWN])
    MT = dscr("s_mt", [16, 128, OWN])
    dbg_outs = {}

    def csb(name, shape, dt):
        return Buf(nc.alloc_sbuf_tensor(name, shape, dt), name)
    uniq = [0]

    def sb(name, shape, dt):
        uniq[0] += 1
        return AR.alloc("%s_%d" % (name, uniq[0]), shape, dt)

    def ps(name, shape, dt=F32):
        return Buf(nc.alloc_psum_tensor(name, shape, dt), name)

    BIG0 = ps("big0", [128, 1024])
    BIG1 = ps("big1", [128, 1024])
    PA0 = ps("pa0", [128, 512])
    PA1 = ps("pa1", [128, 512])
    PTB = [ps("ptb0", [128, 1024], BF16), ps("ptb1", [128, 1024], BF16)]
    PTk = [PTB[0].k, PTB[1].k]

    class Bank:
        def __init__(self, ap, k):
            self.ap = ap
            self.k = k
    b0a, b0b, b1a, b1b = Tok("b0a"), Tok("b0b"), Tok("b1a"), Tok("b1b")
    banks = [Bank(PA0[:, :], PA0.k), Bank(PA1[:, :], PA1.k),
             Bank(BIG0[:, 0:512], b0a), Bank(BIG0[:, 512:1024], b0b),
             Bank(BIG1[:, 0:512], b1a), Bank(BIG1[:, 512:1024], b1b)]

    def const(name, src, shape, dt=F32, q="sp"):
        b = csb(name, shape, dt)
        P.dma(q, b[:], src, writes=[b.k])
        return b
    NG = const("ng", c_ng, [128, NCH])
    WGA = const("wga", c_wga, [17, 512])
    GOUT = const("gout", c_gout, [128, 1024])
    QG = const("qg", c_qg, [128, 2])
    BVAL = const("bval", c_bval, [128, 64])
    IDF = const("idf", c_id, [128, 128])
    U2 = const("u2", c_u2, [128, 128])
    MSK = const("msk", c_msk, [128, 128])
    TRIF = const("trif", c_tri, [128, 128])
    JROW = const("jrow", c_jrow, [128, 64])
    SC = const("sc", c_sc, [128, 16])
    NEGT = const("negt", c_negt, [128, 8 * NTO])
    IDB = csb("idb", [128, 128], BF16)
    TRIB = csb("trib", [128, 128], BF16)
    ONESB = csb("onesb", [128, 128], BF16)
    KM = csb("km", [128, 8, 64], F32)
    KMB = csb("kmb", [128, 8, 64], BF16)
    AR = Arena(nc, 47000)
    P.op("dve", lambda e: e.tensor_copy(out=IDB[:], in_=IDF[:]), reads=[IDF.k], writes=[IDB.k])
    P.op("dve", lambda e: e.tensor_copy(out=TRIB[:], in_=TRIF[:]), reads=[TRIF.k], writes=[TRIB.k])
    P.op("pool", lambda e: e.memset(ONESB[:], 1.0), writes=[ONESB.k])

    R = {}

    def alloc_gemm():
        P.barrier()
        AR.reset()
        R["WBIG"] = sb("wbig", [128, NCH, 2560], BF16)
        R["WSTG"] = [sb("wstg%d" % i, [128, 2560], F32) for i in range(3)]
        R["HTG"] = [sb("htg%d" % i, [128, NCH, 512], BF16) for i in range(2)]
    wstg_n = [0]

    def load_w(src, col_list, dst=None, nch=NCH, scale=True):
        dst = dst or R["WBIG"]
        WSTG = R["WSTG"]
        tot = sum(n for _, n in col_list)
        first = True
        for c in range(nch):
            st = WSTG[wstg_n[0] % 3]
            wstg_n[0] += 1
            off = 0
            for (c0, n) in col_list:
                P.dma("sp", st[:, off:off + n], src[c * 128:(c + 1) * 128, c0:c0 + n],
                      **({"writes": [st.k]} if off == 0 else {"wadd": [st.k]}))
                off += n
            ceng = ("pool", "act", "dve")[c % 3]
            if scale:
                if ceng == "act":
                    fn = (lambda e, st=st, c=c: e.activation(out=dst[:, c, 0:tot], in_=st[:, 0:tot], func=AF.Copy,
                                                             scale=NG[:, c:c + 1]))
                else:
                    fn = (lambda e, st=st, c=c: e.tensor_scalar(out=dst[:, c, 0:tot], in0=st[:, 0:tot],
                                                                 scalar1=NG[:, c:c + 1], scalar2=None, op0=ALU.mult))
                rd = [st.k, NG.k]
            else:
                if ceng == "act":
                    fn = (lambda e, st=st, c=c: e.copy(out=dst[:, c, 0:tot], in_=st[:, 0:tot]))
                else:
                    fn = (lambda e, st=st, c=c: e.tensor_copy(out=dst[:, c, 0:tot], in_=st[:, 0:tot]))
                rd = [st.k]
            if first:
                P.op(ceng, fn, reads=rd, writes=[dst.k])
                first = False
            else:
                P.op(ceng, fn, reads=rd, wadd=[dst.k])

    cp_n = [0]

    def evac_copy(out_ap, in_ap, reads, writes=(), wadd=()):
        cp_n[0] += 1
        if cp_n[0] % 2 == 0:
            P.op("act", lambda e: e.copy(out=out_ap, in_=in_ap), reads=reads, writes=writes, wadd=wadd)
        else:
            P.op("dve", lambda e: e.tensor_copy(out=out_ap, in_=in_ap), reads=reads, writes=writes, wadd=wadd)

    if stop == 0:
        return done()
    XT = [sb("xt%d" % i, [128, D], F32) for i in range(4)]
    JUNK = sb("junk", [128, D], BF16)
    HB = [sb("hb%d" % i, [128, D], BF16) for i in range(2)]
    SSQ = [sb("ssq%d" % i, [128, 1], F32) for i in range(2)]
    HTT = [sb("htt%d" % i, [128, NCH, 128], BF16) for i in range(2)]
    def ph1_a(i):
        xt, hb, ssq = XT[i % 4], HB[i % 2], SSQ[i % 2]
        if i + 3 < NT:
            xn = XT[(i + 3) % 4]
            P.dma("sp", xn[:], x[(i + 3) * 128:(i + 4) * 128, :], writes=[xn.k])
        P.op("act", lambda e: e.activation(out=JUNK[:], in_=xt[:], func=AF.Square, accum_out=ssq[:]),
             reads=[xt.k], writes=[JUNK.k, ssq.k])
        P.op("dve", lambda e: e.tensor_scalar(out=ssq[:], in0=ssq[:], scalar1=1.0 / D, scalar2=EPS,
                                              op0=ALU.mult, op1=ALU.add), reads=[ssq.k], writes=[ssq.k])
        P.op("act", lambda e: e.activation(out=ssq[:], in_=ssq[:], func=AF.Sqrt), reads=[ssq.k], writes=[ssq.k])
        P.op("dve", lambda e: e.reciprocal(out=ssq[:], in_=ssq[:]), reads=[ssq.k], writes=[ssq.k])
        P.op("dve", lambda e: e.tensor_scalar(out=hb[:], in0=xt[:], scalar1=ssq[:, 0:1], scalar2=None, op0=ALU.mult),
             reads=[xt.k, ssq.k], writes=[hb.k])

    def ph1_b(i):
        hb, htt = HB[i % 2], HTT[i % 2]
        for g4 in range(4):
            half = g4 % 2
            for j in range(4):
                c = g4 * 4 + j
                P.op("pe", lambda e, c=c, half=half, j=j: e.transpose(
                    out=PTB[half][:, j * 128:(j + 1) * 128], in_=hb[:, c * 128:(c + 1) * 128],
                    identity=IDB[:]), reads=[hb.k, IDB.k],
                    **({"writes": [PTk[half]]} if j == 0 else {"wadd": [PTk[half]]}))
            evac_copy(htt[:, g4 * 4:(g4 + 1) * 4, :], PTB[half][:, 0:512].rearrange("p (a b) -> p a b", b=128),
                      [PTk[half]], **({"writes": [htt.k]} if g4 == 0 else {"wadd": [htt.k]}))
        for c4 in range(4):
            P.dma("pool", HT[c4 * 4:(c4 + 1) * 4, :, i * 128:(i + 1) * 128].rearrange("c p t -> p c t"),
                  htt[:, c4 * 4:(c4 + 1) * 4, :], reads=[htt.k], wadd=[HT.k], st=htt.k)
    for i0 in range(min(3, NT)):
        P.dma("sp", XT[i0][:], x[i0 * 128:(i0 + 1) * 128, :], writes=[XT[i0].k])
    ph1_a(0)
    for i in range(NT):
        if i + 1 < NT:
            ph1_a(i + 1)
        ph1_b(i)

    if stop == 1:
        return done()
    bank_n = [0]

    def next_bank():
        b = banks[bank_n[0] % len(banks)]
        bank_n[0] += 1
        return b

    def load_htg(tok0, gi):
        htg = R["HTG"][gi % 2]
        P.dma("sp", htg[:], HT[:, :, tok0:tok0 + 512].rearrange("c p t -> p c t"), reads=[HT.k], writes=[htg.k])
        return htg

    def pass_fm(tok0, ntok, jobs):
        pend = None
        WBIG = R["WBIG"]
        ng_ = ntok // 512
        nxt = load_htg(tok0, 0)
        for g in range(ng_):
            htg = nxt
            if g + 1 < ng_:
                nxt = load_htg(tok0 + (g + 1) * 512, g + 1)
            for (woff, M, epi) in jobs:
                b = next_bank()
                for c in range(NCH):
                    P.op("pe", lambda e, b=b, c=c, htg=htg, woff=woff, M=M: e.matmul(
                        b.ap[0:M, :], WBIG[:, c, woff:woff + M], htg[:, c, :], start=(c == 0), stop=(c == NCH - 1)),
                        reads=[WBIG.k, htg.k], **({"writes": [b.k]} if c == 0 else {"wadd": [b.k]}))
                if pend is not None:
                    pend()
                pend = epi(g, b)
        if pend is not None:
            pend()

    def pass_tm(tok0, ntok, jobs):
        WBIG = R["WBIG"]
        ng_ = ntok // 512
        nxt = load_htg(tok0, 0)
        for g in range(ng_):
            htg = nxt
            if g + 1 < ng_:
                nxt = load_htg(tok0 + (g + 1) * 512, g + 1)
            for t4 in range(4):
                for (woff, N, epi) in jobs:
                    b = next_bank()
                    for c in range(NCH):
                        P.op("pe", lambda e, b=b, c=c, htg=htg, woff=woff, N=N, t4=t4: e.matmul(
                            b.ap[:, 0:N], htg[:, c, t4 * 128:(t4 + 1) * 128], WBIG[:, c, woff:woff + N],
                            start=(c == 0), stop=(c == NCH - 1)),
                            reads=[WBIG.k, htg.k], **({"writes": [b.k]} if c == 0 else {"wadd": [b.k]}))
                    epi(g * 4 + t4, b)

    alloc_gemm()
    FST = [sb("fst%d" % i, [128, 512], BF16) for i in range(4)]
    fst_n = [0]
    SQB = [sb("sqb%d" % i, [128, 512], BF16) for i in range(2)]
    RINV = [sb("rinv%d" % i, [128, 512], F32) for i in range(2)]
    KNF = [sb("knf%d" % i, [128, 512], F32) for i in range(2)]
    LRS = [sb("lrs%d" % i, [16, 512], F32) for i in range(2)]
    TST = [sb("tst%d" % i, [128, 2560], BF16) for i in range(2)]
    qk_n = [0]

    def epi_store_fm(dst, chunk, ntok_total, func=None):
        def epi(g, b):
            st = FST[fst_n[0] % 4]
            fst_n[0] += 1
            if func is None:
                evac_copy(st[:], b.ap, [b.k], writes=[st.k])
            else:
                P.op("act", lambda e: e.activation(out=st[:], in_=b.ap, func=func), reads=[b.k], writes=[st.k])
            P.dma("pool", dst[chunk, :, g * 512:(g + 1) * 512], st[:], reads=[st.k], wadd=[dst.k], st=st.k)
            return None
        return epi

    def epi_qknorm(dst, head, gcol, want_mean):
        def epi(g, b):
            i = qk_n[0] % 2
            qk_n[0] += 1
            sqb, rinv, knf = SQB[i], RINV[i], KNF[i]
            P.op("act", lambda e: e.activation(out=sqb[:], in_=b.ap, func=AF.Square), reads=[b.k], writes=[sqb.k])

            def deferred():
                b2 = next_bank()
                P.op("pe", lambda e: e.matmul(b2.ap, ONESB[:], sqb[:], start=True, stop=True),
                     reads=[ONESB.k, sqb.k], writes=[b2.k])
                P.op("dve", lambda e: e.tensor_scalar(out=rinv[:], in0=b2.ap, scalar1=1.0 / 128, scalar2=EPS,
                                                      op0=ALU.mult, op1=ALU.add), reads=[b2.k], writes=[rinv.k])
                P.op("act", lambda e: e.activation(out=rinv[:], in_=rinv[:], func=AF.Sqrt), reads=[rinv.k], writes=[rinv.k])
                P.op("dve", lambda e: e.reciprocal(out=rinv[:], in_=rinv[:]), reads=[rinv.k], writes=[rinv.k])
                st = FST[fst_n[0] % 4]
                fst_n[0] += 1
                if want_mean:
                    P.op("dve", lambda e: e.scalar_tensor_tensor(out=knf[:], in0=b.ap, scalar=QG[:, gcol:gcol + 1],
                                                                  in1=rinv[:], op0=ALU.mult, op1=ALU.mult),
                         reads=[b.k, QG.k, rinv.k], writes=[knf.k])
                    P.op("dve", lambda e: e.tensor_reduce(out=KM[:, head, 2 * g:2 * g + 2],
                                                          in_=knf[:].rearrange("p (a b) -> p a b", b=256),
                                                          axis=AX.X, op=ALU.add), reads=[knf.k], wadd=[KM.k])
                    P.op("pool", lambda e: e.tensor_copy(out=st[:], in_=knf[:]), reads=[knf.k], writes=[st.k])
                else:
                    P.op("dve", lambda e: e.scalar_tensor_tensor(out=st[:], in0=b.ap, scalar=QG[:, gcol:gcol + 1],
                                                                  in1=rinv[:], op0=ALU.mult, op1=ALU.mult),
                         reads=[b.k, QG.k, rinv.k], writes=[st.k])
                P.dma("pool", dst[head, :, g * 512:(g + 1) * 512], st[:], reads=[st.k], wadd=[dst.k], st=st.k)
            return deferred
        return epi

    load_w(w_in, [(C_MK, 1024), (C_GK, 512), (C_LR, 16)])
    lr_n = [0]

    def epi_lr(g, b):
        st = LRS[lr_n[0] % 2]
        lr_n[0] += 1
        evac_copy(st[:], b.ap[0:16, :], [b.k], writes=[st.k])
        P.dma("pool", LRT[:, g * 512:(g + 1) * 512], st[:], reads=[st.k], wadd=[LRT.k], st=st.k)
        return None
    jobsA = [(h * 128, 128, epi_qknorm(KT, h, 1, True)) for h in range(8)]
    jobsA += [(1024 + h * 128, 128, epi_store_fm(GKT, h, EXT)) for h in range(4)]
    jobsA += [(1536, 16, epi_lr)]
    pass_fm(0, EXT, jobsA)
    if stop == 2:
        return done()
    P.op("dve", lambda e: e.tensor_scalar(out=KMB[:], in0=KM[:], scalar1=1.0 / 256, scalar2=None, op0=ALU.mult),
         reads=[KM.k], writes=[KMB.k])

    load_w(w_in, [(C_MV, 1024), (C_GK, 512), (C_GV, 1024)])

    def mk_epi_tm(col0, N, last, dsts):
        def epi(ti, b):
            st = TST[ti % 2]
            evac_copy(st[:, col0:col0 + N], b.ap[:, 0:N], [b.k], **({"writes": [st.k]} if col0 == 0 else {"wadd": [st.k]}))
            if last:
                for (dst, s0, n) in dsts:
                    P.dma("pool", dst[ti * 128:(ti + 1) * 128, :], st[:, s0:s0 + n], reads=[st.k], wadd=[dst.k], st=st.k)
        return epi
    dstsB = [(VV, 0, 1024), (GKV, 1024, 1536)]
    jobsB = [(i * 512, 512, mk_epi_tm(i * 512, 512, i == 4, dstsB)) for i in range(5)]
    pass_tm(0, EXT, jobsB)

    if stop == 3:
        return done()
    load_w(w_in, [(C_MQ, 1024), (C_GQ, 512)])
    jobsD = [(h * 128, 128, epi_qknorm(MQT, h, 0, False)) for h in range(8)]
    jobsD += [(1024 + h * 128, 128, epi_store_fm(GQT, h, OWN)) for h in range(4)]
    pass_fm(PRE, OWN, jobsD)
    for gi, c0 in enumerate((C_GA, C_GB)):
        load_w(w_in, [(c0, 2048)])
        pass_fm(PRE, OWN, [(n * 128, 128, epi_store_fm(SGT, gi * 16 + n, OWN, func=AF.Sigmoid)) for n in range(16)])
    load_w(w_in, [(C_GS, 1024), (C_MS, 1024)])

    def mk_epi_silu(col0, last):
        def epi(ti, b):
            st = TST[ti % 2]
            P.op("act", lambda e: e.activation(out=st[:, col0:col0 + 512], in_=b.ap, func=AF.Silu), reads=[b.k],
                 **({"writes": [st.k]} if col0 == 0 else {"wadd": [st.k]}))
            if last:
                P.dma("pool", GS[ti * 128:(ti + 1) * 128, :], st[:, 0:2048], reads=[st.k], wadd=[GS.k], st=st.k)
        return epi
    pass_tm(PRE, OWN, [(i * 512, 512, mk_epi_silu(i * 512, i == 3)) for i in range(4)])

    if stop == 4:
        return done()
    P.barrier()
    AR.reset()
    KTS = sb("kts", [128, EXT], BF16)
    VP = sb("vp", [128, NT, 129], BF16)
    QS = sb("qs", [128, OWN], BF16)
    GSH = sb("gsh", [128, NTO, 128], BF16)
    SELB = [(sb("gate", [128, 64], F32), sb("t8", [128, 8], F32), sb("msel", [128, 64], F32),
             sb("dex", [128, 64], F32), sb("dm", [128, 64], F32)) for _ in range(2)]
    PTS = [sb("pts%d" % i, [128, 512], BF16) for i in range(4)]
    ACC = sb("acc", [128, 129], F32)
    ACC2 = sb("acc2", [128, 2, 129], F32)
    TMPB = [sb("tmpb%d" % i, [128, 2, 129], F32) for i in range(3)]
    RDEN = sb("rden", [128, 1], F32)
    OB = [sb("ob%d" % i, [128, 128], BF16) for i in range(2)]
    OST = [sb("ost%d" % i, [128, 512], BF16) for i in range(2)]
    SBANKS = [banks[0], banks[1], banks[2]]
    OBANKS = [banks[4], banks[5]]
    GBANK = banks[3]
    sct = [0, 0, 0, 0]
    pend_fin = [None]
    for h in range(8):
        slope = float(2.0 ** (-(h + 1)))
        P.dma("sp", KTS[:], KT[h, :, :], reads=[KT.k], writes=[KTS.k])
        vsrc = VV[:, h * 128:(h + 1) * 128].rearrange("(t p) d -> p t d", p=128)
        for t0_ in range(0, NT, 16):
            t1_ = min(NT, t0_ + 16)
            P.dma("sp", VP[:, t0_:t1_, 0:128], vsrc[:, t0_:t1_, :], reads=[VV.k],
                  **({"writes": [VP.k]} if t0_ == 0 else {"wadd": [VP.k]}))
        P.op("pool", lambda e: e.memset(VP[:, :, 128:129], 1.0), wadd=[VP.k])
        vpv = VP[:].rearrange("p (a two) d -> p a two d", two=2)
        for par in range(2):
            P.op("dve", lambda e, par=par, h=h: e.tensor_scalar(
                out=vpv[:, :, par, :], in0=vpv[:, :, par, :], scalar1=SC[:, 2 * h + par:2 * h + par + 1],
                scalar2=None, op0=ALU.mult), reads=[VP.k, SC.k], writes=[VP.k])
        P.dma("sp", QS[:], MQT[h, :, :], reads=[MQT.k], writes=[QS.k])
        gsrc = GS[:, 1024 + h * 128:1024 + (h + 1) * 128].rearrange("(t p) d -> p t d", p=128)
        for t0_ in range(0, NTO, 16):
            t1_ = min(NTO, t0_ + 16)
            P.dma("sp", GSH[:, t0_:t1_, :], gsrc[:, t0_:t1_, :], reads=[GS.k],
                  **({"writes": [GSH.k]} if t0_ == 0 else {"wadd": [GSH.k]}))
        def prologue(qt, bufs, h=h, slope=slope):
            G, T8, MSEL, DEX, DM = bufs
            eq = PRE // 128 + qt
            ob_ = eq // 2
            nblk = ob_ + 1
            qsl = QS[:, qt * 128:(qt + 1) * 128]
            P.op("pool", lambda e: e.memset(G[:], NEG), writes=[G.k])
            P.op("pool", lambda e: e.memset(MSEL[:], 1.0), writes=[MSEL.k])
            if ob_ > 0:
                P.op("pe", lambda e: e.matmul(GBANK.ap[:, 0:ob_], qsl, KMB[:, h, 0:ob_], start=True, stop=True),
                     reads=[QS.k, KMB.k], writes=[GBANK.k])
                P.op("dve", lambda e: e.tensor_tensor(out=G[:, 0:ob_], in0=GBANK.ap[:, 0:ob_],
                                                      in1=BVAL[:, 0:ob_], op=ALU.add),
                     reads=[GBANK.k, BVAL.k], wadd=[G.k])
                P.op("dve", lambda e: e.max(out=T8[:], in_=G[:]), reads=[G.k], writes=[T8.k])
                P.op("dve", lambda e: e.tensor_scalar_max(out=T8[:, 2:3], in0=T8[:, 2:3], scalar1=-1.0e29),
                     reads=[T8.k], writes=[T8.k])
                P.op("dve", lambda e: e.tensor_scalar(out=MSEL[:, 0:ob_], in0=G[:, 0:ob_],
                                                      scalar1=T8[:, 2:3], scalar2=None, op0=ALU.is_ge),
                     reads=[G.k, T8.k], wadd=[MSEL.k])
            P.op("act", lambda e: e.activation(
                out=DEX[:, 0:nblk], in_=JROW[:, 0:nblk], func=AF.Exp, scale=slope,
                bias=NEGT[:, h * NTO + qt:h * NTO + qt + 1]), reads=[JROW.k, NEGT.k], writes=[DEX.k])
            P.op("dve", lambda e: e.tensor_tensor(out=DM[:, 0:nblk], in0=DEX[:, 0:nblk],
                                                  in1=MSEL[:, 0:nblk], op=ALU.mult),
                 reads=[DEX.k, MSEL.k], writes=[DM.k])

        prologue(0, SELB[0])
        for qt in range(NTO):
            eq = PRE // 128 + qt
            ob_ = eq // 2
            qsl = QS[:, qt * 128:(qt + 1) * 128]
            DM = SELB[qt % 2][4]
            last_kt = 2 * ob_ + (1 if eq % 2 == 1 else 0)
            kts = list(range(last_kt + 1))
            groups = [kts[g0:g0 + 4] for g0 in range(0, len(kts), 4)]

            def emit_qk(gi, groups=groups, qsl=qsl):
                grp = groups[gi]
                sbk = SBANKS[sct[0] % 3]
                sct[0] += 1
                for i_, kt in enumerate(grp):
                    P.op("pe", lambda e, sbk=sbk, i_=i_, kt=kt: e.matmul(
                        sbk.ap[:, i_ * 128:(i_ + 1) * 128], KTS[:, kt * 128:(kt + 1) * 128], qsl, start=True, stop=True),
                        reads=[KTS.k, QS.k], **({"writes": [sbk.k]} if i_ == 0 else {"wadd": [sbk.k]}))
                return sbk
            P.op("pool", lambda e: e.memset(ACC2[:], 0.0), writes=[ACC2.k])
            sbq = [emit_qk(0)]
            if len(groups) > 1:
                sbq.append(emit_qk(1))
            for gi, grp in enumerate(groups):
                sbk = sbq.pop(0)
                if gi + 2 < len(groups):
                    sbq.append(emit_qk(gi + 2))
                if gi == min(1, len(groups) - 1) and qt + 1 < NTO:
                    prologue(qt + 1, SELB[(qt + 1) % 2])
                if gi == min(2, len(groups) - 1) and pend_fin[0] is not None:
                    pend_fin[0]()
                    pend_fin[0] = None
                pts = PTS[sct[1] % 4]
                sct[1] += 1
                n = len(grp)
                P.op("act", lambda e, pts=pts, sbk=sbk, n=n: e.activation(
                    out=pts[:, 0:n * 128], in_=sbk.ap[:, 0:n * 128], func=AF.Exp, scale=float(128 ** -0.5)),
                    reads=[sbk.k], writes=[pts.k])
                if last_kt in grp:
                    i_ = grp.index(last_kt)
                    P.op("pool", lambda e, pts=pts, i_=i_: e.tensor_tensor(
                        out=pts[:, i_ * 128:(i_ + 1) * 128], in0=pts[:, i_ * 128:(i_ + 1) * 128], in1=TRIB[:],
                        op=ALU.mult), reads=[pts.k, TRIB.k], writes=[pts.k])
                blks = sorted(set(kt // 2 for kt in grp))
                obk = OBANKS[sct[2] % 2]
                sct[2] += 1
                for bi, j in enumerate(blks):
                    jk = [kt for kt in grp if kt // 2 == j]
                    for ii, kt in enumerate(jk):
                        i_ = grp.index(kt)
                        P.op("pe", lambda e, obk=obk, pts=pts, i_=i_, kt=kt, ii=ii, jk=jk, bi=bi: e.matmul(
                            obk.ap[:, bi * 256:bi * 256 + 129], pts[:, i_ * 128:(i_ + 1) * 128], VP[:, kt, :],
                            start=(ii == 0), stop=(ii == len(jk) - 1)),
                            reads=[pts.k, VP.k],
                            **({"writes": [obk.k]} if (ii == 0 and bi == 0) else {"wadd": [obk.k]}))
                nb_ = len(blks)
                j0 = blks[0]
                tmp = TMPB[sct[3] % 3]
                sct[3] += 1
                P.op("dve", lambda e, obk=obk, tmp=tmp, nb_=nb_, j0=j0, DM=DM: e.tensor_tensor(
                    out=tmp[:, 0:nb_, :],
                    in0=obk.ap[:, 0:512].rearrange("p (a b) -> p a b", b=256)[:, 0:nb_, 0:129],
                    in1=DM[:, j0:j0 + nb_].unsqueeze(2).to_broadcast([128, nb_, 129]), op=ALU.mult),
                    reads=[obk.k, DM.k], writes=[tmp.k])
                P.op("pool", lambda e, tmp=tmp, nb_=nb_: e.tensor_tensor(
                    out=ACC2[:, 0:nb_, :], in0=ACC2[:, 0:nb_, :], in1=tmp[:, 0:nb_, :], op=ALU.add),
                    reads=[tmp.k, ACC2.k], writes=[ACC2.k])
            P.op("dve", lambda e: e.tensor_tensor(out=ACC[:], in0=ACC2[:, 0, :], in1=ACC2[:, 1, :], op=ALU.add),
                 reads=[ACC2.k], writes=[ACC.k])
            ob16 = OB[qt % 2]
            P.op("dve", lambda e: e.reciprocal(out=RDEN[:], in_=ACC[:, 128:129]), reads=[ACC.k], writes=[RDEN.k])
            P.op("dve", lambda e, ob16=ob16, qt=qt: e.scalar_tensor_tensor(
                out=ob16[:], in0=ACC[:, 0:128], scalar=RDEN[:, 0:1], in1=GSH[:, qt, :], op0=ALU.mult, op1=ALU.mult),
                reads=[ACC.k, RDEN.k, GSH.k], writes=[ob16.k])

            def fin_pe(ob16=ob16, qt=qt, h=h):
                half = (qt // 4) % 2
                j4 = qt % 4
                P.op("pe", lambda e: e.transpose(
                    out=PTB[half][:, j4 * 128:(j4 + 1) * 128], in_=ob16[:], identity=IDB[:]),
                    reads=[ob16.k, IDB.k], **({"writes": [PTk[half]]} if j4 == 0 else {"wadd": [PTk[half]]}))
                if j4 == 3:
                    ost = OST[(qt // 4) % 2]
                    evac_copy(ost[:], PTB[half][:, 0:512], [PTk[half]], writes=[ost.k])
                    P.dma("sp", OT[8 + h, :, (qt - 3) * 128:(qt + 1) * 128], ost[:], reads=[ost.k], wadd=[OT.k],
                          st=ost.k)
            pend_fin[0] = fin_pe
        if pend_fin[0] is not None:
            pend_fin[0]()
            pend_fin[0] = None

    if stop == 5:
        return done()
    P.barrier()
    AR.reset()
    JUNK2 = sb("junk2", [128, 256], BF16)
    GKVT = [sb("gkvt%d" % i, [128, 1536], BF16) for i in range(2)]
    LRA = [sb("lra%d" % i, [17, 128], F32) for i in range(2)]
    KQT = [sb("kqt%d" % i, [128, 8, 128], BF16) for i in range(2)]
    GSL = [sb("gsl%d" % i, [128, 1024], BF16) for i in range(2)]
    E1 = sb("e1", [128, 512], F32)
    SP_ = sb("sp", [128, 512], F32)
    EKTM = sb("ektm", [128, 512], F32)
    KTT = sb("ktt", [128, 512], BF16)
    EQT = sb("eqt", [128, 512], F32)
    EKT = sb("ekt", [128, 512], F32)
    KTF = sb("ktf", [128, 4, 128], BF16)
    QZ = sb("qz", [128, 4, 192], BF16)
    S = sb("S", [128, 1024], F32)
    TS_ = sb("Ts", [128, 1024], F32)
    SBF = [sb("sbf%d" % i, [128, 1024], BF16) for i in range(2)]
    ATB = sb("atb", [128, 512], BF16)
    SSG = sb("ssg", [128, 4], F32)
    GG = sb("gg", [128, 1024], F32)
    OG = sb("og", [128, 1024], BF16)
    OGT = [sb("ogt%d" % i, [128, 8, 128], BF16) for i in range(2)]
    P.op("pool", lambda e: e.memset(S[:], 0.0), writes=[S.k])
    P.op("pool", lambda e: e.memset(SBF[0][:], 0.0), writes=[SBF[0].k])
    P.op("pool", lambda e: e.memset(QZ[:], 0.0), writes=[QZ.k])
    for i in range(2):
        P.op("pool", lambda e, i=i: e.memset(LRA[i][:], 1.0), writes=[LRA[i].k])
    KTT2 = [KTT, sb("ktt_b", [128, 512], BF16)]
    EQT2 = [EQT, sb("eqt_b", [128, 512], F32)]
    KTF2 = [KTF, sb("ktf_b", [128, 4, 128], BF16)]
    QZ2 = [QZ, sb("qz_b", [128, 4, 192], BF16)]
    ATB2 = [ATB, sb("atb_b", [128, 512], BF16)]
    P.op("pool", lambda e: e.memset(QZ2[1][:], 0.0), writes=[QZ2[1].k])

    def gla_prep(ti):
        own = ti >= PRE // 128
        to = ti - PRE // 128
        gkv, lra = GKVT[ti % 2], LRA[ti % 2]
        KTTc, EQTc, KTFc, QZc, ATBc = KTT2[ti % 2], EQT2[ti % 2], KTF2[ti % 2], QZ2[ti % 2], ATB2[ti % 2]
        P.dma("sp", gkv[:], GKV[ti * 128:(ti + 1) * 128, :], reads=[GKV.k], writes=[gkv.k])
        P.dma("sp", lra[0:16, :], LRT[:, ti * 128:(ti + 1) * 128], reads=[LRT.k], wadd=[lra.k])
        if own:
            kqt, gsl = KQT[ti % 2], GSL[ti % 2]
            P.dma("sp", kqt[:, 0:4, :], GKT[:, :, ti * 128:(ti + 1) * 128].rearrange("c p t -> p c t"),
                  reads=[GKT.k], writes=[kqt.k])
            P.dma("sp", kqt[:, 4:8, :], GQT[:, :, to * 128:(to + 1) * 128].rearrange("c p t -> p c t"),
                  reads=[GQT.k], wadd=[kqt.k])
            P.dma("sp", gsl[:], GS[to * 128:(to + 1) * 128, 0:1024], reads=[GS.k], writes=[gsl.k])
        zb = banks[0]
        P.op("pe", lambda e: e.matmul(zb.ap, lra[:], WGA[:], start=True, stop=True),
             reads=[lra.k, WGA.k], writes=[zb.k])
        P.op("act", lambda e: e.activation(out=E1[:], in_=zb.ap, func=AF.Exp, scale=-1.0), reads=[zb.k], writes=[E1.k])
        P.op("act", lambda e: e.activation(out=SP_[:], in_=E1[:], func=AF.Ln, bias=1.0), reads=[E1.k], writes=[SP_.k])
        cb = banks[1]
        P.op("pe", lambda e: e.matmul(cb.ap, U2[:], SP_[:], start=True, stop=True), reads=[U2.k, SP_.k], writes=[cb.k])
        tb = banks[0]
        for hh in range(4):
            P.op("pe", lambda e, hh=hh: e.matmul(tb.ap[:, hh * 128:(hh + 1) * 128], SP_[:, hh * 128:(hh + 1) * 128], U2[:],
                                                 start=True, stop=True), reads=[SP_.k, U2.k],
                 **({"writes": [tb.k]} if hh == 0 else {"wadd": [tb.k]}))
        P.op("act", lambda e: e.activation(out=EKTM[:], in_=cb.ap, func=AF.Exp), reads=[cb.k], writes=[EKTM.k])
        P.op("dve", lambda e: e.tensor_tensor(out=KTTc[:], in0=gkv[:, 0:512], in1=EKTM[:], op=ALU.mult),
             reads=[gkv.k, EKTM.k], writes=[KTTc.k])
        P.op("act", lambda e: e.activation(out=EQTc[:], in_=tb.ap, func=AF.Exp, scale=-1.0), reads=[tb.k], writes=[EQTc.k])
        if own:
            P.op("act", lambda e: e.activation(out=EKT[:], in_=tb.ap, func=AF.Exp), reads=[tb.k], writes=[EKT.k])
            P.op("dve", lambda e: e.tensor_tensor(
                out=KTFc[:], in0=kqt[:, 0:4, :], in1=EKT[:].rearrange("p (a b) -> p a b", b=128), op=ALU.mult),
                reads=[kqt.k, EKT.k], writes=[KTFc.k])
            for c in range(2):
                P.op("dve", lambda e, c=c: e.scalar_tensor_tensor(
                    out=QZc[:, :, c * 128:c * 128 + 64], in0=kqt[:, 4:8, c * 64:(c + 1) * 64], scalar=float(128 ** -0.5),
                    in1=EQTc[:].rearrange("p (a b) -> p a b", b=128)[:, :, c * 64:(c + 1) * 64],
                    op0=ALU.mult, op1=ALU.mult), reads=[kqt.k, EQTc.k], wadd=[QZc.k])
            ab = banks[1]
            for hh in range(4):
                P.op("pe", lambda e, hh=hh: e.matmul(
                    ab.ap[:, hh * 128:(hh + 1) * 128].rearrange("p (a b) -> p a b", b=64), KTFc[:, hh, :],
                    QZc[:, hh, :].rearrange("p (a b) -> p a b", b=64)[:, 0:3:2, :], start=True, stop=True),
                    reads=[KTFc.k, QZc.k], **({"writes": [ab.k]} if hh == 0 else {"wadd": [ab.k]}))
            P.op("dve", lambda e: e.tensor_tensor(
                out=ATBc[:].rearrange("p (a b) -> p a b", b=128), in0=ab.ap.rearrange("p (a b) -> p a b", b=128),
                in1=MSK[:].unsqueeze(1).to_broadcast([128, 4, 128]), op=ALU.mult), reads=[ab.k, MSK.k], writes=[ATBc.k])

    def gla_rec(ti):
        own = ti >= PRE // 128
        to = ti - PRE // 128
        gkv = GKVT[ti % 2]
        KTTc, EQTc, QZc, ATBc = KTT2[ti % 2], EQT2[ti % 2], QZ2[ti % 2], ATB2[ti % 2]

        def state_update(c):
            sout = SBF[(c + 1) % 2]
            for hh in range(4):
                P.op("pe", lambda e, hh=hh: e.matmul(
                    BIG1[:, hh * 256:(hh + 1) * 256], KTTc[c * 64:(c + 1) * 64, hh * 128:(hh + 1) * 128],
                    gkv[c * 64:(c + 1) * 64, 512 + hh * 256:512 + (hh + 1) * 256], start=True, stop=True),
                    reads=[KTTc.k, gkv.k], **({"writes": [b1a, b1b]} if hh == 0 else {"wadd": [b1a, b1b]}))
            P.op("dve", lambda e: e.tensor_tensor(out=TS_[:], in0=BIG1[:, :], in1=S[:], op=ALU.add),
                 reads=[b1a, b1b, S.k], writes=[TS_.k])
            for hh in range(4):
                col = hh * 128 + c * 64 + 63
                P.op("act", lambda e, hh=hh, col=col: e.activation(
                    out=S[:, hh * 256:(hh + 1) * 256], in_=TS_[:, hh * 256:(hh + 1) * 256], func=AF.Copy,
                    scale=EQTc[:, col:col + 1]), reads=[TS_.k, EQTc.k], **({"writes": [S.k]} if hh == 0 else {"wadd": [S.k]}))
            P.op("pool", lambda e: e.tensor_copy(out=sout[:], in_=S[:]), reads=[S.k], writes=[sout.k])
        state_update(0)
        if own:
            for hh in range(4):
                P.op("pe", lambda e, hh=hh: e.matmul(
                    BIG0[:, hh * 256:(hh + 1) * 256], ATBc[:, hh * 128:(hh + 1) * 128],
                    gkv[:, 512 + hh * 256:512 + (hh + 1) * 256], start=True, stop=False),
                    reads=[ATBc.k, gkv.k], **({"writes": [b0a, b0b]} if hh == 0 else {"wadd": [b0a, b0b]}))
                P.op("pe", lambda e, hh=hh: e.matmul(
                    BIG0[:, hh * 256:(hh + 1) * 256], QZc[:, hh, 0:128], SBF[0][:, hh * 256:(hh + 1) * 256],
                    start=False, stop=False), reads=[QZc.k, SBF[0].k], wadd=[b0a, b0b])
                P.op("pe", lambda e, hh=hh: e.matmul(
                    BIG0[:, hh * 256:(hh + 1) * 256], QZc[:, hh, 64:192], SBF[1][:, hh * 256:(hh + 1) * 256],
                    start=False, stop=True), reads=[QZc.k, SBF[1].k], wadd=[b0a, b0b])
        state_update(1)
        if own:
            gsl = GSL[ti % 2]
            for hh in range(4):
                P.op("act", lambda e, hh=hh: e.activation(out=JUNK2[:, 0:256], in_=BIG0[:, hh * 256:(hh + 1) * 256],
                                                          func=AF.Square, accum_out=SSG[:, hh:hh + 1]),
                     reads=[b0a, b0b], writes=[JUNK2.k], wadd=[SSG.k])
            P.op("dve", lambda e: e.tensor_scalar(out=SSG[:], in0=SSG[:], scalar1=1.0 / 256, scalar2=EPS,
                                                  op0=ALU.mult, op1=ALU.add), reads=[SSG.k], writes=[SSG.k])
            P.op("act", lambda e: e.activation(out=SSG[:], in_=SSG[:], func=AF.Sqrt), reads=[SSG.k], writes=[SSG.k])
            P.op("dve", lambda e: e.reciprocal(out=SSG[:], in_=SSG[:]), reads=[SSG.k], writes=[SSG.k])
            P.op("pool", lambda e: e.tensor_tensor(out=GG[:], in0=gsl[:], in1=GOUT[:], op=ALU.mult),
                 reads=[gsl.k, GOUT.k], writes=[GG.k])
            for hh in range(4):
                P.op("dve", lambda e, hh=hh: e.scalar_tensor_tensor(
                    out=OG[:, hh * 256:(hh + 1) * 256], in0=BIG0[:, hh * 256:(hh + 1) * 256], scalar=SSG[:, hh:hh + 1],
                    in1=GG[:, hh * 256:(hh + 1) * 256], op0=ALU.mult, op1=ALU.mult),
                    reads=[b0a, b0b, SSG.k, GG.k], **({"writes": [OG.k]} if hh == 0 else {"wadd": [OG.k]}))
            ogt = OGT[ti % 2]
            for half in range(2):
                for j in range(4):
                    c8 = half * 4 + j
                    P.op("pe", lambda e, c8=c8, half=half, j=j: e.transpose(
                        out=PTB[half][:, j * 128:(j + 1) * 128], in_=OG[:, c8 * 128:(c8 + 1) * 128],
                        identity=IDB[:]), reads=[OG.k, IDB.k],
                        **({"writes": [PTk[half]]} if j == 0 else {"wadd": [PTk[half]]}))
                evac_copy(ogt[:, half * 4:(half + 1) * 4, :],
                          PTB[half][:, 0:512].rearrange("p (a b) -> p a b", b=128), [PTk[half]],
                          **({"writes": [ogt.k]} if half == 0 else {"wadd": [ogt.k]}))
            P.dma("pool", OT[0:8, :, to * 128:(to + 1) * 128].rearrange("c p t -> p c t"), ogt[:], reads=[ogt.k],
                  wadd=[OT.k], st=ogt.k)

    gla_prep(0)
    for ti in range(NT):
        if ti + 1 < NT:
            gla_prep(ti + 1)
        gla_rec(ti)
    if stop == 6:
        return done()
    alloc_gemm()
    WZ = R["WBIG"]
    WSTG = R["WSTG"]
    load_w(w_bg, [(0, 2048)], nch=8, scale=False)
    first = True
    for c in range(8):
        st = WSTG[wstg_n[0] % 3]
        wstg_n[0] += 1
        P.dma("sp", st[:, 0:2048], w_bm[c * 128:(c + 1) * 128, :], writes=[st.k])
        P.op("pool", lambda e, st=st, c=c: e.tensor_copy(out=WZ[:, 8 + c, 0:2048], in_=st[:, 0:2048]),
             reads=[st.k], wadd=[WZ.k])
    OTG = R["HTG"]
    SGG = [sb("sgg%d" % i, [128, 32, 512], BF16) for i in range(1)]
    T1 = [sb("t1_%d" % i, [128, 512], BF16) for i in range(2)]
    T2 = [sb("t2_%d" % i, [128, 512], BF16) for i in range(2)]
    MTS = [sb("mts%d" % i, [128, 512], BF16) for i in range(2)]
    for g in range(OWN // 512):
        otg = OTG[g % 2]
        sgg = SGG[0]
        P.dma("sp", otg[:], OT[:, :, g * 512:(g + 1) * 512].rearrange("c p t -> p c t"), reads=[OT.k], writes=[otg.k])
        for c0_ in (0, 16):
            P.dma("sp", sgg[:, c0_:c0_ + 16, :], SGT[c0_:c0_ + 16, :, g * 512:(g + 1) * 512].rearrange("c p t -> p c t"),
                  reads=[SGT.k], **({"writes": [sgg.k]} if c0_ == 0 else {"wadd": [sgg.k]}))
        for n in range(16):
            bg, bm = next_bank(), next_bank()
            for br, bnk in ((0, bg), (1, bm)):
                for c in range(8):
                    P.op("pe", lambda e, bnk=bnk, br=br, c=c, n=n, otg=otg: e.matmul(
                        bnk.ap, WZ[:, br * 8 + c, n * 128:(n + 1) * 128], otg[:, br * 8 + c, :],
                        start=(c == 0), stop=(c == 7)), reads=[WZ.k, otg.k],
                        **({"writes": [bnk.k]} if c == 0 else {"wadd": [bnk.k]}))
            t1, t2, mts = T1[n % 2], T2[n % 2], MTS[n % 2]
            P.op("dve", lambda e, t1=t1, bg=bg, n=n, sgg=sgg: e.tensor_tensor(out=t1[:], in0=bg.ap, in1=sgg[:, n, :], op=ALU.mult),
                 reads=[bg.k, sgg.k], writes=[t1.k])
            P.op("dve", lambda e, t2=t2, bm=bm, n=n, sgg=sgg: e.tensor_tensor(out=t2[:], in0=bm.ap, in1=sgg[:, 16 + n, :], op=ALU.mult),
                 reads=[bm.k, sgg.k], writes=[t2.k])
            P.op("pool", lambda e, t1=t1, t2=t2, mts=mts: e.tensor_tensor(out=mts[:], in0=t1[:], in1=t2[:], op=ALU.add),
                 reads=[t1.k, t2.k], writes=[mts.k])
            P.dma("pool", MT[n, :, g * 512:(g + 1) * 512], mts[:], reads=[mts.k], wadd=[MT.k], st=mts.k)

    if stop == 7:
        return done()
    alloc_gemm()
    WBIG = R["WBIG"]
    load_w(w_o, [(0, 2048)], scale=False)
    XT = [sb("xtz%d" % i, [128, D], F32) for i in range(2)]
    YT = [sb("yt%d" % i, [128, D], F32) for i in range(2)]
    for g in range(OWN // 512):
        mtg = R["HTG"][g % 2]
        P.dma("sp", mtg[:], MT[:, :, g * 512:(g + 1) * 512].rearrange("c p t -> p c t"), reads=[MT.k], writes=[mtg.k])
        for t4 in range(4):
            ti = g * 4 + t4
            xt, yt = XT[ti % 2], YT[ti % 2]
            P.dma("sp", xt[:], x[PRE + ti * 128:PRE + (ti + 1) * 128, :], writes=[xt.k])
            for ng in range(4):
                b = next_bank()
                for c in range(NCH):
                    P.op("pe", lambda e, b=b, c=c, mtg=mtg, t4=t4, ng=ng: e.matmul(
                        b.ap, mtg[:, c, t4 * 128:(t4 + 1) * 128], WBIG[:, c, ng * 512:(ng + 1) * 512],
                        start=(c == 0), stop=(c == NCH - 1)), reads=[mtg.k, WBIG.k],
                        **({"writes": [b.k]} if c == 0 else {"wadd": [b.k]}))
                P.op("dve", lambda e, b=b, yt=yt, xt=xt, ng=ng: e.tensor_tensor(
                    out=yt[:, ng * 512:(ng + 1) * 512], in0=b.ap, in1=xt[:, ng * 512:(ng + 1) * 512], op=ALU.add),
                    reads=[b.k, xt.k], **({"writes": [yt.k]} if ng == 0 else {"wadd": [yt.k]}))
            P.dma("pool", y[ti * 128:(ti + 1) * 128, :], yt[:], reads=[yt.k], st=yt.k)

    P.finish()
    P.emit()
    return nc


def make_consts(EXT, OWN):
    NTO = OWN // 128
    PRE = EXT - OWN
    p = np.arange(128)
    same = (p[:, None] // 64) == (p[None, :] // 64)
    le = p[:, None] <= p[None, :]
    msk = (same & le).astype(np.float32)
    slopes = 2.0 ** (-8.0 * np.arange(1, 9, dtype=np.float64) / 8)
    sc = np.zeros((128, 16), np.float32)
    negt = np.zeros((128, 8 * NTO), np.float32)
    for h in range(8):
        sc[:, 2 * h] = np.exp(slopes[h] * (p - 128.0))
        sc[:, 2 * h + 1] = np.exp(slopes[h] * (p * 1.0))
        for qt in range(NTO):
            negt[:, h * NTO + qt] = -slopes[h] * (PRE + qt * 128 + p)
    return {
        "c_id": np.eye(128, dtype=np.float32),
        "c_u2": (msk / 16.0).astype(np.float32),
        "c_msk": msk,
        "c_tri": le.astype(np.float32),
        "c_jrow": np.broadcast_to((256.0 * np.arange(64) + 128.0).astype(np.float32), (128, 64)).copy(),
        "c_sc": sc,
        "c_negt": negt,
    }


def make_in_maps(inputs, EXT, OWN, nseg, ncores=8):
    x = np.asarray(inputs["x"], np.float32)
    B = x.shape[0]
    consts = make_consts(EXT, OWN)
    shared = dict(consts)
    shared["w_in"] = np.ascontiguousarray(np.asarray(inputs["w_in"], np.float32)[0])
    shared["w_bg"] = np.ascontiguousarray(np.asarray(inputs["w_branch_gla"], np.float32)[0])
    shared["w_bm"] = np.ascontiguousarray(np.asarray(inputs["w_branch_moba"], np.float32)[0])
    shared["w_o"] = np.ascontiguousarray(np.asarray(inputs["w_out"], np.float32)[0])
    shared["c_ng"] = np.ascontiguousarray(np.asarray(inputs["norm_g"], np.float32)[0].reshape(NCH, 128).T)
    shared["c_wga"] = np.concatenate([np.asarray(inputs["w_gla_gate"], np.float32)[0],
                                      np.asarray(inputs["b_gla_gate"], np.float32)[0][None, :]], axis=0)
    shared["c_gout"] = np.ascontiguousarray(np.broadcast_to(
        np.tile(np.asarray(inputs["gla_out_g"], np.float32)[0], 4)[None, :], (128, 1024)))
    shared["c_qg"] = np.ascontiguousarray(np.stack([np.asarray(inputs["q_norm_g"], np.float32)[0],
                                                    np.asarray(inputs["k_norm_g"], np.float32)[0]], axis=1))
    maps = []
    for c in range(ncores):
        b, i = (c // nseg) % B, c % nseg
        end = (i + 1) * OWN
        pad = EXT - end
        xe = np.zeros((EXT, D), np.float32)
        xe[pad:] = x[b, :end]
        bval = np.zeros((128, 64), np.float32)
        bval[:, :pad // 256] = NEG
        m = dict(shared)
        m["x"] = xe
        m["c_bval"] = bval
        maps.append(m)
    return maps


_NC_CACHE = {}


def kernel(**inputs):
    EXT, OWN, nseg = 16384, 4096, 4
    x = np.asarray(inputs["x"])
    B, S, _ = x.shape
    key = (EXT, OWN)
    if key not in _NC_CACHE:
        _NC_CACHE[key] = build(EXT, OWN)
    nc = _NC_CACHE[key]
    maps = make_in_maps(inputs, EXT, OWN, nseg)
    res = run_bass_kernel_spmd(nc, maps, core_ids=list(range(8)))
    out = np.empty((B, S, D), np.float32)
    for c in range(8):
        b, i = c // nseg, c % nseg
        out[b, i * OWN:(i + 1) * OWN] = res.results[c]["y"]
    return out
```

```python
import numpy as np
import ml_dtypes
import concourse.bass as bass
import concourse.mybir as mybir
from concourse.bass_utils import run_bass_kernel_spmd

F32 = mybir.dt.float32
BF16 = mybir.dt.bfloat16
AF = mybir.ActivationFunctionType
ALU = mybir.AluOpType
AX = mybir.AxisListType

D = 2048
NCH = 16
PROJ = 11280
C_GQ, C_GK, C_GV, C_LR, C_GS, C_MQ, C_MK, C_MV, C_MS, C_GA, C_GB = (
    0, 512, 1024, 2048, 2064, 3088, 4112, 5136, 6160, 7184, 9232)
EPS = 1e-6
NEG = -1.0e30


class Tok:
    __slots__ = ("name", "w", "r", "dsem")

    def __init__(self, name):
        self.name = name
        self.w = {}
        self.r = {}
        self.dsem = None


class Prog:
    ENG = ("pe", "act", "dve", "pool", "sp")

    def __init__(self, nc):
        self.nc = nc
        self.q = {e: [] for e in self.ENG}
        self.cnt = {e: 0 for e in self.ENG}
        self.esem = {e: nc.alloc_semaphore("es_" + e) for e in self.ENG}
        self.seen = {e: {} for e in self.ENG}
        self.dsems = []
        self.retired = []
        self.nsem = 0

    def _deps(self, reads, writes):
        deps = {}
        for t in reads:
            for s, v in t.w.items():
                if deps.get(s, 0) < v:
                    deps[s] = v
        for t in writes:
            for s, v in t.w.items():
                if deps.get(s, 0) < v:
                    deps[s] = v
            for s, v in t.r.items():
                if deps.get(s, 0) < v:
                    deps[s] = v
        return deps

    def _waits(self, e, deps, skip_own=False):
        waits = []
        seen = self.seen[e]
        own = self.esem[e]
        for s, v in deps.items():
            if skip_own and s is own:
                continue
            if seen.get(s, 0) < v:
                seen[s] = v
                waits.append((s, v))
        return waits

    def op(self, e, fn, reads=(), writes=(), wadd=()):
        allw = tuple(writes) + tuple(wadd)
        deps = self._deps(reads, allw)
        waits = self._waits(e, deps, skip_own=(e == "pe"))
        if self.cnt[e] >= 30000:
            self.retired.append((self.esem[e], self.cnt[e]))
            self.nsem += 1
            self.esem[e] = self.nc.alloc_semaphore("es_%s_%d" % (e, self.nsem))
            self.cnt[e] = 0
        self.cnt[e] += 1
        c = self.cnt[e]
        sem = self.esem[e]

        def run(eng, waits=waits, fn=fn, sem=sem):
            for s, v in waits:
                eng.wait_ge(s, v)
            fn(eng).then_inc(sem, 1)
        self.q[e].append(run)
        for t in writes:
            t.w = {sem: c}
            t.r = {}
        for t in wadd:
            t.w[sem] = c
        for t in reads:
            t.r[sem] = c

    def dma(self, qe, out, in_, reads=(), writes=(), wadd=(), st=None):
        allw = tuple(writes) + tuple(wadd)
        if st is None:
            st = (allw + tuple(reads))[0]
        if st.dsem is None:
            st.dsem = [self.nc.alloc_semaphore("d_" + st.name), 0]
            self.dsems.append(st)
        sem, tot = st.dsem
        deps = self._deps(reads, allw)
        if tot > 0:
            deps[sem] = max(deps.get(sem, 0), tot)
        waits = self._waits(qe, deps)
        v = tot + 16
        st.dsem[1] = v

        def run(eng, waits=waits, sem=sem, out=out, in_=in_):
            for s, vv in waits:
                eng.wait_ge(s, vv)
            eng.dma_start(out=out, in_=in_).then_inc(sem, 16)
        self.q[qe].append(run)
        for t in writes:
            t.w = {sem: v}
            t.r = {}
        for t in wadd:
            t.w[sem] = v
        for t in reads:
            t.r[sem] = max(t.r.get(sem, 0), v)

    def barrier(self):
        for e in self.ENG:
            deps = {}
            for st in self.dsems:
                sem, tot = st.dsem
                deps[sem] = tot
            for f in self.ENG:
                if f != e and self.cnt[f] > 0:
                    deps[self.esem[f]] = self.cnt[f]
            for rs, rv in self.retired:
                deps[rs] = rv
            waits = self._waits(e, deps)

            def run(eng, waits=waits):
                for s, v in waits:
                    eng.wait_ge(s, v)
            self.q[e].append(run)

    def finish(self):
        deps = {}
        for st in self.dsems:
            sem, tot = st.dsem
            deps[sem] = tot
        for e in self.ENG:
            if e != "sp" and self.cnt[e] > 0:
                deps[self.esem[e]] = self.cnt[e]
        for rs, rv in self.retired:
            deps[rs] = rv
        waits = self._waits("sp", deps)

        def run(eng, waits=waits):
            for s, v in waits:
                eng.wait_ge(s, v)
        self.q["sp"].append(run)

    def emit(self):
        nc = self.nc
        q = self.q
        with nc.Block() as block:
            @block.tensor
            def _(eng):
                for f in q["pe"]:
                    f(eng)

            @block.scalar
            def _(eng):
                for f in q["act"]:
                    f(eng)

            @block.vector
            def _(eng):
                for f in q["dve"]:
                    f(eng)

            @block.gpsimd
            def _(eng):
                for f in q["pool"]:
                    f(eng)

            @block.sync
            def _(eng):
                for f in q["sp"]:
                    f(eng)


class Buf:
    def __init__(self, t, name):
        self.t = t
        self.k = Tok(name)

    def __getitem__(self, key):
        return self.t[key]


class Arena:
    def __init__(self, nc, words):
        self.t = nc.alloc_sbuf_tensor("arena", [128, words], F32)
        self.words = words
        self.off = 0

    def reset(self):
        self.off = 0

    def alloc(self, name, shape, dt):
        nb = 2 if dt == BF16 else 4
        free = int(np.prod(shape[1:]))
        w = (free * nb + 3) // 4
        w = (w + 7) // 8 * 8
        assert self.off + w <= self.words, (name, self.off, w, self.words)
        v = self.t[0:shape[0], self.off:self.off + w]
        self.off += w
        if dt == BF16:
            v = v.bitcast(BF16)
        v = v[:, 0:free]
        if len(shape) == 3:
            v = v.rearrange("p (a b) -> p a b", b=shape[2])
        return Buf(v, name)


def build(EXT, OWN, stop=99):
    nc = bass.Bass("TRN2", target_bir_lowering=False)
    P = Prog(nc)

    def done():
        P.finish()
        P.emit()
        return nc
    NT = EXT // 128
    NTO = OWN // 128
    NB = EXT // 256
    PRE = EXT - OWN
    assert OWN % 512 == 0 and EXT % 512 == 0 and NB <= 64

    def din(name, shape, dt=F32):
        return nc.dram_tensor(name, shape, dt, kind="ExternalInput").ap()

    x = din("x", [EXT, D])
    w_in = din("w_in", [D, PROJ])
    w_bg = din("w_bg", [1024, D])
    w_bm = din("w_bm", [1024, D])
    w_o = din("w_o", [D, D])
    c_ng = din("c_ng", [128, NCH])
    c_wga = din("c_wga", [17, 512])
    c_gout = din("c_gout", [128, 1024])
    c_qg = din("c_qg", [128, 2])
    c_bval = din("c_bval", [128, 64])
    c_id = din("c_id", [128, 128])
    c_u2 = din("c_u2", [128, 128])
    c_msk = din("c_msk", [128, 128])
    c_tri = din("c_tri", [128, 128])
    c_jrow = din("c_jrow", [128, 64])
    c_sc = din("c_sc", [128, 16])
    c_negt = din("c_negt", [128, 8 * NTO])
    y = nc.dram_tensor("y", [OWN, D], F32, kind="ExternalOutput").ap()

    import os as _os0
    _dbg = _os0.environ.get("KDEBUG", "0") == "1"

    def dscr(name, shape, dt=BF16):
        if _dbg:
            return Buf(nc.dram_tensor(name, shape, dt, kind="ExternalOutput").ap(), name)
        return Buf(nc.dram_tensor(name, shape, dt).ap(), name)

    HT = dscr("s_ht", [NCH, 128, EXT])
    KT = dscr("s_kt", [8, 128, EXT])
    VV = dscr("s_v", [EXT, 1024])
    GKV = dscr("s_gkv", [EXT, 1536])
    GKT = dscr("s_gkt", [4, 128, EXT])
    LRT = dscr("s_lrt", [16, EXT], F32)
    GQT = dscr("s_gqt", [4, 128, OWN])
    MQT = dscr("s_mqt", [8, 128, OWN])
    SGT = dscr("s_sgt", [32, 128, OWN])
    GS = dscr("s_gs", [OWN, 2048])
    OT = dscr("s_ot", [16, 128, OWN])
    MT = dscr("s_mt", [16, 128, OWN])
    dbg_outs = {}

    def csb(name, shape, dt):
        return Buf(nc.alloc_sbuf_tensor(name, shape, dt), name)
    uniq = [0]

    def sb(name, shape, dt):
        uniq[0] += 1
        return AR.alloc("%s_%d" % (name, uniq[0]), shape, dt)

    def ps(name, shape, dt=F32):
        return Buf(nc.alloc_psum_tensor(name, shape, dt), name)

    BIG0 = ps("big0", [128, 1024])
    BIG1 = ps("big1", [128, 1024])
    PA0 = ps("pa0", [128, 512])
    PA1 = ps("pa1", [128, 512])
    PTB = [ps("ptb0", [128, 1024], BF16), ps("ptb1", [128, 1024], BF16)]
    PTk = [PTB[0].k, PTB[1].k]

    class Bank:
        def __init__(self, ap, k):
            self.ap = ap
            self.k = k
    b0a, b0b, b1a, b1b = Tok("b0a"), Tok("b0b"), Tok("b1a"), Tok("b1b")
    banks = [Bank(PA0[:, :], PA0.k), Bank(PA1[:, :], PA1.k),
             Bank(BIG0[:, 0:512], b0a), Bank(BIG0[:, 512:1024], b0b),
             Bank(BIG1[:, 0:512], b1a), Bank(BIG1[:, 512:1024], b1b)]

    def const(name, src, shape, dt=F32, q="sp"):
        b = csb(name, shape, dt)
        P.dma(q, b[:], src, writes=[b.k])
        return b
    NG = const("ng", c_ng, [128, NCH])
    WGA = const("wga", c_wga, [17, 512])
    GOUT = const("gout", c_gout, [128, 1024])
    QG = const("qg", c_qg, [128, 2])
    BVAL = const("bval", c_bval, [128, 64])
    IDF = const("idf", c_id, [128, 128])
    U2 = const("u2", c_u2, [128, 128])
    MSK = const("msk", c_msk, [128, 128])
    TRIF = const("trif", c_tri, [128, 128])
    JROW = const("jrow", c_jrow, [128, 64])
    SC = const("sc", c_sc, [128, 16])
    NEGT = const("negt", c_negt, [128, 8 * NTO])
    IDB = csb("idb", [128, 128], BF16)
    TRIB = csb("trib", [128, 128], BF16)
    ONESB = csb("onesb", [128, 128], BF16)
    KM = csb("km", [128, 8, 64], F32)
    KMB = csb("kmb", [128, 8, 64], BF16)
    AR = Arena(nc, 47000)
    P.op("dve", lambda e: e.tensor_copy(out=IDB[:], in_=IDF[:]), reads=[IDF.k], writes=[IDB.k])
    P.op("dve", lambda e: e.tensor_copy(out=TRIB[:], in_=TRIF[:]), reads=[TRIF.k], writes=[TRIB.k])
    P.op("pool", lambda e: e.memset(ONESB[:], 1.0), writes=[ONESB.k])

    R = {}

    def alloc_gemm():
        P.barrier()
        AR.reset()
        R["WBIG"] = sb("wbig", [128, NCH, 2560], BF16)
        R["WSTG"] = [sb("wstg%d" % i, [128, 2560], F32) for i in range(3)]
        R["HTG"] = [sb("htg%d" % i, [128, NCH, 512], BF16) for i in range(2)]
    wstg_n = [0]

    def load_w(src, col_list, dst=None, nch=NCH, scale=True):
        dst = dst or R["WBIG"]
        WSTG = R["WSTG"]
        tot = sum(n for _, n in col_list)
        first = True
        for c in range(nch):
            st = WSTG[wstg_n[0] % 3]
            wstg_n[0] += 1
            off = 0
            for (c0, n) in col_list:
                P.dma("sp", st[:, off:off + n], src[c * 128:(c + 1) * 128, c0:c0 + n],
                      **({"writes": [st.k]} if off == 0 else {"wadd": [st.k]}))
                off += n
            ceng = ("pool", "act", "dve")[c % 3]
            if scale:
                if ceng == "act":
                    fn = (lambda e, st=st, c=c: e.activation(out=dst[:, c, 0:tot], in_=st[:, 0:tot], func=AF.Copy,
                                                             scale=NG[:, c:c + 1]))
                else:
                    fn = (lambda e, st=st, c=c: e.tensor_scalar(out=dst[:, c, 0:tot], in0=st[:, 0:tot],
                                                                 scalar1=NG[:, c:c + 1], scalar2=None, op0=ALU.mult))
                rd = [st.k, NG.k]
            else:
                if ceng == "act":
                    fn = (lambda e, st=st, c=c: e.copy(out=dst[:, c, 0:tot], in_=st[:, 0:tot]))
                else:
                    fn = (lambda e, st=st, c=c: e.tensor_copy(out=dst[:, c, 0:tot], in_=st[:, 0:tot]))
                rd = [st.k]
            if first:
                P.op(ceng, fn, reads=rd, writes=[dst.k])
                first = False
            else:
                P.op(ceng, fn, reads=rd, wadd=[dst.k])

    cp_n = [0]

    def evac_copy(out_ap, in_ap, reads, writes=(), wadd=()):
        cp_n[0] += 1
        if cp_n[0] % 2 == 0:
            P.op("act", lambda e: e.copy(out=out_ap, in_=in_ap), reads=reads, writes=writes, wadd=wadd)
        else:
            P.op("dve", lambda e: e.tensor_copy(out=out_ap, in_=in_ap), reads=reads, writes=writes, wadd=wadd)

    if stop == 0:
        return done()
    XT = [sb("xt%d" % i, [128, D], F32) for i in range(4)]
    JUNK = sb("junk", [128, D], BF16)
    HB = [sb("hb%d" % i, [128, D], BF16) for i in range(2)]
    SSQ = [sb("ssq%d" % i, [128, 1], F32) for i in range(2)]
    HTT = [sb("htt%d" % i, [128, NCH, 128], BF16) for i in range(2)]
    def ph1_a(i):
        xt, hb, ssq = XT[i % 4], HB[i % 2], SSQ[i % 2]
        if i + 3 < NT:
            xn = XT[(i + 3) % 4]
            P.dma("sp", xn[:], x[(i + 3) * 128:(i + 4) * 128, :], writes=[xn.k])
        P.op("act", lambda e: e.activation(out=JUNK[:], in_=xt[:], func=AF.Square, accum_out=ssq[:]),
             reads=[xt.k], writes=[JUNK.k, ssq.k])
        P.op("dve", lambda e: e.tensor_scalar(out=ssq[:], in0=ssq[:], scalar1=1.0 / D, scalar2=EPS,
                                              op0=ALU.mult, op1=ALU.add), reads=[ssq.k], writes=[ssq.k])
        P.op("act", lambda e: e.activation(out=ssq[:], in_=ssq[:], func=AF.Sqrt), reads=[ssq.k], writes=[ssq.k])
        P.op("dve", lambda e: e.reciprocal(out=ssq[:], in_=ssq[:]), reads=[ssq.k], writes=[ssq.k])
        P.op("dve", lambda e: e.tensor_scalar(out=hb[:], in0=xt[:], scalar1=ssq[:, 0:1], scalar2=None, op0=ALU.mult),
             reads=[xt.k, ssq.k], writes=[hb.k])

    def ph1_b(i):
        hb, htt = HB[i % 2], HTT[i % 2]
        for g4 in range(4):
            half = g4 % 2
            for j in range(4):
                c = g4 * 4 + j
                P.op("pe", lambda e, c=c, half=half, j=j: e.transpose(
                    out=PTB[half][:, j * 128:(j + 1) * 128], in_=hb[:, c * 128:(c + 1) * 128],
                    identity=IDB[:]), reads=[hb.k, IDB.k],
                    **({"writes": [PTk[half]]} if j == 0 else {"wadd": [PTk[half]]}))
            evac_copy(htt[:, g4 * 4:(g4 + 1) * 4, :], PTB[half][:, 0:512].rearrange("p (a b) -> p a b", b=128),
                      [PTk[half]], **({"writes": [htt.k]} if g4 == 0 else {"wadd": [htt.k]}))
        for c4 in range(4):
            P.dma("pool", HT[c4 * 4:(c4 + 1) * 4, :, i * 128:(i + 1) * 128].rearrange("c p t -> p c t"),
                  htt[:, c4 * 4:(c4 + 1) * 4, :], reads=[htt.k], wadd=[HT.k], st=htt.k)
    for i0 in range(min(3, NT)):
        P.dma("sp", XT[i0][:], x[i0 * 128:(i0 + 1) * 128, :], writes=[XT[i0].k])
    ph1_a(0)
    for i in range(NT):
        if i + 1 < NT:
            ph1_a(i + 1)
        ph1_b(i)

    if stop == 1:
        return done()
    bank_n = [0]

    def next_bank():
        b = banks[bank_n[0] % len(banks)]
        bank_n[0] += 1
        return b

    def load_htg(tok0, gi):
        htg = R["HTG"][gi % 2]
        P.dma("sp", htg[:], HT[:, :, tok0:tok0 + 512].rearrange("c p t -> p c t"), reads=[HT.k], writes=[htg.k])
        return htg

    def pass_fm(tok0, ntok, jobs):
        pend = None
        WBIG = R["WBIG"]
        ng_ = ntok // 512
        nxt = load_htg(tok0, 0)
        for g in range(ng_):
            htg = nxt
            if g + 1 < ng_:
                nxt = load_htg(tok0 + (g + 1) * 512, g + 1)
            for (woff, M, epi) in jobs:
                b = next_bank()
                for c in range(NCH):
                    P.op("pe", lambda e, b=b, c=c, htg=htg, woff=woff, M=M: e.matmul(
                        b.ap[0:M, :], WBIG[:, c, woff:woff + M], htg[:, c, :], start=(c == 0), stop=(c == NCH - 1)),
                        reads=[WBIG.k, htg.k], **({"writes": [b.k]} if c == 0 else {"wadd": [b.k]}))
                if pend is not None:
                    pend()
                pend = epi(g, b)
        if pend is not None:
            pend()

    def pass_tm(tok0, ntok, jobs):
        WBIG = R["WBIG"]
        ng_ = ntok // 512
        nxt = load_htg(tok0, 0)
        for g in range(ng_):
            htg = nxt
            if g + 1 < ng_:
                nxt = load_htg(tok0 + (g + 1) * 512, g + 1)
            for t4 in range(4):
                for (woff, N, epi) in jobs:
                    b = next_bank()
                    for c in range(NCH):
                        P.op("pe", lambda e, b=b, c=c, htg=htg, woff=woff, N=N, t4=t4: e.matmul(
                            b.ap[:, 0:N], htg[:, c, t4 * 128:(t4 + 1) * 128], WBIG[:, c, woff:woff + N],
                            start=(c == 0), stop=(c == NCH - 1)),
                            reads=[WBIG.k, htg.k], **({"writes": [b.k]} if c == 0 else {"wadd": [b.k]}))
                    epi(g * 4 + t4, b)

    alloc_gemm()
    FST = [sb("fst%d" % i, [128, 512], BF16) for i in range(4)]
    fst_n = [0]
    SQB = [sb("sqb%d" % i, [128, 512], BF16) for i in range(2)]
    RINV = [sb("rinv%d" % i, [128, 512], F32) for i in range(2)]
    KNF = [sb("knf%d" % i, [128, 512], F32) for i in range(2)]
    LRS = [sb("lrs%d" % i, [16, 512], F32) for i in range(2)]
    TST = [sb("tst%d" % i, [128, 2560], BF16) for i in range(2)]
    qk_n = [0]

    def epi_store_fm(dst, chunk, ntok_total, func=None):
        def epi(g, b):
            st = FST[fst_n[0] % 4]
            fst_n[0] += 1
            if func is None:
                evac_copy(st[:], b.ap, [b.k], writes=[st.k])
            else:
                P.op("act", lambda e: e.activation(out=st[:], in_=b.ap, func=func), reads=[b.k], writes=[st.k])
            P.dma("pool", dst[chunk, :, g * 512:(g + 1) * 512], st[:], reads=[st.k], wadd=[dst.k], st=st.k)
            return None
        return epi

    def epi_qknorm(dst, head, gcol, want_mean):
        def epi(g, b):
            i = qk_n[0] % 2
            qk_n[0] += 1
            sqb, rinv, knf = SQB[i], RINV[i], KNF[i]
            P.op("act", lambda e: e.activation(out=sqb[:], in_=b.ap, func=AF.Square), reads=[b.k], writes=[sqb.k])

            def deferred():
                b2 = next_bank()
                P.op("pe", lambda e: e.matmul(b2.ap, ONESB[:], sqb[:], start=True, stop=True),
                     reads=[ONESB.k, sqb.k], writes=[b2.k])
                P.op("dve", lambda e: e.tensor_scalar(out=rinv[:], in0=b2.ap, scalar1=1.0 / 128, scalar2=EPS,
                                                      op0=ALU.mult, op1=ALU.add), reads=[b2.k], writes=[rinv.k])
                P.op("act", lambda e: e.activation(out=rinv[:], in_=rinv[:], func=AF.Sqrt), reads=[rinv.k], writes=[rinv.k])
                P.op("dve", lambda e: e.reciprocal(out=rinv[:], in_=rinv[:]), reads=[rinv.k], writes=[rinv.k])
                st = FST[fst_n[0] % 4]
                fst_n[0] += 1
                if want_mean:
                    P.op("dve", lambda e: e.scalar_tensor_tensor(out=knf[:], in0=b.ap, scalar=QG[:, gcol:gcol + 1],
                                                                  in1=rinv[:], op0=ALU.mult, op1=ALU.mult),
                         reads=[b.k, QG.k, rinv.k], writes=[knf.k])
                    P.op("dve", lambda e: e.tensor_reduce(out=KM[:, head, 2 * g:2 * g + 2],
                                                          in_=knf[:].rearrange("p (a b) -> p a b", b=256),
                                                          axis=AX.X, op=ALU.add), reads=[knf.k], wadd=[KM.k])
                    P.op("pool", lambda e: e.tensor_copy(out=st[:], in_=knf[:]), reads=[knf.k], writes=[st.k])
                else:
                    P.op("dve", lambda e: e.scalar_tensor_tensor(out=st[:], in0=b.ap, scalar=QG[:, gcol:gcol + 1],
                                                                  in1=rinv[:], op0=ALU.mult, op1=ALU.mult),
                         reads=[b.k, QG.k, rinv.k], writes=[st.k])
                P.dma("pool", dst[head, :, g * 512:(g + 1) * 512], st[:], reads=[st.k], wadd=[dst.k], st=st.k)
            return deferred
        return epi

    load_w(w_in, [(C_MK, 1024), (C_GK, 512), (C_LR, 16)])
    lr_n = [0]

    def epi_lr(g, b):
        st = LRS[lr_n[0] % 2]
        lr_n[0] += 1
        evac_copy(st[:], b.ap[0:16, :], [b.k], writes=[st.k])
        P.dma("pool", LRT[:, g * 512:(g + 1) * 512], st[:], reads=[st.k], wadd=[LRT.k], st=st.k)
        return None
    jobsA = [(h * 128, 128, epi_qknorm(KT, h, 1, True)) for h in range(8)]
    jobsA += [(1024 + h * 128, 128, epi_store_fm(GKT, h, EXT)) for h in range(4)]
    jobsA += [(1536, 16, epi_lr)]
    pass_fm(0, EXT, jobsA)
    if stop == 2:
        return done()
    P.op("dve", lambda e: e.tensor_scalar(out=KMB[:], in0=KM[:], scalar1=1.0 / 256, scalar2=None, op0=ALU.mult),
         reads=[KM.k], writes=[KMB.k])

    load_w(w_in, [(C_MV, 1024), (C_GK, 512), (C_GV, 1024)])

    def mk_epi_tm(col0, N, last, dsts):
        def epi(ti, b):
            st = TST[ti % 2]
            evac_copy(st[:, col0:col0 + N], b.ap[:, 0:N], [b.k], **({"writes": [st.k]} if col0 == 0 else {"wadd": [st.k]}))
            if last:
                for (dst, s0, n) in dsts:
                    P.dma("pool", dst[ti * 128:(ti + 1) * 128, :], st[:, s0:s0 + n], reads=[st.k], wadd=[dst.k], st=st.k)
        return epi
    dstsB = [(VV, 0, 1024), (GKV, 1024, 1536)]
    jobsB = [(i * 512, 512, mk_epi_tm(i * 512, 512, i == 4, dstsB)) for i in range(5)]
    pass_tm(0, EXT, jobsB)

    if stop == 3:
        return done()
    load_w(w_in, [(C_MQ, 1024), (C_GQ, 512)])
    jobsD = [(h * 128, 128, epi_qknorm(MQT, h, 0, False)) for h in range(8)]
    jobsD += [(1024 + h * 128, 128, epi_store_fm(GQT, h, OWN)) for h in range(4)]
    pass_fm(PRE, OWN, jobsD)
    for gi, c0 in enumerate((C_GA, C_GB)):
        load_w(w_in, [(c0, 2048)])
        pass_fm(PRE, OWN, [(n * 128, 128, epi_store_fm(SGT, gi * 16 + n, OWN, func=AF.Sigmoid)) for n in range(16)])
    load_w(w_in, [(C_GS, 1024), (C_MS, 1024)])

    def mk_epi_silu(col0, last):
        def epi(ti, b):
            st = TST[ti % 2]
            P.op("act", lambda e: e.activation(out=st[:, col0:col0 + 512], in_=b.ap, func=AF.Silu), reads=[b.k],
                 **({"writes": [st.k]} if col0 == 0 else {"wadd": [st.k]}))
            if last:
                P.dma("pool", GS[ti * 128:(ti + 1) * 128, :], st[:, 0:2048], reads=[st.k], wadd=[GS.k], st=st.k)
        return epi
    pass_tm(PRE, OWN, [(i * 512, 512, mk_epi_silu(i * 512, i == 3)) for i in range(4)])

    if stop == 4:
        return done()
    P.barrier()
    AR.reset()
    KTS = sb("kts", [128, EXT], BF16)
    VP = sb("vp", [128, NT, 129], BF16)
    QS = sb("qs", [128, OWN], BF16)
    GSH = sb("gsh", [128, NTO, 128], BF16)
    SELB = [(sb("gate", [128, 64], F32), sb("t8", [128, 8], F32), sb("msel", [128, 64], F32),
             sb("dex", [128, 64], F32), sb("dm", [128, 64], F32)) for _ in range(2)]
    PTS = [sb("pts%d" % i, [128, 512], BF16) for i in range(4)]
    ACC = sb("acc", [128, 129], F32)
    ACC2 = sb("acc2", [128, 2, 129], F32)
    ACC3 = sb("acc3", [128, 2, 129], F32)
    TMPB = [sb("tmpb%d" % i, [128, 2, 129], F32) for i in range(4)]
    RDEN = sb("rden", [128, 1], F32)
    OB = [sb("ob%d" % i, [128, 128], BF16) for i in range(2)]
    OST = [sb("ost%d" % i, [128, 512], BF16) for i in range(2)]
    SBANKS = [banks[0], banks[1], banks[2]]
    OBANKS = [banks[4], banks[5]]
    GBANK = banks[3]
    sct = [0, 0, 0, 0]
    pend_fin = [None]
    for h in range(8):
        slope = float(2.0 ** (-(h + 1)))
        P.dma("sp", KTS[:], KT[h, :, :], reads=[KT.k], writes=[KTS.k])
        vsrc = VV[:, h * 128:(h + 1) * 128].rearrange("(t p) d -> p t d", p=128)
        for t0_ in range(0, NT, 16):
            t1_ = min(NT, t0_ + 16)
            P.dma("sp", VP[:, t0_:t1_, 0:128], vsrc[:, t0_:t1_, :], reads=[VV.k],
                  **({"writes": [VP.k]} if t0_ == 0 else {"wadd": [VP.k]}))
        P.op("pool", lambda e: e.memset(VP[:, :, 128:129], 1.0), wadd=[VP.k])
        vpv = VP[:].rearrange("p (a two) d -> p a two d", two=2)
        for par in range(2):
            P.op("dve", lambda e, par=par, h=h: e.tensor_scalar(
                out=vpv[:, :, par, :], in0=vpv[:, :, par, :], scalar1=SC[:, 2 * h + par:2 * h + par + 1],
                scalar2=None, op0=ALU.mult), reads=[VP.k, SC.k], writes=[VP.k])
        P.dma("sp", QS[:], MQT[h, :, :], reads=[MQT.k], writes=[QS.k])
        gsrc = GS[:, 1024 + h * 128:1024 + (h + 1) * 128].rearrange("(t p) d -> p t d", p=128)
        for t0_ in range(0, NTO, 16):
            t1_ = min(NTO, t0_ + 16)
            P.dma("sp", GSH[:, t0_:t1_, :], gsrc[:, t0_:t1_, :], reads=[GS.k],
                  **({"writes": [GSH.k]} if t0_ == 0 else {"wadd": [GSH.k]}))
        def prologue(qt, bufs, h=h, slope=slope):
            G, T8, MSEL, DEX, DM = bufs
            eq = PRE // 128 + qt
            ob_ = eq // 2
            nblk = ob_ + 1
            qsl = QS[:, qt * 128:(qt + 1) * 128]
            P.op("pool", lambda e: e.memset(G[:], NEG), writes=[G.k])
            P.op("pool", lambda e: e.memset(MSEL[:], 1.0), writes=[MSEL.k])
            if ob_ > 0:
                P.op("pe", lambda e: e.matmul(GBANK.ap[:, 0:ob_], qsl, KMB[:, h, 0:ob_], start=True, stop=True),
                     reads=[QS.k, KMB.k], writes=[GBANK.k])
                P.op("dve", lambda e: e.tensor_tensor(out=G[:, 0:ob_], in0=GBANK.ap[:, 0:ob_],
                                                      in1=BVAL[:, 0:ob_], op=ALU.add),
                     reads=[GBANK.k, BVAL.k], wadd=[G.k])
                P.op("dve", lambda e: e.max(out=T8[:], in_=G[:]), reads=[G.k], writes=[T8.k])
                P.op("dve", lambda e: e.tensor_scalar_max(out=T8[:, 2:3], in0=T8[:, 2:3], scalar1=-1.0e29),
                     reads=[T8.k], writes=[T8.k])
                P.op("dve", lambda e: e.tensor_scalar(out=MSEL[:, 0:ob_], in0=G[:, 0:ob_],
                                                      scalar1=T8[:, 2:3], scalar2=None, op0=ALU.is_ge),
                     reads=[G.k, T8.k], wadd=[MSEL.k])
            P.op("act", lambda e: e.activation(
                out=DEX[:, 0:nblk], in_=JROW[:, 0:nblk], func=AF.Exp, scale=slope,
                bias=NEGT[:, h * NTO + qt:h * NTO + qt + 1]), reads=[JROW.k, NEGT.k], writes=[DEX.k])
            P.op("dve", lambda e: e.tensor_tensor(out=DM[:, 0:nblk], in0=DEX[:, 0:nblk],
                                                  in1=MSEL[:, 0:nblk], op=ALU.mult),
                 reads=[DEX.k, MSEL.k], writes=[DM.k])

        prologue(0, SELB[0])
        for qt in range(NTO):
            eq = PRE // 128 + qt
            ob_ = eq // 2
            qsl = QS[:, qt * 128:(qt + 1) * 128]
            DM = SELB[qt % 2][4]
            last_kt = 2 * ob_ + (1 if eq % 2 == 1 else 0)
            kts = list(range(last_kt + 1))
            groups = [kts[g0:g0 + 4] for g0 in range(0, len(kts), 4)]

            def emit_qk(gi, groups=groups, qsl=qsl):
                grp = groups[gi]
                sbk = SBANKS[sct[0] % 3]
                sct[0] += 1
                for i_, kt in enumerate(grp):
                    P.op("pe", lambda e, sbk=sbk, i_=i_, kt=kt: e.matmul(
                        sbk.ap[:, i_ * 128:(i_ + 1) * 128], KTS[:, kt * 128:(kt + 1) * 128], qsl, start=True, stop=True),
                        reads=[KTS.k, QS.k], **({"writes": [sbk.k]} if i_ == 0 else {"wadd": [sbk.k]}))
                return sbk
            P.op("pool", lambda e: e.memset(ACC2[:], 0.0), writes=[ACC2.k])
            P.op("pool", lambda e: e.memset(ACC3[:], 0.0), writes=[ACC3.k])
            sbq = [emit_qk(0)]
            if len(groups) > 1:
                sbq.append(emit_qk(1))
            for gi, grp in enumerate(groups):
                sbk = sbq.pop(0)
                if gi + 2 < len(groups):
                    sbq.append(emit_qk(gi + 2))
                if gi == min(1, len(groups) - 1) and qt + 1 < NTO:
                    prologue(qt + 1, SELB[(qt + 1) % 2])
                if gi == min(2, len(groups) - 1) and pend_fin[0] is not None:
                    pend_fin[0]()
                    pend_fin[0] = None
                pts = PTS[sct[1] % 4]
                sct[1] += 1
                n = len(grp)
                P.op("act", lambda e, pts=pts, sbk=sbk, n=n: e.activation(
                    out=pts[:, 0:n * 128], in_=sbk.ap[:, 0:n * 128], func=AF.Exp, scale=float(128 ** -0.5)),
                    reads=[sbk.k], writes=[pts.k])
                if last_kt in grp:
                    i_ = grp.index(last_kt)
                    P.op("pool", lambda e, pts=pts, i_=i_: e.tensor_tensor(
                        out=pts[:, i_ * 128:(i_ + 1) * 128], in0=pts[:, i_ * 128:(i_ + 1) * 128], in1=TRIB[:],
                        op=ALU.mult), reads=[pts.k, TRIB.k], writes=[pts.k])
                blks = sorted(set(kt // 2 for kt in grp))
                obk = OBANKS[sct[2] % 2]
                sct[2] += 1
                for bi, j in enumerate(blks):
                    jk = [kt for kt in grp if kt // 2 == j]
                    for ii, kt in enumerate(jk):
                        i_ = grp.index(kt)
                        P.op("pe", lambda e, obk=obk, pts=pts, i_=i_, kt=kt, ii=ii, jk=jk, bi=bi: e.matmul(
                            obk.ap[:, bi * 256:bi * 256 + 129], pts[:, i_ * 128:(i_ + 1) * 128], VP[:, kt, :],
                            start=(ii == 0), stop=(ii == len(jk) - 1)),
                            reads=[pts.k, VP.k],
                            **({"writes": [obk.k]} if (ii == 0 and bi == 0) else {"wadd": [obk.k]}))
                nb_ = len(blks)
                j0 = blks[0]
                tmp = TMPB[sct[3] % 4]
                sct[3] += 1
                P.op("dve", lambda e, obk=obk, tmp=tmp, nb_=nb_, j0=j0, DM=DM: e.tensor_tensor(
                    out=tmp[:, 0:nb_, :],
                    in0=obk.ap[:, 0:512].rearrange("p (a b) -> p a b", b=256)[:, 0:nb_, 0:129],
                    in1=DM[:, j0:j0 + nb_].unsqueeze(2).to_broadcast([128, nb_, 129]), op=ALU.mult),
                    reads=[obk.k, DM.k], writes=[tmp.k])
                if gi % 3 != 2:
                    P.op("pool", lambda e, tmp=tmp, nb_=nb_: e.tensor_tensor(
                        out=ACC2[:, 0:nb_, :], in0=ACC2[:, 0:nb_, :], in1=tmp[:, 0:nb_, :], op=ALU.add),
                        reads=[tmp.k, ACC2.k], writes=[ACC2.k])
                else:
                    P.op("dve", lambda e, tmp=tmp, nb_=nb_: e.tensor_tensor(
                        out=ACC3[:, 0:nb_, :], in0=ACC3[:, 0:nb_, :], in1=tmp[:, 0:nb_, :], op=ALU.add),
                        reads=[tmp.k, ACC3.k], writes=[ACC3.k])
            P.op("dve", lambda e: e.tensor_tensor(out=ACC3[:], in0=ACC3[:], in1=ACC2[:], op=ALU.add),
                 reads=[ACC2.k, ACC3.k], writes=[ACC3.k])
            P.op("dve", lambda e: e.tensor_tensor(out=ACC[:], in0=ACC3[:, 0, :], in1=ACC3[:, 1, :], op=ALU.add),
                 reads=[ACC3.k], writes=[ACC.k])
            ob16 = OB[qt % 2]
            P.op("dve", lambda e: e.reciprocal(out=RDEN[:], in_=ACC[:, 128:129]), reads=[ACC.k], writes=[RDEN.k])
            P.op("dve", lambda e, ob16=ob16, qt=qt: e.scalar_tensor_tensor(
                out=ob16[:], in0=ACC[:, 0:128], scalar=RDEN[:, 0:1], in1=GSH[:, qt, :], op0=ALU.mult, op1=ALU.mult),
                reads=[ACC.k, RDEN.k, GSH.k], writes=[ob16.k])

            def fin_pe(ob16=ob16, qt=qt, h=h):
                half = (qt // 4) % 2
                j4 = qt % 4
                P.op("pe", lambda e: e.transpose(
                    out=PTB[half][:, j4 * 128:(j4 + 1) * 128], in_=ob16[:], identity=IDB[:]),
                    reads=[ob16.k, IDB.k], **({"writes": [PTk[half]]} if j4 == 0 else {"wadd": [PTk[half]]}))
                if j4 == 3:
                    ost = OST[(qt // 4) % 2]
                    evac_copy(ost[:], PTB[half][:, 0:512], [PTk[half]], writes=[ost.k])
                    P.dma("sp", OT[8 + h, :, (qt - 3) * 128:(qt + 1) * 128], ost[:], reads=[ost.k], wadd=[OT.k],
                          st=ost.k)
            pend_fin[0] = fin_pe
        if pend_fin[0] is not None:
            pend_fin[0]()
            pend_fin[0] = None

    if stop == 5:
        return done()
    P.barrier()
    AR.reset()
    JUNK2 = sb("junk2", [128, 256], BF16)
    GKVT = [sb("gkvt%d" % i, [128, 1536], BF16) for i in range(2)]
    LRA = [sb("lra%d" % i, [17, 128], F32) for i in range(2)]
    KQT = [sb("kqt%d" % i, [128, 8, 128], BF16) for i in range(2)]
    GSL = [sb("gsl%d" % i, [128, 1024], BF16) for i in range(2)]
    E1 = sb("e1", [128, 512], F32)
    SP_ = sb("sp", [128, 512], F32)
    EKTM = sb("ektm", [128, 512], F32)
    KTT = sb("ktt", [128, 512], BF16)
    EQT = sb("eqt", [128, 512], F32)
    EKT = sb("ekt", [128, 512], F32)
    KTF = sb("ktf", [128, 4, 128], BF16)
    QZ = sb("qz", [128, 4, 192], BF16)
    S = sb("S", [128, 1024], F32)
    TS_ = sb("Ts", [128, 1024], F32)
    SBF = [sb("sbf%d" % i, [128, 1024], BF16) for i in range(2)]
    ATB = sb("atb", [128, 512], BF16)
    SSG = sb("ssg", [128, 4], F32)
    GG = sb("gg", [128, 1024], F32)
    OG = sb("og", [128, 1024], BF16)
    OGT = [sb("ogt%d" % i, [128, 8, 128], BF16) for i in range(2)]
    P.op("pool", lambda e: e.memset(S[:], 0.0), writes=[S.k])
    P.op("pool", lambda e: e.memset(SBF[0][:], 0.0), writes=[SBF[0].k])
    P.op("pool", lambda e: e.memset(QZ[:], 0.0), writes=[QZ.k])
    for i in range(2):
        P.op("pool", lambda e, i=i: e.memset(LRA[i][:], 1.0), writes=[LRA[i].k])
    KTT2 = [KTT, sb("ktt_b", [128, 512], BF16)]
    EQT2 = [EQT, sb("eqt_b", [128, 512], F32)]
    KTF2 = [KTF, sb("ktf_b", [128, 4, 128], BF16)]
    QZ2 = [QZ, sb("qz_b", [128, 4, 192], BF16)]
    ATB2 = [ATB, sb("atb_b", [128, 512], BF16)]
    P.op("pool", lambda e: e.memset(QZ2[1][:], 0.0), writes=[QZ2[1].k])

    def gla_prep(ti):
        own = ti >= PRE // 128
        to = ti - PRE // 128
        gkv, lra = GKVT[ti % 2], LRA[ti % 2]
        KTTc, EQTc, KTFc, QZc, ATBc = KTT2[ti % 2], EQT2[ti % 2], KTF2[ti % 2], QZ2[ti % 2], ATB2[ti % 2]
        P.dma("sp", gkv[:], GKV[ti * 128:(ti + 1) * 128, :], reads=[GKV.k], writes=[gkv.k])
        P.dma("sp", lra[0:16, :], LRT[:, ti * 128:(ti + 1) * 128], reads=[LRT.k], wadd=[lra.k])
        if own:
            kqt, gsl = KQT[ti % 2], GSL[ti % 2]
            P.dma("sp", kqt[:, 0:4, :], GKT[:, :, ti * 128:(ti + 1) * 128].rearrange("c p t -> p c t"),
                  reads=[GKT.k], writes=[kqt.k])
            P.dma("sp", kqt[:, 4:8, :], GQT[:, :, to * 128:(to + 1) * 128].rearrange("c p t -> p c t"),
                  reads=[GQT.k], wadd=[kqt.k])
            P.dma("sp", gsl[:], GS[to * 128:(to + 1) * 128, 0:1024], reads=[GS.k], writes=[gsl.k])
        zb = banks[0]
        P.op("pe", lambda e: e.matmul(zb.ap, lra[:], WGA[:], start=True, stop=True),
             reads=[lra.k, WGA.k], writes=[zb.k])
        P.op("act", lambda e: e.activation(out=E1[:], in_=zb.ap, func=AF.Exp, scale=-1.0), reads=[zb.k], writes=[E1.k])
        P.op("act", lambda e: e.activation(out=SP_[:], in_=E1[:], func=AF.Ln, bias=1.0), reads=[E1.k], writes=[SP_.k])
        cb = banks[1]
        P.op("pe", lambda e: e.matmul(cb.ap, U2[:], SP_[:], start=True, stop=True), reads=[U2.k, SP_.k], writes=[cb.k])
        tb = banks[0]
        for hh in range(4):
            P.op("pe", lambda e, hh=hh: e.matmul(tb.ap[:, hh * 128:(hh + 1) * 128], SP_[:, hh * 128:(hh + 1) * 128], U2[:],
                                                 start=True, stop=True), reads=[SP_.k, U2.k],
                 **({"writes": [tb.k]} if hh == 0 else {"wadd": [tb.k]}))
        P.op("act", lambda e: e.activation(out=EKTM[:], in_=cb.ap, func=AF.Exp), reads=[cb.k], writes=[EKTM.k])
        P.op("dve", lambda e: e.tensor_tensor(out=KTTc[:], in0=gkv[:, 0:512], in1=EKTM[:], op=ALU.mult),
             reads=[gkv.k, EKTM.k], writes=[KTTc.k])
        P.op("act", lambda e: e.activation(out=EQTc[:], in_=tb.ap, func=AF.Exp, scale=-1.0), reads=[tb.k], writes=[EQTc.k])
        if own:
            P.op("act", lambda e: e.activation(out=EKT[:], in_=tb.ap, func=AF.Exp), reads=[tb.k], writes=[EKT.k])
            P.op("dve", lambda e: e.tensor_tensor(
                out=KTFc[:], in0=kqt[:, 0:4, :], in1=EKT[:].rearrange("p (a b) -> p a b", b=128), op=ALU.mult),
                reads=[kqt.k, EKT.k], writes=[KTFc.k])
            for c in range(2):
                P.op("dve", lambda e, c=c: e.scalar_tensor_tensor(
                    out=QZc[:, :, c * 128:c * 128 + 64], in0=kqt[:, 4:8, c * 64:(c + 1) * 64], scalar=float(128 ** -0.5),
                    in1=EQTc[:].rearrange("p (a b) -> p a b", b=128)[:, :, c * 64:(c + 1) * 64],
                    op0=ALU.mult, op1=ALU.mult), reads=[kqt.k, EQTc.k], wadd=[QZc.k])
            ab = banks[1]
            for hh in range(4):
                P.op("pe", lambda e, hh=hh: e.matmul(
                    ab.ap[:, hh * 128:(hh + 1) * 128].rearrange("p (a b) -> p a b", b=64), KTFc[:, hh, :],
                    QZc[:, hh, :].rearrange("p (a b) -> p a b", b=64)[:, 0:3:2, :], start=True, stop=True),
                    reads=[KTFc.k, QZc.k], **({"writes": [ab.k]} if hh == 0 else {"wadd": [ab.k]}))
            P.op("dve", lambda e: e.tensor_tensor(
                out=ATBc[:].rearrange("p (a b) -> p a b", b=128), in0=ab.ap.rearrange("p (a b) -> p a b", b=128),
                in1=MSK[:].unsqueeze(1).to_broadcast([128, 4, 128]), op=ALU.mult), reads=[ab.k, MSK.k], writes=[ATBc.k])

    def gla_rec(ti):
        own = ti >= PRE // 128
        to = ti - PRE // 128
        gkv = GKVT[ti % 2]
        KTTc, EQTc, QZc, ATBc = KTT2[ti % 2], EQT2[ti % 2], QZ2[ti % 2], ATB2[ti % 2]

        def state_update(c):
            sout = SBF[(c + 1) % 2]
            for hh in range(4):
                P.op("pe", lambda e, hh=hh: e.matmul(
                    BIG1[:, hh * 256:(hh + 1) * 256], KTTc[c * 64:(c + 1) * 64, hh * 128:(hh + 1) * 128],
                    gkv[c * 64:(c + 1) * 64, 512 + hh * 256:512 + (hh + 1) * 256], start=True, stop=True),
                    reads=[KTTc.k, gkv.k], **({"writes": [b1a, b1b]} if hh == 0 else {"wadd": [b1a, b1b]}))
            P.op("dve", lambda e: e.tensor_tensor(out=TS_[:], in0=BIG1[:, :], in1=S[:], op=ALU.add),
                 reads=[b1a, b1b, S.k], writes=[TS_.k])
            for hh in range(4):
                col = hh * 128 + c * 64 + 63
                P.op("act", lambda e, hh=hh, col=col: e.activation(
                    out=S[:, hh * 256:(hh + 1) * 256], in_=TS_[:, hh * 256:(hh + 1) * 256], func=AF.Copy,
                    scale=EQTc[:, col:col + 1]), reads=[TS_.k, EQTc.k], **({"writes": [S.k]} if hh == 0 else {"wadd": [S.k]}))
            P.op("pool", lambda e: e.tensor_copy(out=sout[:], in_=S[:]), reads=[S.k], writes=[sout.k])
        state_update(0)
        if own:
            for hh in range(4):
                P.op("pe", lambda e, hh=hh: e.matmul(
                    BIG0[:, hh * 256:(hh + 1) * 256], ATBc[:, hh * 128:(hh + 1) * 128],
                    gkv[:, 512 + hh * 256:512 + (hh + 1) * 256], start=True, stop=False),
                    reads=[ATBc.k, gkv.k], **({"writes": [b0a, b0b]} if hh == 0 else {"wadd": [b0a, b0b]}))
                P.op("pe", lambda e, hh=hh: e.matmul(
                    BIG0[:, hh * 256:(hh + 1) * 256], QZc[:, hh, 0:128], SBF[0][:, hh * 256:(hh + 1) * 256],
                    start=False, stop=False), reads=[QZc.k, SBF[0].k], wadd=[b0a, b0b])
                P.op("pe", lambda e, hh=hh: e.matmul(
                    BIG0[:, hh * 256:(hh + 1) * 256], QZc[:, hh, 64:192], SBF[1][:, hh * 256:(hh + 1) * 256],
                    start=False, stop=True), reads=[QZc.k, SBF[1].k], wadd=[b0a, b0b])
        state_update(1)
        if own:
            gsl = GSL[ti % 2]
            for hh in range(4):
                P.op("act", lambda e, hh=hh: e.activation(out=JUNK2[:, 0:256], in_=BIG0[:, hh * 256:(hh + 1) * 256],
                                                          func=AF.Square, accum_out=SSG[:, hh:hh + 1]),
                     reads=[b0a, b0b], writes=[JUNK2.k], wadd=[SSG.k])
            P.op("dve", lambda e: e.tensor_scalar(out=SSG[:], in0=SSG[:], scalar1=1.0 / 256, scalar2=EPS,
                                                  op0=ALU.mult, op1=ALU.add), reads=[SSG.k], writes=[SSG.k])
            P.op("act", lambda e: e.activation(out=SSG[:], in_=SSG[:], func=AF.Sqrt), reads=[SSG.k], writes=[SSG.k])
            P.op("dve", lambda e: e.reciprocal(out=SSG[:], in_=SSG[:]), reads=[SSG.k], writes=[SSG.k])
            P.op("pool", lambda e: e.tensor_tensor(out=GG[:], in0=gsl[:], in1=GOUT[:], op=ALU.mult),
                 reads=[gsl.k, GOUT.k], writes=[GG.k])
            for hh in range(4):
                P.op("dve", lambda e, hh=hh: e.scalar_tensor_tensor(
                    out=OG[:, hh * 256:(hh + 1) * 256], in0=BIG0[:, hh * 256:(hh + 1) * 256], scalar=SSG[:, hh:hh + 1],
                    in1=GG[:, hh * 256:(hh + 1) * 256], op0=ALU.mult, op1=ALU.mult),
                    reads=[b0a, b0b, SSG.k, GG.k], **({"writes": [OG.k]} if hh == 0 else {"wadd": [OG.k]}))
            ogt = OGT[ti % 2]
            for half in range(2):
                for j in range(4):
                    c8 = half * 4 + j
                    P.op("pe", lambda e, c8=c8, half=half, j=j: e.transpose(
                        out=PTB[half][:, j * 128:(j + 1) * 128], in_=OG[:, c8 * 128:(c8 + 1) * 128],
                        identity=IDB[:]), reads=[OG.k, IDB.k],
                        **({"writes": [PTk[half]]} if j == 0 else {"wadd": [PTk[half]]}))
                evac_copy(ogt[:, half * 4:(half + 1) * 4, :],
                          PTB[half][:, 0:512].rearrange("p (a b) -> p a b", b=128), [PTk[half]],
                          **({"writes": [ogt.k]} if half == 0 else {"wadd": [ogt.k]}))
            P.dma("pool", OT[0:8, :, to * 128:(to + 1) * 128].rearrange("c p t -> p c t"), ogt[:], reads=[ogt.k],
                  wadd=[OT.k], st=ogt.k)

    gla_prep(0)
    for ti in range(NT):
        if ti + 1 < NT:
            gla_prep(ti + 1)
        gla_rec(ti)
    if stop == 6:
        return done()
    alloc_gemm()
    WZ = R["WBIG"]
    WSTG = R["WSTG"]
    load_w(w_bg, [(0, 2048)], nch=8, scale=False)
    first = True
    for c in range(8):
        st = WSTG[wstg_n[0] % 3]
        wstg_n[0] += 1
        P.dma("sp", st[:, 0:2048], w_bm[c * 128:(c + 1) * 128, :], writes=[st.k])
        P.op("pool", lambda e, st=st, c=c: e.tensor_copy(out=WZ[:, 8 + c, 0:2048], in_=st[:, 0:2048]),
             reads=[st.k], wadd=[WZ.k])
    OTG = R["HTG"]
    SGG = [sb("sgg%d" % i, [128, 32, 512], BF16) for i in range(1)]
    T1 = [sb("t1_%d" % i, [128, 512], BF16) for i in range(2)]
    T2 = [sb("t2_%d" % i, [128, 512], BF16) for i in range(2)]
    MTS = [sb("mts%d" % i, [128, 512], BF16) for i in range(2)]
    for g in range(OWN // 512):
        otg = OTG[g % 2]
        sgg = SGG[0]
        P.dma("sp", otg[:], OT[:, :, g * 512:(g + 1) * 512].rearrange("c p t -> p c t"), reads=[OT.k], writes=[otg.k])
        for c0_ in (0, 16):
            P.dma("sp", sgg[:, c0_:c0_ + 16, :], SGT[c0_:c0_ + 16, :, g * 512:(g + 1) * 512].rearrange("c p t -> p c t"),
                  reads=[SGT.k], **({"writes": [sgg.k]} if c0_ == 0 else {"wadd": [sgg.k]}))
        for n in range(16):
            bg, bm = next_bank(), next_bank()
            for br, bnk in ((0, bg), (1, bm)):
                for c in range(8):
                    P.op("pe", lambda e, bnk=bnk, br=br, c=c, n=n, otg=otg: e.matmul(
                        bnk.ap, WZ[:, br * 8 + c, n * 128:(n + 1) * 128], otg[:, br * 8 + c, :],
                        start=(c == 0), stop=(c == 7)), reads=[WZ.k, otg.k],
                        **({"writes": [bnk.k]} if c == 0 else {"wadd": [bnk.k]}))
            t1, t2, mts = T1[n % 2], T2[n % 2], MTS[n % 2]
            P.op("dve", lambda e, t1=t1, bg=bg, n=n, sgg=sgg: e.tensor_tensor(out=t1[:], in0=bg.ap, in1=sgg[:, n, :], op=ALU.mult),
                 reads=[bg.k, sgg.k], writes=[t1.k])
            P.op("dve", lambda e, t2=t2, bm=bm, n=n, sgg=sgg: e.tensor_tensor(out=t2[:], in0=bm.ap, in1=sgg[:, 16 + n, :], op=ALU.mult),
                 reads=[bm.k, sgg.k], writes=[t2.k])
            P.op("pool", lambda e, t1=t1, t2=t2, mts=mts: e.tensor_tensor(out=mts[:], in0=t1[:], in1=t2[:], op=ALU.add),
                 reads=[t1.k, t2.k], writes=[mts.k])
            P.dma("pool", MT[n, :, g * 512:(g + 1) * 512], mts[:], reads=[mts.k], wadd=[MT.k], st=mts.k)

    if stop == 7:
        return done()
    alloc_gemm()
    WBIG = R["WBIG"]
    load_w(w_o, [(0, 2048)], scale=False)
    XT = [sb("xtz%d" % i, [128, D], F32) for i in range(2)]
    YT = [sb("yt%d" % i, [128, D], F32) for i in range(2)]
    for g in range(OWN // 512):
        mtg = R["HTG"][g % 2]
        P.dma("sp", mtg[:], MT[:, :, g * 512:(g + 1) * 512].rearrange("c p t -> p c t"), reads=[MT.k], writes=[mtg.k])
        for t4 in range(4):
            ti = g * 4 + t4
            xt, yt = XT[ti % 2], YT[ti % 2]
            P.dma("sp", xt[:], x[PRE + ti * 128:PRE + (ti + 1) * 128, :], writes=[xt.k])
            for ng in range(4):
                b = next_bank()
                for c in range(NCH):
                    P.op("pe", lambda e, b=b, c=c, mtg=mtg, t4=t4, ng=ng: e.matmul(
                        b.ap, mtg[:, c, t4 * 128:(t4 + 1) * 128], WBIG[:, c, ng * 512:(ng + 1) * 512],
                        start=(c == 0), stop=(c == NCH - 1)), reads=[mtg.k, WBIG.k],
                        **({"writes": [b.k]} if c == 0 else {"wadd": [b.k]}))
                P.op("dve", lambda e, b=b, yt=yt, xt=xt, ng=ng: e.tensor_tensor(
                    out=yt[:, ng * 512:(ng + 1) * 512], in0=b.ap, in1=xt[:, ng * 512:(ng + 1) * 512], op=ALU.add),
                    reads=[b.k, xt.k], **({"writes": [yt.k]} if ng == 0 else {"wadd": [yt.k]}))
            P.dma("pool", y[ti * 128:(ti + 1) * 128, :], yt[:], reads=[yt.k], st=yt.k)

    P.finish()
    P.emit()
    return nc


def make_consts(EXT, OWN):
    NTO = OWN // 128
    PRE = EXT - OWN
    p = np.arange(128)
    same = (p[:, None] // 64) == (p[None, :] // 64)
    le = p[:, None] <= p[None, :]
    msk = (same & le).astype(np.float32)
    slopes = 2.0 ** (-8.0 * np.arange(1, 9, dtype=np.float64) / 8)
    sc = np.zeros((128, 16), np.float32)
    negt = np.zeros((128, 8 * NTO), np.float32)
    for h in range(8):
        sc[:, 2 * h] = np.exp(slopes[h] * (p - 128.0))
        sc[:, 2 * h + 1] = np.exp(slopes[h] * (p * 1.0))
        for qt in range(NTO):
            negt[:, h * NTO + qt] = -slopes[h] * (PRE + qt * 128 + p)
    return {
        "c_id": np.eye(128, dtype=np.float32),
        "c_u2": (msk / 16.0).astype(np.float32),
        "c_msk": msk,
        "c_tri": le.astype(np.float32),
        "c_jrow": np.broadcast_to((256.0 * np.arange(64) + 128.0).astype(np.float32), (128, 64)).copy(),
        "c_sc": sc,
        "c_negt": negt,
    }


def make_in_maps(inputs, EXT, OWN, nseg, ncores=8):
    x = np.asarray(inputs["x"], np.float32)
    B = x.shape[0]
    consts = make_consts(EXT, OWN)
    shared = dict(consts)
    shared["w_in"] = np.ascontiguousarray(np.asarray(inputs["w_in"], np.float32)[0])
    shared["w_bg"] = np.ascontiguousarray(np.asarray(inputs["w_branch_gla"], np.float32)[0])
    shared["w_bm"] = np.ascontiguousarray(np.asarray(inputs["w_branch_moba"], np.float32)[0])
    shared["w_o"] = np.ascontiguousarray(np.asarray(inputs["w_out"], np.float32)[0])
    shared["c_ng"] = np.ascontiguousarray(np.asarray(inputs["norm_g"], np.float32)[0].reshape(NCH, 128).T)
    shared["c_wga"] = np.concatenate([np.asarray(inputs["w_gla_gate"], np.float32)[0],
                                      np.asarray(inputs["b_gla_gate"], np.float32)[0][None, :]], axis=0)
    shared["c_gout"] = np.ascontiguousarray(np.broadcast_to(
        np.tile(np.asarray(inputs["gla_out_g"], np.float32)[0], 4)[None, :], (128, 1024)))
    shared["c_qg"] = np.ascontiguousarray(np.stack([np.asarray(inputs["q_norm_g"], np.float32)[0],
                                                    np.asarray(inputs["k_norm_g"], np.float32)[0]], axis=1))
    maps = []
    for c in range(ncores):
        b, i = (c // nseg) % B, c % nseg
        end = (i + 1) * OWN
        pad = EXT - end
        xe = np.zeros((EXT, D), np.float32)
        xe[pad:] = x[b, :end]
        bval = np.zeros((128, 64), np.float32)
        bval[:, :pad // 256] = NEG
        m = dict(shared)
        m["x"] = xe
        m["c_bval"] = bval
        maps.append(m)
    return maps


_NC_CACHE = {}


def kernel(**inputs):
    EXT, OWN, nseg = 16384, 4096, 4
    x = np.asarray(inputs["x"])
    B, S, _ = x.shape
    key = (EXT, OWN)
    if key not in _NC_CACHE:
        _NC_CACHE[key] = build(EXT, OWN)
    nc = _NC_CACHE[key]
    maps = make_in_maps(inputs, EXT, OWN, nseg)
    res = run_bass_kernel_spmd(nc, maps, core_ids=list(range(8)))
    out = np.empty((B, S, D), np.float32)
    for c in range(8):
        b, i = c // nseg, c % nseg
        out[b, i * OWN:(i + 1) * OWN] = res.results[c]["y"]
    return out
```

```python
import numpy as np
import ml_dtypes
import concourse.bass as bass
import concourse.mybir as mybir
from concourse.bass_utils import run_bass_kernel_spmd

F32 = mybir.dt.float32
BF16 = mybir.dt.bfloat16
AF = mybir.ActivationFunctionType
ALU = mybir.AluOpType
AX = mybir.AxisListType

D = 2048
NCH = 16
PROJ = 11280
C_GQ, C_GK, C_GV, C_LR, C_GS, C_MQ, C_MK, C_MV, C_MS, C_GA, C_GB = (
    0, 512, 1024, 2048, 2064, 3088, 4112, 5136, 6160, 7184, 9232)
EPS = 1e-6
NEG = -1.0e30


class Tok:
    __slots__ = ("name", "w", "r", "dsem")

    def __init__(self, name):
        self.name = name
        self.w = {}
        self.r = {}
        self.dsem = None


class Prog:
    ENG = ("pe", "act", "dve", "pool", "sp")

    def __init__(self, nc):
        self.nc = nc
        self.q = {e: [] for e in self.ENG}
        self.cnt = {e: 0 for e in self.ENG}
        self.esem = {e: nc.alloc_semaphore("es_" + e) for e in self.ENG}
        self.seen = {e: {} for e in self.ENG}
        self.dsems = []
        self.retired = []
        self.nsem = 0

    def _deps(self, reads, writes):
        deps = {}
        for t in reads:
            for s, v in t.w.items():
                if deps.get(s, 0) < v:
                    deps[s] = v
        for t in writes:
            for s, v in t.w.items():
                if deps.get(s, 0) < v:
                    deps[s] = v
            for s, v in t.r.items():
                if deps.get(s, 0) < v:
                    deps[s] = v
        return deps

    def _waits(self, e, deps, skip_own=False):
        waits = []
        seen = self.seen[e]
        own = self.esem[e]
        for s, v in deps.items():
            if skip_own and s is own:
                continue
            if seen.get(s, 0) < v:
                seen[s] = v
                waits.append((s, v))
        return waits

    def op(self, e, fn, reads=(), writes=(), wadd=()):
        allw = tuple(writes) + tuple(wadd)
        deps = self._deps(reads, allw)
        waits = self._waits(e, deps, skip_own=(e == "pe"))
        if self.cnt[e] >= 30000:
            self.retired.append((self.esem[e], self.cnt[e]))
            self.nsem += 1
            self.esem[e] = self.nc.alloc_semaphore("es_%s_%d" % (e, self.nsem))
            self.cnt[e] = 0
        self.cnt[e] += 1
        c = self.cnt[e]
        sem = self.esem[e]

        def run(eng, waits=waits, fn=fn, sem=sem):
            for s, v in waits:
                eng.wait_ge(s, v)
            fn(eng).then_inc(sem, 1)
        self.q[e].append(run)
        for t in writes:
            t.w = {sem: c}
            t.r = {}
        for t in wadd:
            t.w[sem] = c
        for t in reads:
            t.r[sem] = c

    def dma(self, qe, out, in_, reads=(), writes=(), wadd=(), st=None):
        allw = tuple(writes) + tuple(wadd)
        if st is None:
            st = (allw + tuple(reads))[0]
        if st.dsem is None:
            st.dsem = [self.nc.alloc_semaphore("d_" + st.name), 0]
            self.dsems.append(st)
        sem, tot = st.dsem
        deps = self._deps(reads, allw)
        if tot > 0:
            deps[sem] = max(deps.get(sem, 0), tot)
        waits = self._waits(qe, deps)
        v = tot + 16
        st.dsem[1] = v

        def run(eng, waits=waits, sem=sem, out=out, in_=in_):
            for s, vv in waits:
                eng.wait_ge(s, vv)
            eng.dma_start(out=out, in_=in_).then_inc(sem, 16)
        self.q[qe].append(run)
        for t in writes:
            t.w = {sem: v}
            t.r = {}
        for t in wadd:
            t.w[sem] = v
        for t in reads:
            t.r[sem] = max(t.r.get(sem, 0), v)

    def barrier(self):
        for e in self.ENG:
            deps = {}
            for st in self.dsems:
                sem, tot = st.dsem
                deps[sem] = tot
            for f in self.ENG:
                if f != e and self.cnt[f] > 0:
                    deps[self.esem[f]] = self.cnt[f]
            for rs, rv in self.retired:
                deps[rs] = rv
            waits = self._waits(e, deps)

            def run(eng, waits=waits):
                for s, v in waits:
                    eng.wait_ge(s, v)
            self.q[e].append(run)

    def finish(self):
        deps = {}
        for st in self.dsems:
            sem, tot = st.dsem
            deps[sem] = tot
        for e in self.ENG:
            if e != "sp" and self.cnt[e] > 0:
                deps[self.esem[e]] = self.cnt[e]
        for rs, rv in self.retired:
            deps[rs] = rv
        waits = self._waits("sp", deps)

        def run(eng, waits=waits):
            for s, v in waits:
                eng.wait_ge(s, v)
        self.q["sp"].append(run)

    def emit(self):
        nc = self.nc
        q = self.q
        with nc.Block() as block:
            @block.tensor
            def _(eng):
                for f in q["pe"]:
                    f(eng)

            @block.scalar
            def _(eng):
                for f in q["act"]:
                    f(eng)

            @block.vector
            def _(eng):
                for f in q["dve"]:
                    f(eng)

            @block.gpsimd
            def _(eng):
                for f in q["pool"]:
                    f(eng)

            @block.sync
            def _(eng):
                for f in q["sp"]:
                    f(eng)


class Buf:
    def __init__(self, t, name):
        self.t = t
        self.k = Tok(name)

    def __getitem__(self, key):
        return self.t[key]


class Arena:
    def __init__(self, nc, words):
        self.t = nc.alloc_sbuf_tensor("arena", [128, words], F32)
        self.words = words
        self.off = 0

    def reset(self):
        self.off = 0

    def alloc(self, name, shape, dt):
        nb = 2 if dt == BF16 else 4
        free = int(np.prod(shape[1:]))
        w = (free * nb + 3) // 4
        w = (w + 7) // 8 * 8
        assert self.off + w <= self.words, (name, self.off, w, self.words)
        v = self.t[0:shape[0], self.off:self.off + w]
        self.off += w
        if dt == BF16:
            v = v.bitcast(BF16)
        v = v[:, 0:free]
        if len(shape) == 3:
            v = v.rearrange("p (a b) -> p a b", b=shape[2])
        return Buf(v, name)


def build(EXT, OWN, stop=99):
    nc = bass.Bass("TRN2", target_bir_lowering=False)
    P = Prog(nc)

    def done():
        P.finish()
        P.emit()
        return nc
    NT = EXT // 128
    NTO = OWN // 128
    NB = EXT // 256
    PRE = EXT - OWN
    assert OWN % 512 == 0 and EXT % 512 == 0 and NB <= 64

    def din(name, shape, dt=F32):
        return nc.dram_tensor(name, shape, dt, kind="ExternalInput").ap()

    x = din("x", [EXT, D])
    w_in = din("w_in", [D, PROJ])
    w_bg = din("w_bg", [1024, D])
    w_bm = din("w_bm", [1024, D])
    w_o = din("w_o", [D, D])
    c_ng = din("c_ng", [128, NCH])
    c_wga = din("c_wga", [17, 512])
    c_gout = din("c_gout", [128, 1024])
    c_qg = din("c_qg", [128, 2])
    c_bval = din("c_bval", [128, 64])
    c_id = din("c_id", [128, 128])
    c_u2 = din("c_u2", [128, 128])
    c_msk = din("c_msk", [128, 128])
    c_tri = din("c_tri", [128, 128])
    c_jrow = din("c_jrow", [128, 64])
    c_sc = din("c_sc", [128, 16])
    c_negt = din("c_negt", [128, 8 * NTO])
    y = nc.dram_tensor("y", [OWN, D], F32, kind="ExternalOutput").ap()

    import os as _os0
    _dbg = _os0.environ.get("KDEBUG", "0") == "1"

    def dscr(name, shape, dt=BF16):
        if _dbg:
            return Buf(nc.dram_tensor(name, shape, dt, kind="ExternalOutput").ap(), name)
        return Buf(nc.dram_tensor(name, shape, dt).ap(), name)

    HT = dscr("s_ht", [NCH, 128, EXT])
    KT = dscr("s_kt", [8, 128, EXT])
    VV = dscr("s_v", [EXT, 1024])
    GKV = dscr("s_gkv", [EXT, 1536])
    GKT = dscr("s_gkt", [4, 128, EXT])
    LRT = dscr("s_lrt", [16, EXT], F32)
    GQT = dscr("s_gqt", [4, 128, OWN])
    MQT = dscr("s_mqt", [8, 128, OWN])
    SGT = dscr("s_sgt", [32, 128, OWN])
    GS = dscr("s_gs", [OWN, 2048])
    OT = dscr("s_ot", [16, 128, OWN])
    MT = dscr("s_mt", [16, 128, OWN])
    dbg_outs = {}

    def csb(name, shape, dt):
        return Buf(nc.alloc_sbuf_tensor(name, shape, dt), name)
    uniq = [0]

    def sb(name, shape, dt):
        uniq[0] += 1
        return AR.alloc("%s_%d" % (name, uniq[0]), shape, dt)

    def ps(name, shape, dt=F32):
        return Buf(nc.alloc_psum_tensor(name, shape, dt), name)

    BIG0 = ps("big0", [128, 1024])
    BIG1 = ps("big1", [128, 1024])
    PA0 = ps("pa0", [128, 512])
    PA1 = ps("pa1", [128, 512])
    PTB = [ps("ptb0", [128, 1024], BF16), ps("ptb1", [128, 1024], BF16)]
    PTk = [PTB[0].k, PTB[1].k]

    class Bank:
        def __init__(self, ap, k):
            self.ap = ap
            self.k = k
    b0a, b0b, b1a, b1b = Tok("b0a"), Tok("b0b"), Tok("b1a"), Tok("b1b")
    banks = [Bank(PA0[:, :], PA0.k), Bank(PA1[:, :], PA1.k),
             Bank(BIG0[:, 0:512], b0a), Bank(BIG0[:, 512:1024], b0b),
             Bank(BIG1[:, 0:512], b1a), Bank(BIG1[:, 512:1024], b1b)]

    def const(name, src, shape, dt=F32, q="sp"):
        b = csb(name, shape, dt)
        P.dma(q, b[:], src, writes=[b.k])
        return b
    NG = const("ng", c_ng, [128, NCH])
    WGA = const("wga", c_wga, [17, 512])
    GOUT = const("gout", c_gout, [128, 1024])
    QG = const("qg", c_qg, [128, 2])
    BVAL = const("bval", c_bval, [128, 64])
    IDF = const("idf", c_id, [128, 128])
    U2 = const("u2", c_u2, [128, 128])
    MSK = const("msk", c_msk, [128, 128])
    TRIF = const("trif", c_tri, [128, 128])
    JROW = const("jrow", c_jrow, [128, 64])
    SC = const("sc", c_sc, [128, 16])
    NEGT = const("negt", c_negt, [128, 8 * NTO])
    IDB = csb("idb", [128, 128], BF16)
    TRIB = csb("trib", [128, 128], BF16)
    ONESB = csb("onesb", [128, 128], BF16)
    KM = csb("km", [128, 8, 64], F32)
    KMB = csb("kmb", [128, 8, 64], BF16)
    AR = Arena(nc, 47000)
    P.op("dve", lambda e: e.tensor_copy(out=IDB[:], in_=IDF[:]), reads=[IDF.k], writes=[IDB.k])
    P.op("dve", lambda e: e.tensor_copy(out=TRIB[:], in_=TRIF[:]), reads=[TRIF.k], writes=[TRIB.k])
    P.op("pool", lambda e: e.memset(ONESB[:], 1.0), writes=[ONESB.k])

    R = {}

    def alloc_gemm():
        P.barrier()
        AR.reset()
        R["WBIG"] = sb("wbig", [128, NCH, 2560], BF16)
        R["WSTG"] = [sb("wstg%d" % i, [128, 2560], F32) for i in range(3)]
        R["HTG"] = [sb("htg%d" % i, [128, NCH, 512], BF16) for i in range(2)]
    wstg_n = [0]

    def load_w(src, col_list, dst=None, nch=NCH, scale=True):
        dst = dst or R["WBIG"]
        WSTG = R["WSTG"]
        tot = sum(n for _, n in col_list)
        first = True
        for c in range(nch):
            st = WSTG[wstg_n[0] % 3]
            wstg_n[0] += 1
            off = 0
            for (c0, n) in col_list:
                P.dma("sp", st[:, off:off + n], src[c * 128:(c + 1) * 128, c0:c0 + n],
                      **({"writes": [st.k]} if off == 0 else {"wadd": [st.k]}))
                off += n
            ceng = ("pool", "act", "dve")[c % 3]
            if scale:
                if ceng == "act":
                    fn = (lambda e, st=st, c=c: e.activation(out=dst[:, c, 0:tot], in_=st[:, 0:tot], func=AF.Copy,
                                                             scale=NG[:, c:c + 1]))
                else:
                    fn = (lambda e, st=st, c=c: e.tensor_scalar(out=dst[:, c, 0:tot], in0=st[:, 0:tot],
                                                                 scalar1=NG[:, c:c + 1], scalar2=None, op0=ALU.mult))
                rd = [st.k, NG.k]
            else:
                if ceng == "act":
                    fn = (lambda e, st=st, c=c: e.copy(out=dst[:, c, 0:tot], in_=st[:, 0:tot]))
                else:
                    fn = (lambda e, st=st, c=c: e.tensor_copy(out=dst[:, c, 0:tot], in_=st[:, 0:tot]))
                rd = [st.k]
            if first:
                P.op(ceng, fn, reads=rd, writes=[dst.k])
                first = False
            else:
                P.op(ceng, fn, reads=rd, wadd=[dst.k])

    cp_n = [0]

    def evac_copy(out_ap, in_ap, reads, writes=(), wadd=()):
        cp_n[0] += 1
        if cp_n[0] % 2 == 0:
            P.op("act", lambda e: e.copy(out=out_ap, in_=in_ap), reads=reads, writes=writes, wadd=wadd)
        else:
            P.op("dve", lambda e: e.tensor_copy(out=out_ap, in_=in_ap), reads=reads, writes=writes, wadd=wadd)

    if stop == 0:
        return done()
    XT = [sb("xt%d" % i, [128, D], F32) for i in range(4)]
    JUNK = sb("junk", [128, D], BF16)
    HB = [sb("hb%d" % i, [128, D], BF16) for i in range(2)]
    SSQ = [sb("ssq%d" % i, [128, 1], F32) for i in range(2)]
    HTT = [sb("htt%d" % i, [128, NCH, 512], BF16) for i in range(2)]
    def ph1_a(i):
        xt, hb, ssq = XT[i % 4], HB[i % 2], SSQ[i % 2]
        if i + 3 < NT:
            xn = XT[(i + 3) % 4]
            P.dma("sp", xn[:], x[(i + 3) * 128:(i + 4) * 128, :], writes=[xn.k])
        P.op("act", lambda e: e.activation(out=JUNK[:], in_=xt[:], func=AF.Square, accum_out=ssq[:]),
             reads=[xt.k], writes=[JUNK.k, ssq.k])
        P.op("dve", lambda e: e.tensor_scalar(out=ssq[:], in0=ssq[:], scalar1=1.0 / D, scalar2=EPS,
                                              op0=ALU.mult, op1=ALU.add), reads=[ssq.k], writes=[ssq.k])
        P.op("act", lambda e: e.activation(out=ssq[:], in_=ssq[:], func=AF.Sqrt), reads=[ssq.k], writes=[ssq.k])
        P.op("dve", lambda e: e.reciprocal(out=ssq[:], in_=ssq[:]), reads=[ssq.k], writes=[ssq.k])
        P.op("dve", lambda e: e.tensor_scalar(out=hb[:], in0=xt[:], scalar1=ssq[:, 0:1], scalar2=None, op0=ALU.mult),
             reads=[xt.k, ssq.k], writes=[hb.k])

    def ph1_b(i):
        hb, htt = HB[i % 2], HTT[(i // 4) % 2]
        o4 = (i % 4) * 128
        for g4 in range(4):
            half = g4 % 2
            for j in range(4):
                c = g4 * 4 + j
                P.op("pe", lambda e, c=c, half=half, j=j: e.transpose(
                    out=PTB[half][:, j * 128:(j + 1) * 128], in_=hb[:, c * 128:(c + 1) * 128],
                    identity=IDB[:]), reads=[hb.k, IDB.k],
                    **({"writes": [PTk[half]]} if j == 0 else {"wadd": [PTk[half]]}))
            evac_copy(htt[:, g4 * 4:(g4 + 1) * 4, o4:o4 + 128], PTB[half][:, 0:512].rearrange("p (a b) -> p a b", b=128),
                      [PTk[half]], **({"writes": [htt.k]} if (g4 == 0 and i % 4 == 0) else {"wadd": [htt.k]}))
        if i % 4 == 3:
            i0 = i - 3
            for c4 in range(4):
                P.dma("pool", HT[c4 * 4:(c4 + 1) * 4, :, i0 * 128:(i0 + 4) * 128].rearrange("c p t -> p c t"),
                      htt[:, c4 * 4:(c4 + 1) * 4, :], reads=[htt.k], wadd=[HT.k], st=htt.k)
    for i0 in range(min(3, NT)):
        P.dma("sp", XT[i0][:], x[i0 * 128:(i0 + 1) * 128, :], writes=[XT[i0].k])
    ph1_a(0)
    for i in range(NT):
        if i + 1 < NT:
            ph1_a(i + 1)
        ph1_b(i)

    if stop == 1:
        return done()
    bank_n = [0]

    def next_bank():
        b = banks[bank_n[0] % len(banks)]
        bank_n[0] += 1
        return b

    def load_htg(tok0, gi):
        htg = R["HTG"][gi % 2]
        P.dma("sp", htg[:], HT[:, :, tok0:tok0 + 512].rearrange("c p t -> p c t"), reads=[HT.k], writes=[htg.k])
        return htg

    def pass_fm(tok0, ntok, jobs):
        pend = None
        WBIG = R["WBIG"]
        ng_ = ntok // 512
        nxt = load_htg(tok0, 0)
        for g in range(ng_):
            htg = nxt
            if g + 1 < ng_:
                nxt = load_htg(tok0 + (g + 1) * 512, g + 1)
            for (woff, M, epi) in jobs:
                b = next_bank()
                for c in range(NCH):
                    P.op("pe", lambda e, b=b, c=c, htg=htg, woff=woff, M=M: e.matmul(
                        b.ap[0:M, :], WBIG[:, c, woff:woff + M], htg[:, c, :], start=(c == 0), stop=(c == NCH - 1)),
                        reads=[WBIG.k, htg.k], **({"writes": [b.k]} if c == 0 else {"wadd": [b.k]}))
                if pend is not None:
                    pend()
                pend = epi(g, b)
        if pend is not None:
            pend()

    def pass_tm(tok0, ntok, jobs):
        WBIG = R["WBIG"]
        ng_ = ntok // 512
        nxt = load_htg(tok0, 0)
        for g in range(ng_):
            htg = nxt
            if g + 1 < ng_:
                nxt = load_htg(tok0 + (g + 1) * 512, g + 1)
            for t4 in range(4):
                for (woff, N, epi) in jobs:
                    b = next_bank()
                    for c in range(NCH):
                        P.op("pe", lambda e, b=b, c=c, htg=htg, woff=woff, N=N, t4=t4: e.matmul(
                            b.ap[:, 0:N], htg[:, c, t4 * 128:(t4 + 1) * 128], WBIG[:, c, woff:woff + N],
                            start=(c == 0), stop=(c == NCH - 1)),
                            reads=[WBIG.k, htg.k], **({"writes": [b.k]} if c == 0 else {"wadd": [b.k]}))
                    epi(g * 4 + t4, b)

    alloc_gemm()
    FST = [sb("fst%d" % i, [128, 512], BF16) for i in range(4)]
    fst_n = [0]
    SQB = [sb("sqb%d" % i, [128, 512], BF16) for i in range(2)]
    RINV = [sb("rinv%d" % i, [128, 512], F32) for i in range(2)]
    KNF = [sb("knf%d" % i, [128, 512], F32) for i in range(2)]
    LRS = [sb("lrs%d" % i, [16, 512], F32) for i in range(2)]
    TST = [sb("tst%d" % i, [128, 2560], BF16) for i in range(2)]
    qk_n = [0]

    def epi_store_fm(dst, chunk, ntok_total, func=None):
        def epi(g, b):
            st = FST[fst_n[0] % 4]
            fst_n[0] += 1
            if func is None:
                evac_copy(st[:], b.ap, [b.k], writes=[st.k])
            else:
                P.op("act", lambda e: e.activation(out=st[:], in_=b.ap, func=func), reads=[b.k], writes=[st.k])
            P.dma("pool", dst[chunk, :, g * 512:(g + 1) * 512], st[:], reads=[st.k], wadd=[dst.k], st=st.k)
            return None
        return epi

    def epi_qknorm(dst, head, gcol, want_mean):
        def epi(g, b):
            i = qk_n[0] % 2
            qk_n[0] += 1
            sqb, rinv, knf = SQB[i], RINV[i], KNF[i]
            P.op("act", lambda e: e.activation(out=sqb[:], in_=b.ap, func=AF.Square), reads=[b.k], writes=[sqb.k])

            def deferred():
                b2 = next_bank()
                P.op("pe", lambda e: e.matmul(b2.ap, ONESB[:], sqb[:], start=True, stop=True),
                     reads=[ONESB.k, sqb.k], writes=[b2.k])
                P.op("dve", lambda e: e.tensor_scalar(out=rinv[:], in0=b2.ap, scalar1=1.0 / 128, scalar2=EPS,
                                                      op0=ALU.mult, op1=ALU.add), reads=[b2.k], writes=[rinv.k])
                P.op("act", lambda e: e.activation(out=rinv[:], in_=rinv[:], func=AF.Sqrt), reads=[rinv.k], writes=[rinv.k])
                P.op("dve", lambda e: e.reciprocal(out=rinv[:], in_=rinv[:]), reads=[rinv.k], writes=[rinv.k])
                st = FST[fst_n[0] % 4]
                fst_n[0] += 1
                if want_mean:
                    P.op("dve", lambda e: e.scalar_tensor_tensor(out=knf[:], in0=b.ap, scalar=QG[:, gcol:gcol + 1],
                                                                  in1=rinv[:], op0=ALU.mult, op1=ALU.mult),
                         reads=[b.k, QG.k, rinv.k], writes=[knf.k])
                    P.op("dve", lambda e: e.tensor_reduce(out=KM[:, head, 2 * g:2 * g + 2],
                                                          in_=knf[:].rearrange("p (a b) -> p a b", b=256),
                                                          axis=AX.X, op=ALU.add), reads=[knf.k], wadd=[KM.k])
                    P.op("pool", lambda e: e.tensor_copy(out=st[:], in_=knf[:]), reads=[knf.k], writes=[st.k])
                else:
                    P.op("dve", lambda e: e.scalar_tensor_tensor(out=st[:], in0=b.ap, scalar=QG[:, gcol:gcol + 1],
                                                                  in1=rinv[:], op0=ALU.mult, op1=ALU.mult),
                         reads=[b.k, QG.k, rinv.k], writes=[st.k])
                P.dma("pool", dst[head, :, g * 512:(g + 1) * 512], st[:], reads=[st.k], wadd=[dst.k], st=st.k)
            return deferred
        return epi

    load_w(w_in, [(C_MK, 1024), (C_GK, 512), (C_LR, 16)])
    lr_n = [0]

    def epi_lr(g, b):
        st = LRS[lr_n[0] % 2]
        lr_n[0] += 1
        evac_copy(st[:], b.ap[0:16, :], [b.k], writes=[st.k])
        P.dma("pool", LRT[:, g * 512:(g + 1) * 512], st[:], reads=[st.k], wadd=[LRT.k], st=st.k)
        return None
    jobsA = [(h * 128, 128, epi_qknorm(KT, h, 1, True)) for h in range(8)]
    jobsA += [(1024 + h * 128, 128, epi_store_fm(GKT, h, EXT)) for h in range(4)]
    jobsA += [(1536, 16, epi_lr)]
    pass_fm(0, EXT, jobsA)
    if stop == 2:
        return done()
    P.op("dve", lambda e: e.tensor_scalar(out=KMB[:], in0=KM[:], scalar1=1.0 / 256, scalar2=None, op0=ALU.mult),
         reads=[KM.k], writes=[KMB.k])

    load_w(w_in, [(C_MV, 1024), (C_GK, 512), (C_GV, 1024)])

    def mk_epi_tm(col0, N, last, dsts):
        def epi(ti, b):
            st = TST[ti % 2]
            evac_copy(st[:, col0:col0 + N], b.ap[:, 0:N], [b.k], **({"writes": [st.k]} if col0 == 0 else {"wadd": [st.k]}))
            if last:
                for (dst, s0, n) in dsts:
                    P.dma("pool", dst[ti * 128:(ti + 1) * 128, :], st[:, s0:s0 + n], reads=[st.k], wadd=[dst.k], st=st.k)
        return epi
    dstsB = [(VV, 0, 1024), (GKV, 1024, 1536)]
    jobsB = [(i * 512, 512, mk_epi_tm(i * 512, 512, i == 4, dstsB)) for i in range(5)]
    pass_tm(0, EXT, jobsB)

    if stop == 3:
        return done()
    load_w(w_in, [(C_MQ, 1024), (C_GQ, 512)])
    jobsD = [(h * 128, 128, epi_qknorm(MQT, h, 0, False)) for h in range(8)]
    jobsD += [(1024 + h * 128, 128, epi_store_fm(GQT, h, OWN)) for h in range(4)]
    pass_fm(PRE, OWN, jobsD)
    for gi, c0 in enumerate((C_GA, C_GB)):
        load_w(w_in, [(c0, 2048)])
        pass_fm(PRE, OWN, [(n * 128, 128, epi_store_fm(SGT, gi * 16 + n, OWN, func=AF.Sigmoid)) for n in range(16)])
    load_w(w_in, [(C_GS, 1024), (C_MS, 1024)])

    def mk_epi_silu(col0, last):
        def epi(ti, b):
            st = TST[ti % 2]
            P.op("act", lambda e: e.activation(out=st[:, col0:col0 + 512], in_=b.ap, func=AF.Silu), reads=[b.k],
                 **({"writes": [st.k]} if col0 == 0 else {"wadd": [st.k]}))
            if last:
                P.dma("pool", GS[ti * 128:(ti + 1) * 128, :], st[:, 0:2048], reads=[st.k], wadd=[GS.k], st=st.k)
        return epi
    pass_tm(PRE, OWN, [(i * 512, 512, mk_epi_silu(i * 512, i == 3)) for i in range(4)])

    if stop == 4:
        return done()
    P.barrier()
    AR.reset()
    KTS = sb("kts", [128, EXT], BF16)
    VP = sb("vp", [128, NT, 129], BF16)
    QS = sb("qs", [128, OWN], BF16)
    GSH = sb("gsh", [128, NTO, 128], BF16)
    SELB = [(sb("gate", [128, 64], F32), sb("t8", [128, 8], F32), sb("msel", [128, 64], F32),
             sb("dex", [128, 64], F32), sb("dm", [128, 64], F32)) for _ in range(2)]
    PTS = [sb("pts%d" % i, [128, 512], BF16) for i in range(4)]
    ACC = sb("acc", [128, 129], F32)
    ACC2 = sb("acc2", [128, 2, 129], F32)
    ACC3 = sb("acc3", [128, 2, 129], F32)
    TMPB = [sb("tmpb%d" % i, [128, 2, 129], F32) for i in range(4)]
    RDEN = sb("rden", [128, 1], F32)
    OB = [sb("ob%d" % i, [128, 128], BF16) for i in range(2)]
    OST = [sb("ost%d" % i, [128, 512], BF16) for i in range(2)]
    SBANKS = [banks[0], banks[1], banks[2]]
    OBANKS = [banks[4], banks[5]]
    GBANK = banks[3]
    sct = [0, 0, 0, 0]
    pend_fin = [None]
    for h in range(8):
        slope = float(2.0 ** (-(h + 1)))
        P.dma("sp", KTS[:], KT[h, :, :], reads=[KT.k], writes=[KTS.k])
        vsrc = VV[:, h * 128:(h + 1) * 128].rearrange("(t p) d -> p t d", p=128)
        for t0_ in range(0, NT, 16):
            t1_ = min(NT, t0_ + 16)
            P.dma("sp", VP[:, t0_:t1_, 0:128], vsrc[:, t0_:t1_, :], reads=[VV.k],
                  **({"writes": [VP.k]} if t0_ == 0 else {"wadd": [VP.k]}))
        P.op("pool", lambda e: e.memset(VP[:, :, 128:129], 1.0), wadd=[VP.k])
        vpv = VP[:].rearrange("p (a two) d -> p a two d", two=2)
        for par in range(2):
            P.op("dve", lambda e, par=par, h=h: e.tensor_scalar(
                out=vpv[:, :, par, :], in0=vpv[:, :, par, :], scalar1=SC[:, 2 * h + par:2 * h + par + 1],
                scalar2=None, op0=ALU.mult), reads=[VP.k, SC.k], writes=[VP.k])
        P.dma("sp", QS[:], MQT[h, :, :], reads=[MQT.k], writes=[QS.k])
        gsrc = GS[:, 1024 + h * 128:1024 + (h + 1) * 128].rearrange("(t p) d -> p t d", p=128)
        for t0_ in range(0, NTO, 16):
            t1_ = min(NTO, t0_ + 16)
            P.dma("sp", GSH[:, t0_:t1_, :], gsrc[:, t0_:t1_, :], reads=[GS.k],
                  **({"writes": [GSH.k]} if t0_ == 0 else {"wadd": [GSH.k]}))
        def prologue(qt, bufs, h=h, slope=slope):
            G, T8, MSEL, DEX, DM = bufs
            eq = PRE // 128 + qt
            ob_ = eq // 2
            nblk = ob_ + 1
            qsl = QS[:, qt * 128:(qt + 1) * 128]
            P.op("pool", lambda e: e.memset(G[:], NEG), writes=[G.k])
            P.op("pool", lambda e: e.memset(MSEL[:], 1.0), writes=[MSEL.k])
            if ob_ > 0:
                P.op("pe", lambda e: e.matmul(GBANK.ap[:, 0:ob_], qsl, KMB[:, h, 0:ob_], start=True, stop=True),
                     reads=[QS.k, KMB.k], writes=[GBANK.k])
                P.op("dve", lambda e: e.tensor_tensor(out=G[:, 0:ob_], in0=GBANK.ap[:, 0:ob_],
                                                      in1=BVAL[:, 0:ob_], op=ALU.add),
                     reads=[GBANK.k, BVAL.k], wadd=[G.k])
                P.op("dve", lambda e: e.max(out=T8[:], in_=G[:]), reads=[G.k], writes=[T8.k])
                P.op("dve", lambda e: e.tensor_scalar_max(out=T8[:, 2:3], in0=T8[:, 2:3], scalar1=-1.0e29),
                     reads=[T8.k], writes=[T8.k])
                P.op("dve", lambda e: e.tensor_scalar(out=MSEL[:, 0:ob_], in0=G[:, 0:ob_],
                                                      scalar1=T8[:, 2:3], scalar2=None, op0=ALU.is_ge),
                     reads=[G.k, T8.k], wadd=[MSEL.k])
            P.op("act", lambda e: e.activation(
                out=DEX[:, 0:nblk], in_=JROW[:, 0:nblk], func=AF.Exp, scale=slope,
                bias=NEGT[:, h * NTO + qt:h * NTO + qt + 1]), reads=[JROW.k, NEGT.k], writes=[DEX.k])
            P.op("dve", lambda e: e.tensor_tensor(out=DM[:, 0:nblk], in0=DEX[:, 0:nblk],
                                                  in1=MSEL[:, 0:nblk], op=ALU.mult),
                 reads=[DEX.k, MSEL.k], writes=[DM.k])

        prologue(0, SELB[0])
        for qt in range(NTO):
            eq = PRE // 128 + qt
            ob_ = eq // 2
            qsl = QS[:, qt * 128:(qt + 1) * 128]
            DM = SELB[qt % 2][4]
            last_kt = 2 * ob_ + (1 if eq % 2 == 1 else 0)
            kts = list(range(last_kt + 1))
            groups = [kts[g0:g0 + 4] for g0 in range(0, len(kts), 4)]

            def emit_qk(gi, groups=groups, qsl=qsl):
                grp = groups[gi]
                sbk = SBANKS[sct[0] % 3]
                sct[0] += 1
                for i_, kt in enumerate(grp):
                    P.op("pe", lambda e, sbk=sbk, i_=i_, kt=kt: e.matmul(
                        sbk.ap[:, i_ * 128:(i_ + 1) * 128], KTS[:, kt * 128:(kt + 1) * 128], qsl, start=True, stop=True),
                        reads=[KTS.k, QS.k], **({"writes": [sbk.k]} if i_ == 0 else {"wadd": [sbk.k]}))
                return sbk
            P.op("pool", lambda e: e.memset(ACC2[:], 0.0), writes=[ACC2.k])
            P.op("pool", lambda e: e.memset(ACC3[:], 0.0), writes=[ACC3.k])
            sbq = [emit_qk(0)]
            if len(groups) > 1:
                sbq.append(emit_qk(1))
            for gi, grp in enumerate(groups):
                sbk = sbq.pop(0)
                if gi + 2 < len(groups):
                    sbq.append(emit_qk(gi + 2))
                if gi == min(1, len(groups) - 1) and qt + 1 < NTO:
                    prologue(qt + 1, SELB[(qt + 1) % 2])
                if gi == min(2, len(groups) - 1) and pend_fin[0] is not None:
                    pend_fin[0]()
                    pend_fin[0] = None
                pts = PTS[sct[1] % 4]
                sct[1] += 1
                n = len(grp)
                P.op("act", lambda e, pts=pts, sbk=sbk, n=n: e.activation(
                    out=pts[:, 0:n * 128], in_=sbk.ap[:, 0:n * 128], func=AF.Exp, scale=float(128 ** -0.5)),
                    reads=[sbk.k], writes=[pts.k])
                if last_kt in grp:
                    i_ = grp.index(last_kt)
                    P.op("pool", lambda e, pts=pts, i_=i_: e.tensor_tensor(
                        out=pts[:, i_ * 128:(i_ + 1) * 128], in0=pts[:, i_ * 128:(i_ + 1) * 128], in1=TRIB[:],
                        op=ALU.mult), reads=[pts.k, TRIB.k], writes=[pts.k])
                blks = sorted(set(kt // 2 for kt in grp))
                obk = OBANKS[sct[2] % 2]
                sct[2] += 1
                for bi, j in enumerate(blks):
                    jk = [kt for kt in grp if kt // 2 == j]
                    for ii, kt in enumerate(jk):
                        i_ = grp.index(kt)
                        P.op("pe", lambda e, obk=obk, pts=pts, i_=i_, kt=kt, ii=ii, jk=jk, bi=bi: e.matmul(
                            obk.ap[:, bi * 256:bi * 256 + 129], pts[:, i_ * 128:(i_ + 1) * 128], VP[:, kt, :],
                            start=(ii == 0), stop=(ii == len(jk) - 1)),
                            reads=[pts.k, VP.k],
                            **({"writes": [obk.k]} if (ii == 0 and bi == 0) else {"wadd": [obk.k]}))
                nb_ = len(blks)
                j0 = blks[0]
                tmp = TMPB[sct[3] % 4]
                sct[3] += 1
                P.op("dve", lambda e, obk=obk, tmp=tmp, nb_=nb_, j0=j0, DM=DM: e.tensor_tensor(
                    out=tmp[:, 0:nb_, :],
                    in0=obk.ap[:, 0:512].rearrange("p (a b) -> p a b", b=256)[:, 0:nb_, 0:129],
                    in1=DM[:, j0:j0 + nb_].unsqueeze(2).to_broadcast([128, nb_, 129]), op=ALU.mult),
                    reads=[obk.k, DM.k], writes=[tmp.k])
                if gi % 3 != 2:
                    P.op("pool", lambda e, tmp=tmp, nb_=nb_: e.tensor_tensor(
                        out=ACC2[:, 0:nb_, :], in0=ACC2[:, 0:nb_, :], in1=tmp[:, 0:nb_, :], op=ALU.add),
                        reads=[tmp.k, ACC2.k], writes=[ACC2.k])
                else:
                    P.op("dve", lambda e, tmp=tmp, nb_=nb_: e.tensor_tensor(
                        out=ACC3[:, 0:nb_, :], in0=ACC3[:, 0:nb_, :], in1=tmp[:, 0:nb_, :], op=ALU.add),
                        reads=[tmp.k, ACC3.k], writes=[ACC3.k])
            P.op("dve", lambda e: e.tensor_tensor(out=ACC3[:], in0=ACC3[:], in1=ACC2[:], op=ALU.add),
                 reads=[ACC2.k, ACC3.k], writes=[ACC3.k])
            P.op("dve", lambda e: e.tensor_tensor(out=ACC[:], in0=ACC3[:, 0, :], in1=ACC3[:, 1, :], op=ALU.add),
                 reads=[ACC3.k], writes=[ACC.k])
            ob16 = OB[qt % 2]
            P.op("dve", lambda e: e.reciprocal(out=RDEN[:], in_=ACC[:, 128:129]), reads=[ACC.k], writes=[RDEN.k])
            P.op("dve", lambda e, ob16=ob16, qt=qt: e.scalar_tensor_tensor(
                out=ob16[:], in0=ACC[:, 0:128], scalar=RDEN[:, 0:1], in1=GSH[:, qt, :], op0=ALU.mult, op1=ALU.mult),
                reads=[ACC.k, RDEN.k, GSH.k], writes=[ob16.k])

            def fin_pe(ob16=ob16, qt=qt, h=h):
                half = (qt // 4) % 2
                j4 = qt % 4
                P.op("pe", lambda e: e.transpose(
                    out=PTB[half][:, j4 * 128:(j4 + 1) * 128], in_=ob16[:], identity=IDB[:]),
                    reads=[ob16.k, IDB.k], **({"writes": [PTk[half]]} if j4 == 0 else {"wadd": [PTk[half]]}))
                if j4 == 3:
                    ost = OST[(qt // 4) % 2]
                    evac_copy(ost[:], PTB[half][:, 0:512], [PTk[half]], writes=[ost.k])
                    P.dma("sp", OT[8 + h, :, (qt - 3) * 128:(qt + 1) * 128], ost[:], reads=[ost.k], wadd=[OT.k],
                          st=ost.k)
            pend_fin[0] = fin_pe
        if pend_fin[0] is not None:
            pend_fin[0]()
            pend_fin[0] = None

    if stop == 5:
        return done()
    P.barrier()
    AR.reset()
    JUNK2 = sb("junk2", [128, 256], BF16)
    GKVT = [sb("gkvt%d" % i, [128, 1536], BF16) for i in range(2)]
    LRA = [sb("lra%d" % i, [17, 128], F32) for i in range(2)]
    KQT = [sb("kqt%d" % i, [128, 8, 128], BF16) for i in range(2)]
    GSL = [sb("gsl%d" % i, [128, 1024], BF16) for i in range(2)]
    E1 = sb("e1", [128, 512], F32)
    SP_ = sb("sp", [128, 512], F32)
    EKTM = sb("ektm", [128, 512], F32)
    KTT = sb("ktt", [128, 512], BF16)
    EQT = sb("eqt", [128, 512], F32)
    EKT = sb("ekt", [128, 512], F32)
    KTF = sb("ktf", [128, 4, 128], BF16)
    QZ = sb("qz", [128, 4, 192], BF16)
    S = sb("S", [128, 1024], F32)
    TS_ = sb("Ts", [128, 1024], F32)
    SBF = [sb("sbf%d" % i, [128, 1024], BF16) for i in range(2)]
    ATB = sb("atb", [128, 512], BF16)
    SSG = sb("ssg", [128, 4], F32)
    GG = sb("gg", [128, 1024], F32)
    OG = sb("og", [128, 1024], BF16)
    OGT = [sb("ogt%d" % i, [128, 8, 128], BF16) for i in range(2)]
    P.op("pool", lambda e: e.memset(S[:], 0.0), writes=[S.k])
    P.op("pool", lambda e: e.memset(SBF[0][:], 0.0), writes=[SBF[0].k])
    P.op("pool", lambda e: e.memset(QZ[:], 0.0), writes=[QZ.k])
    for i in range(2):
        P.op("pool", lambda e, i=i: e.memset(LRA[i][:], 1.0), writes=[LRA[i].k])
    KTT2 = [KTT, sb("ktt_b", [128, 512], BF16)]
    EQT2 = [EQT, sb("eqt_b", [128, 512], F32)]
    KTF2 = [KTF, sb("ktf_b", [128, 4, 128], BF16)]
    QZ2 = [QZ, sb("qz_b", [128, 4, 192], BF16)]
    ATB2 = [ATB, sb("atb_b", [128, 512], BF16)]
    P.op("pool", lambda e: e.memset(QZ2[1][:], 0.0), writes=[QZ2[1].k])

    def gla_prep(ti):
        own = ti >= PRE // 128
        to = ti - PRE // 128
        gkv, lra = GKVT[ti % 2], LRA[ti % 2]
        KTTc, EQTc, KTFc, QZc, ATBc = KTT2[ti % 2], EQT2[ti % 2], KTF2[ti % 2], QZ2[ti % 2], ATB2[ti % 2]
        P.dma("sp", gkv[:], GKV[ti * 128:(ti + 1) * 128, :], reads=[GKV.k], writes=[gkv.k])
        P.dma("sp", lra[0:16, :], LRT[:, ti * 128:(ti + 1) * 128], reads=[LRT.k], wadd=[lra.k])
        if own:
            kqt, gsl = KQT[ti % 2], GSL[ti % 2]
            P.dma("sp", kqt[:, 0:4, :], GKT[:, :, ti * 128:(ti + 1) * 128].rearrange("c p t -> p c t"),
                  reads=[GKT.k], writes=[kqt.k])
            P.dma("sp", kqt[:, 4:8, :], GQT[:, :, to * 128:(to + 1) * 128].rearrange("c p t -> p c t"),
                  reads=[GQT.k], wadd=[kqt.k])
            P.dma("sp", gsl[:], GS[to * 128:(to + 1) * 128, 0:1024], reads=[GS.k], writes=[gsl.k])
        zb = banks[0]
        P.op("pe", lambda e: e.matmul(zb.ap, lra[:], WGA[:], start=True, stop=True),
             reads=[lra.k, WGA.k], writes=[zb.k])
        P.op("act", lambda e: e.activation(out=E1[:], in_=zb.ap, func=AF.Exp, scale=-1.0), reads=[zb.k], writes=[E1.k])
        P.op("act", lambda e: e.activation(out=SP_[:], in_=E1[:], func=AF.Ln, bias=1.0), reads=[E1.k], writes=[SP_.k])
        cb = banks[1]
        P.op("pe", lambda e: e.matmul(cb.ap, U2[:], SP_[:], start=True, stop=True), reads=[U2.k, SP_.k], writes=[cb.k])
        tb = banks[0]
        for hh in range(4):
            P.op("pe", lambda e, hh=hh: e.matmul(tb.ap[:, hh * 128:(hh + 1) * 128], SP_[:, hh * 128:(hh + 1) * 128], U2[:],
                                                 start=True, stop=True), reads=[SP_.k, U2.k],
                 **({"writes": [tb.k]} if hh == 0 else {"wadd": [tb.k]}))
        P.op("act", lambda e: e.activation(out=EKTM[:], in_=cb.ap, func=AF.Exp), reads=[cb.k], writes=[EKTM.k])
        P.op("dve", lambda e: e.tensor_tensor(out=KTTc[:], in0=gkv[:, 0:512], in1=EKTM[:], op=ALU.mult),
             reads=[gkv.k, EKTM.k], writes=[KTTc.k])
        P.op("act", lambda e: e.activation(out=EQTc[:], in_=tb.ap, func=AF.Exp, scale=-1.0), reads=[tb.k], writes=[EQTc.k])
        if own:
            P.op("act", lambda e: e.activation(out=EKT[:], in_=tb.ap, func=AF.Exp), reads=[tb.k], writes=[EKT.k])
            P.op("dve", lambda e: e.tensor_tensor(
                out=KTFc[:], in0=kqt[:, 0:4, :], in1=EKT[:].rearrange("p (a b) -> p a b", b=128), op=ALU.mult),
                reads=[kqt.k, EKT.k], writes=[KTFc.k])
            for c in range(2):
                P.op("dve", lambda e, c=c: e.scalar_tensor_tensor(
                    out=QZc[:, :, c * 128:c * 128 + 64], in0=kqt[:, 4:8, c * 64:(c + 1) * 64], scalar=float(128 ** -0.5),
                    in1=EQTc[:].rearrange("p (a b) -> p a b", b=128)[:, :, c * 64:(c + 1) * 64],
                    op0=ALU.mult, op1=ALU.mult), reads=[kqt.k, EQTc.k], wadd=[QZc.k])
            ab = banks[1]
            for hh in range(4):
                P.op("pe", lambda e, hh=hh: e.matmul(
                    ab.ap[:, hh * 128:(hh + 1) * 128].rearrange("p (a b) -> p a b", b=64), KTFc[:, hh, :],
                    QZc[:, hh, :].rearrange("p (a b) -> p a b", b=64)[:, 0:3:2, :], start=True, stop=True),
                    reads=[KTFc.k, QZc.k], **({"writes": [ab.k]} if hh == 0 else {"wadd": [ab.k]}))
            P.op("dve", lambda e: e.tensor_tensor(
                out=ATBc[:].rearrange("p (a b) -> p a b", b=128), in0=ab.ap.rearrange("p (a b) -> p a b", b=128),
                in1=MSK[:].unsqueeze(1).to_broadcast([128, 4, 128]), op=ALU.mult), reads=[ab.k, MSK.k], writes=[ATBc.k])

    def gla_rec(ti):
        own = ti >= PRE // 128
        to = ti - PRE // 128
        gkv = GKVT[ti % 2]
        KTTc, EQTc, QZc, ATBc = KTT2[ti % 2], EQT2[ti % 2], QZ2[ti % 2], ATB2[ti % 2]

        def state_update(c):
            sout = SBF[(c + 1) % 2]
            for hh in range(4):
                P.op("pe", lambda e, hh=hh: e.matmul(
                    BIG1[:, hh * 256:(hh + 1) * 256], KTTc[c * 64:(c + 1) * 64, hh * 128:(hh + 1) * 128],
                    gkv[c * 64:(c + 1) * 64, 512 + hh * 256:512 + (hh + 1) * 256], start=True, stop=True),
                    reads=[KTTc.k, gkv.k], **({"writes": [b1a, b1b]} if hh == 0 else {"wadd": [b1a, b1b]}))
            P.op("dve", lambda e: e.tensor_tensor(out=TS_[:], in0=BIG1[:, :], in1=S[:], op=ALU.add),
                 reads=[b1a, b1b, S.k], writes=[TS_.k])
            for hh in range(4):
                col = hh * 128 + c * 64 + 63
                P.op("act", lambda e, hh=hh, col=col: e.activation(
                    out=S[:, hh * 256:(hh + 1) * 256], in_=TS_[:, hh * 256:(hh + 1) * 256], func=AF.Copy,
                    scale=EQTc[:, col:col + 1]), reads=[TS_.k, EQTc.k], **({"writes": [S.k]} if hh == 0 else {"wadd": [S.k]}))
            P.op("pool", lambda e: e.tensor_copy(out=sout[:], in_=S[:]), reads=[S.k], writes=[sout.k])
        state_update(0)
        if own:
            for hh in range(4):
                P.op("pe", lambda e, hh=hh: e.matmul(
                    BIG0[:, hh * 256:(hh + 1) * 256], ATBc[:, hh * 128:(hh + 1) * 128],
                    gkv[:, 512 + hh * 256:512 + (hh + 1) * 256], start=True, stop=False),
                    reads=[ATBc.k, gkv.k], **({"writes": [b0a, b0b]} if hh == 0 else {"wadd": [b0a, b0b]}))
                P.op("pe", lambda e, hh=hh: e.matmul(
                    BIG0[:, hh * 256:(hh + 1) * 256], QZc[:, hh, 0:128], SBF[0][:, hh * 256:(hh + 1) * 256],
                    start=False, stop=False), reads=[QZc.k, SBF[0].k], wadd=[b0a, b0b])
                P.op("pe", lambda e, hh=hh: e.matmul(
                    BIG0[:, hh * 256:(hh + 1) * 256], QZc[:, hh, 64:192], SBF[1][:, hh * 256:(hh + 1) * 256],
                    start=False, stop=True), reads=[QZc.k, SBF[1].k], wadd=[b0a, b0b])
        state_update(1)
        if own:
            gsl = GSL[ti % 2]
            for hh in range(4):
                P.op("act", lambda e, hh=hh: e.activation(out=JUNK2[:, 0:256], in_=BIG0[:, hh * 256:(hh + 1) * 256],
                                                          func=AF.Square, accum_out=SSG[:, hh:hh + 1]),
                     reads=[b0a, b0b], writes=[JUNK2.k], wadd=[SSG.k])
            P.op("dve", lambda e: e.tensor_scalar(out=SSG[:], in0=SSG[:], scalar1=1.0 / 256, scalar2=EPS,
                                                  op0=ALU.mult, op1=ALU.add), reads=[SSG.k], writes=[SSG.k])
            P.op("act", lambda e: e.activation(out=SSG[:], in_=SSG[:], func=AF.Sqrt), reads=[SSG.k], writes=[SSG.k])
            P.op("dve", lambda e: e.reciprocal(out=SSG[:], in_=SSG[:]), reads=[SSG.k], writes=[SSG.k])
            P.op("pool", lambda e: e.tensor_tensor(out=GG[:], in0=gsl[:], in1=GOUT[:], op=ALU.mult),
                 reads=[gsl.k, GOUT.k], writes=[GG.k])
            for hh in range(4):
                P.op("dve", lambda e, hh=hh: e.scalar_tensor_tensor(
                    out=OG[:, hh * 256:(hh + 1) * 256], in0=BIG0[:, hh * 256:(hh + 1) * 256], scalar=SSG[:, hh:hh + 1],
                    in1=GG[:, hh * 256:(hh + 1) * 256], op0=ALU.mult, op1=ALU.mult),
                    reads=[b0a, b0b, SSG.k, GG.k], **({"writes": [OG.k]} if hh == 0 else {"wadd": [OG.k]}))
            ogt = OGT[ti % 2]
            for half in range(2):
                for j in range(4):
                    c8 = half * 4 + j
                    P.op("pe", lambda e, c8=c8, half=half, j=j: e.transpose(
                        out=PTB[half][:, j * 128:(j + 1) * 128], in_=OG[:, c8 * 128:(c8 + 1) * 128],
                        identity=IDB[:]), reads=[OG.k, IDB.k],
                        **({"writes": [PTk[half]]} if j == 0 else {"wadd": [PTk[half]]}))
                evac_copy(ogt[:, half * 4:(half + 1) * 4, :],
                          PTB[half][:, 0:512].rearrange("p (a b) -> p a b", b=128), [PTk[half]],
                          **({"writes": [ogt.k]} if half == 0 else {"wadd": [ogt.k]}))
            P.dma("pool", OT[0:8, :, to * 128:(to + 1) * 128].rearrange("c p t -> p c t"), ogt[:], reads=[ogt.k],
                  wadd=[OT.k], st=ogt.k)

    gla_prep(0)
    for ti in range(NT):
        if ti + 1 < NT:
            gla_prep(ti + 1)
        gla_rec(ti)
    if stop == 6:
        return done()
    alloc_gemm()
    WZ = R["WBIG"]
    WSTG = R["WSTG"]
    load_w(w_bg, [(0, 2048)], nch=8, scale=False)
    first = True
    for c in range(8):
        st = WSTG[wstg_n[0] % 3]
        wstg_n[0] += 1
        P.dma("sp", st[:, 0:2048], w_bm[c * 128:(c + 1) * 128, :], writes=[st.k])
        P.op("pool", lambda e, st=st, c=c: e.tensor_copy(out=WZ[:, 8 + c, 0:2048], in_=st[:, 0:2048]),
             reads=[st.k], wadd=[WZ.k])
    OTG = R["HTG"]
    SGG = [sb("sgg%d" % i, [128, 32, 512], BF16) for i in range(1)]
    T1 = [sb("t1_%d" % i, [128, 512], BF16) for i in range(2)]
    T2 = [sb("t2_%d" % i, [128, 512], BF16) for i in range(2)]
    MTS = [sb("mts%d" % i, [128, 512], BF16) for i in range(2)]
    for g in range(OWN // 512):
        otg = OTG[g % 2]
        sgg = SGG[0]
        P.dma("sp", otg[:], OT[:, :, g * 512:(g + 1) * 512].rearrange("c p t -> p c t"), reads=[OT.k], writes=[otg.k])
        for c0_ in (0, 16):
            P.dma("sp", sgg[:, c0_:c0_ + 16, :], SGT[c0_:c0_ + 16, :, g * 512:(g + 1) * 512].rearrange("c p t -> p c t"),
                  reads=[SGT.k], **({"writes": [sgg.k]} if c0_ == 0 else {"wadd": [sgg.k]}))
        for n in range(16):
            bg, bm = next_bank(), next_bank()
            for br, bnk in ((0, bg), (1, bm)):
                for c in range(8):
                    P.op("pe", lambda e, bnk=bnk, br=br, c=c, n=n, otg=otg: e.matmul(
                        bnk.ap, WZ[:, br * 8 + c, n * 128:(n + 1) * 128], otg[:, br * 8 + c, :],
                        start=(c == 0), stop=(c == 7)), reads=[WZ.k, otg.k],
                        **({"writes": [bnk.k]} if c == 0 else {"wadd": [bnk.k]}))
            t1, t2, mts = T1[n % 2], T2[n % 2], MTS[n % 2]
            P.op("dve", lambda e, t1=t1, bg=bg, n=n, sgg=sgg: e.tensor_tensor(out=t1[:], in0=bg.ap, in1=sgg[:, n, :], op=ALU.mult),
                 reads=[bg.k, sgg.k], writes=[t1.k])
            P.op("dve", lambda e, t2=t2, bm=bm, n=n, sgg=sgg: e.tensor_tensor(out=t2[:], in0=bm.ap, in1=sgg[:, 16 + n, :], op=ALU.mult),
                 reads=[bm.k, sgg.k], writes=[t2.k])
            P.op("pool", lambda e, t1=t1, t2=t2, mts=mts: e.tensor_tensor(out=mts[:], in0=t1[:], in1=t2[:], op=ALU.add),
                 reads=[t1.k, t2.k], writes=[mts.k])
            P.dma("pool", MT[n, :, g * 512:(g + 1) * 512], mts[:], reads=[mts.k], wadd=[MT.k], st=mts.k)

    if stop == 7:
        return done()
    alloc_gemm()
    WBIG = R["WBIG"]
    load_w(w_o, [(0, 2048)], scale=False)
    XT = [sb("xtz%d" % i, [128, D], F32) for i in range(2)]
    YT = [sb("yt%d" % i, [128, D], F32) for i in range(2)]
    for g in range(OWN // 512):
        mtg = R["HTG"][g % 2]
        P.dma("sp", mtg[:], MT[:, :, g * 512:(g + 1) * 512].rearrange("c p t -> p c t"), reads=[MT.k], writes=[mtg.k])
        for t4 in range(4):
            ti = g * 4 + t4
            xt, yt = XT[ti % 2], YT[ti % 2]
            P.dma("sp", xt[:], x[PRE + ti * 128:PRE + (ti + 1) * 128, :], writes=[xt.k])
            for ng in range(4):
                b = next_bank()
                for c in range(NCH):
                    P.op("pe", lambda e, b=b, c=c, mtg=mtg, t4=t4, ng=ng: e.matmul(
                        b.ap, mtg[:, c, t4 * 128:(t4 + 1) * 128], WBIG[:, c, ng * 512:(ng + 1) * 512],
                        start=(c == 0), stop=(c == NCH - 1)), reads=[mtg.k, WBIG.k],
                        **({"writes": [b.k]} if c == 0 else {"wadd": [b.k]}))
                P.op("dve", lambda e, b=b, yt=yt, xt=xt, ng=ng: e.tensor_tensor(
                    out=yt[:, ng * 512:(ng + 1) * 512], in0=b.ap, in1=xt[:, ng * 512:(ng + 1) * 512], op=ALU.add),
                    reads=[b.k, xt.k], **({"writes": [yt.k]} if ng == 0 else {"wadd": [yt.k]}))
            P.dma("pool", y[ti * 128:(ti + 1) * 128, :], yt[:], reads=[yt.k], st=yt.k)

    P.finish()
    P.emit()
    return nc


def make_consts(EXT, OWN):
    NTO = OWN // 128
    PRE = EXT - OWN
    p = np.arange(128)
    same = (p[:, None] // 64) == (p[None, :] // 64)
    le = p[:, None] <= p[None, :]
    msk = (same & le).astype(np.float32)
    slopes = 2.0 ** (-8.0 * np.arange(1, 9, dtype=np.float64) / 8)
    sc = np.zeros((128, 16), np.float32)
    negt = np.zeros((128, 8 * NTO), np.float32)
    for h in range(8):
        sc[:, 2 * h] = np.exp(slopes[h] * (p - 128.0))
        sc[:, 2 * h + 1] = np.exp(slopes[h] * (p * 1.0))
        for qt in range(NTO):
            negt[:, h * NTO + qt] = -slopes[h] * (PRE + qt * 128 + p)
    return {
        "c_id": np.eye(128, dtype=np.float32),
        "c_u2": (msk / 16.0).astype(np.float32),
        "c_msk": msk,
        "c_tri": le.astype(np.float32),
        "c_jrow": np.broadcast_to((256.0 * np.arange(64) + 128.0).astype(np.float32), (128, 64)).copy(),
        "c_sc": sc,
        "c_negt": negt,
    }


def make_in_maps(inputs, EXT, OWN, nseg, ncores=8):
    x = np.asarray(inputs["x"], np.float32)
    B = x.shape[0]
    consts = make_consts(EXT, OWN)
    shared = dict(consts)
    shared["w_in"] = np.ascontiguousarray(np.asarray(inputs["w_in"], np.float32)[0])
    shared["w_bg"] = np.ascontiguousarray(np.asarray(inputs["w_branch_gla"], np.float32)[0])
    shared["w_bm"] = np.ascontiguousarray(np.asarray(inputs["w_branch_moba"], np.float32)[0])
    shared["w_o"] = np.ascontiguousarray(np.asarray(inputs["w_out"], np.float32)[0])
    shared["c_ng"] = np.ascontiguousarray(np.asarray(inputs["norm_g"], np.float32)[0].reshape(NCH, 128).T)
    shared["c_wga"] = np.concatenate([np.asarray(inputs["w_gla_gate"], np.float32)[0],
                                      np.asarray(inputs["b_gla_gate"], np.float32)[0][None, :]], axis=0)
    shared["c_gout"] = np.ascontiguousarray(np.broadcast_to(
        np.tile(np.asarray(inputs["gla_out_g"], np.float32)[0], 4)[None, :], (128, 1024)))
    shared["c_qg"] = np.ascontiguousarray(np.stack([np.asarray(inputs["q_norm_g"], np.float32)[0],
                                                    np.asarray(inputs["k_norm_g"], np.float32)[0]], axis=1))
    maps = []
    for c in range(ncores):
        b, i = (c // nseg) % B, c % nseg
        end = (i + 1) * OWN
        pad = EXT - end
        xe = np.zeros((EXT, D), np.float32)
        xe[pad:] = x[b, :end]
        bval = np.zeros((128, 64), np.float32)
        bval[:, :pad // 256] = NEG
        m = dict(shared)
        m["x"] = xe
        m["c_bval"] = bval
        maps.append(m)
    return maps


_NC_CACHE = {}


def kernel(**inputs):
    EXT, OWN, nseg = 16384, 4096, 4
    x = np.asarray(inputs["x"])
    B, S, _ = x.shape
    key = (EXT, OWN)
    if key not in _NC_CACHE:
        _NC_CACHE[key] = build(EXT, OWN)
    nc = _NC_CACHE[key]
    maps = make_in_maps(inputs, EXT, OWN, nseg)
    res = run_bass_kernel_spmd(nc, maps, core_ids=list(range(8)))
    out = np.empty((B, S, D), np.float32)
    for c in range(8):
        b, i = c // nseg, c % nseg
        out[b, i * OWN:(i + 1) * OWN] = res.results[c]["y"]
    return out
```
